# Optimizing a Trainium2 kernel written in Bass

```python
import jax, jax.numpy as jnp
from jax import lax
import numpy as np

D_MODEL = 1024
BATCH = 16
SEQ = 2048
DEPTH = 2

N_MIXERS = 2
D_RNN = 1280
LRU_BLOCKS = 16
LRU_BLOCK_W = D_RNN // LRU_BLOCKS
CONV_W = 4
LRU_C = 8.0
N_HEADS = 16
N_KV_HEADS = 4
HEAD_DIM = 64
GROUP = N_HEADS // N_KV_HEADS
WINDOW = 128
ATT_BLOCK = WINDOW
Q_DIM = N_HEADS * HEAD_DIM
KV_DIM = N_KV_HEADS * HEAD_DIM
QKV_DIM = Q_DIM + 2 * KV_DIM
N_GROUPS = 4
EXPERTS_PER_GROUP = 8
N_EXPERTS = N_GROUPS * EXPERTS_PER_GROUP
TOP_K = 2
D_EXPERT = 512
MOE_BLOCK = 128
ALPHA = (2 * DEPTH) ** 0.25
BETA = (8 * DEPTH) ** -0.25
LN_EPS = 1e-5
N_REC_LAYERS = (DEPTH + N_MIXERS - 1) // N_MIXERS
N_ATT_LAYERS = DEPTH // N_MIXERS

kernel_name = 'hybrid_rglru_swa_sink_alibi_hmoe'


def layer_norm(x, g, b):
    xf = x.astype(jnp.float32)
    mu = jnp.mean(xf, axis=-1, keepdims=True)
    xc = xf - mu
    var = jnp.mean(xc * xc, axis=-1, keepdims=True)
    y = xc * lax.rsqrt(var + LN_EPS) * g.astype(jnp.float32) + b.astype(jnp.float32)
    return y.astype(x.dtype)


def _linear_recurrence_combine(c1, c2):
    a1, b1 = c1
    a2, b2 = c2
    return a1 * a2, a2 * b1 + b2


def rglru_block(x, w_in, conv_w, conv_b, w_r, b_r, w_i, b_i, lam, w_out):
    B, S, _ = x.shape
    gate, xr = jnp.split(x @ w_in, 2, axis=-1)
    xp = jnp.pad(xr, ((0, 0), (CONV_W - 1, 0), (0, 0)))
    xc = conv_b + sum(xp[:, k:k + S] * conv_w[k] for k in range(CONV_W))
    xb = xc.reshape(B, S, LRU_BLOCKS, LRU_BLOCK_W)
    r = jax.nn.sigmoid(jnp.einsum('bsnk,nkj->bsnj', xb, w_r).reshape(B, S, D_RNN) + b_r)
    i = jax.nn.sigmoid(jnp.einsum('bsnk,nkj->bsnj', xb, w_i).reshape(B, S, D_RNN) + b_i)
    log_a = -LRU_C * r.astype(jnp.float32) * jax.nn.softplus(-lam.astype(jnp.float32))
    a = jnp.exp(log_a)
    u = jnp.sqrt(-jnp.expm1(2.0 * log_a)) * (i * xc).astype(jnp.float32)
    _, h = lax.associative_scan(_linear_recurrence_combine, (a, u), axis=1)
    y = h.astype(x.dtype) * jax.nn.gelu(gate)
    return y @ w_out


def alibi_slopes():
    return 2.0 ** (-8.0 * jnp.arange(1, N_HEADS + 1, dtype=jnp.float32) / N_HEADS)


def _with_prev_block(t):
    prev = jnp.concatenate([jnp.zeros_like(t[:, :1]), t[:, :-1]], axis=1)
    return jnp.concatenate([prev, t], axis=2)


def swa_sink_alibi(x, w_qkv, sinks, w_o):
    B, S, _ = x.shape
    NB = S // ATT_BLOCK
    qkv = x @ w_qkv
    q = qkv[..., :Q_DIM].reshape(B, NB, ATT_BLOCK, N_KV_HEADS, GROUP, HEAD_DIM) * (HEAD_DIM ** -0.5)
    k = qkv[..., Q_DIM:Q_DIM + KV_DIM].reshape(B, NB, ATT_BLOCK, N_KV_HEADS, HEAD_DIM)
    v = qkv[..., Q_DIM + KV_DIM:].reshape(B, NB, ATT_BLOCK, N_KV_HEADS, HEAD_DIM)
    kk = _with_prev_block(k)
    vv = _with_prev_block(v)
    s = jnp.einsum('bnqkgd,bnskd->bnkgqs', q, kk).astype(jnp.float32)
    qi = jnp.arange(ATT_BLOCK)[:, None]
    sj = jnp.arange(2 * ATT_BLOCK)[None, :]
    dist = qi - sj + ATT_BLOCK
    valid = (dist >= 0) & (dist < WINDOW)
    not_before_start = (jnp.arange(NB)[:, None, None] > 0) | (sj[None] >= ATT_BLOCK)
    mask = valid[None] & not_before_start
    slopes = alibi_slopes().reshape(N_KV_HEADS, GROUP)
    s = s - slopes[:, :, None, None] * dist.astype(jnp.float32)
    s = jnp.where(mask[None, :, None, None], s, -jnp.inf)
    sink = sinks.astype(jnp.float32).reshape(N_KV_HEADS, GROUP)[None, None, :, :, None, None]
    m = jnp.maximum(jnp.max(s, axis=-1, keepdims=True), sink)
    p = jnp.exp(s - m)
    denom = jnp.sum(p, axis=-1, keepdims=True) + jnp.exp(sink - m)
    o = jnp.einsum('bnkgqs,bnskd->bnqkgd', (p / denom).astype(x.dtype), vv)
    return o.reshape(B, S, Q_DIM) @ w_o


def hier_moe(x, w_rg, b_rg, w_re, b_re, w1, w3, w2):
    B, S, D = x.shape
    T = B * S
    TK = T * TOP_K
    xf = x.reshape(T, D)
    g_logits = (xf @ w_rg + b_rg).astype(jnp.float32)
    g_idx = jnp.argmax(g_logits, axis=-1)
    g_gate = jnp.take_along_axis(jax.nn.softmax(g_logits, axis=-1), g_idx[:, None], axis=1)
    e_logits = (xf @ w_re + b_re).astype(jnp.float32).reshape(T, N_GROUPS, EXPERTS_PER_GROUP)
    e_in_group = jnp.take_along_axis(e_logits, g_idx[:, None, None], axis=1)[:, 0]
    top_v, top_i = lax.top_k(e_in_group, TOP_K)
    gates = jax.nn.softmax(top_v, axis=-1) * g_gate
    expert = g_idx[:, None] * EXPERTS_PER_GROUP + top_i
    flat_e = expert.reshape(TK)
    order = jnp.argsort(flat_e)
    sorted_e = flat_e[order]
    counts = jnp.bincount(flat_e, length=N_EXPERTS)
    padded = ((counts + MOE_BLOCK - 1) // MOE_BLOCK) * MOE_BLOCK
    starts = jnp.cumsum(counts) - counts
    pends = jnp.cumsum(padded)
    pstarts = pends - padded
    dest = pstarts[sorted_e] + (jnp.arange(TK) - starts[sorted_e])
    slot_dest = jnp.zeros_like(dest).at[order].set(dest)
    n_blocks = -(-(TK + N_EXPERTS * (MOE_BLOCK - 1)) // MOE_BLOCK)
    n_rows = n_blocks * MOE_BLOCK
    xbuf = jnp.zeros((n_rows, D), x.dtype).at[slot_dest].set(jnp.repeat(xf, TOP_K, axis=0))
    block_start = jnp.arange(n_blocks) * MOE_BLOCK
    block_e = jnp.minimum(jnp.sum(block_start[:, None] >= pends[None, :], axis=1), N_EXPERTS - 1)

    def expert_block(args):
        xb, e = args
        hdn = jax.nn.silu(xb @ w1[e]) * (xb @ w3[e])
        return hdn @ w2[e]

    ybuf = lax.map(expert_block, (xbuf.reshape(n_blocks, MOE_BLOCK, D), block_e)).reshape(n_rows, D)
    y_slots = ybuf[slot_dest].reshape(T, TOP_K, D)
    out = jnp.einsum('tk,tkd->td', gates.astype(x.dtype), y_slots)
    return out.reshape(B, S, D)


def setup_inputs(seed: int = 0) -> dict:
    key = jax.random.key(seed)
    ks = jax.random.split(key, 24)

    def nrm(k, shape, scale):
        return jax.random.normal(k, shape, jnp.float32) * scale

    x = nrm(ks[0], (BATCH, SEQ, D_MODEL), 1.0)
    rec_w_in = nrm(ks[1], (N_REC_LAYERS, D_MODEL, 2 * D_RNN), D_MODEL ** -0.5)
    rec_conv_w = nrm(ks[2], (N_REC_LAYERS, CONV_W, D_RNN), CONV_W ** -0.5)
    rec_conv_b = nrm(ks[3], (N_REC_LAYERS, D_RNN), 0.01)
    rec_w_r = nrm(ks[4], (N_REC_LAYERS, LRU_BLOCKS, LRU_BLOCK_W, LRU_BLOCK_W), LRU_BLOCK_W ** -0.5)
    rec_b_r = nrm(ks[5], (N_REC_LAYERS, D_RNN), 0.01)
    rec_w_i = nrm(ks[6], (N_REC_LAYERS, LRU_BLOCKS, LRU_BLOCK_W, LRU_BLOCK_W), LRU_BLOCK_W ** -0.5)
    rec_b_i = nrm(ks[7], (N_REC_LAYERS, D_RNN), 0.01)
    a_c = jax.random.uniform(ks[8], (N_REC_LAYERS, D_RNN), jnp.float32, minval=0.9, maxval=0.999)
    a0 = a_c ** (1.0 / LRU_C)
    rec_lambda = jnp.log(a0) - jnp.log1p(-a0)
    rec_w_out = nrm(ks[9], (N_REC_LAYERS, D_RNN, D_MODEL), BETA * D_RNN ** -0.5)
    col_scale = jnp.concatenate([jnp.ones((Q_DIM + KV_DIM,), jnp.float32), jnp.full((KV_DIM,), BETA, jnp.float32)])
    att_w_qkv = nrm(ks[10], (N_ATT_LAYERS, D_MODEL, QKV_DIM), D_MODEL ** -0.5) * col_scale
    att_sinks = nrm(ks[11], (N_ATT_LAYERS, N_HEADS), 0.5)
    att_w_o = nrm(ks[12], (N_ATT_LAYERS, Q_DIM, D_MODEL), BETA * Q_DIM ** -0.5)
    moe_w_group = nrm(ks[13], (DEPTH, D_MODEL, N_GROUPS), D_MODEL ** -0.5)
    moe_b_group = nrm(ks[14], (DEPTH, N_GROUPS), 0.01)
    moe_w_expert = nrm(ks[15], (DEPTH, D_MODEL, N_EXPERTS), D_MODEL ** -0.5)
    moe_b_expert = nrm(ks[16], (DEPTH, N_EXPERTS), 0.01)
    moe_w1 = nrm(ks[17], (DEPTH, N_EXPERTS, D_MODEL, D_EXPERT), D_MODEL ** -0.5)
    moe_w3 = nrm(ks[18], (DEPTH, N_EXPERTS, D_MODEL, D_EXPERT), D_MODEL ** -0.5)
    moe_w2 = nrm(ks[19], (DEPTH, N_EXPERTS, D_EXPERT, D_MODEL), BETA * D_EXPERT ** -0.5)
    ln_g = 1.0 + nrm(ks[20], (DEPTH, 2, D_MODEL), 0.02)
    ln_b = nrm(ks[21], (DEPTH, 2, D_MODEL), 0.02)
    return {'x': x, 'rec_w_in': rec_w_in, 'rec_conv_w': rec_conv_w, 'rec_conv_b': rec_conv_b,
            'rec_w_r': rec_w_r, 'rec_b_r': rec_b_r, 'rec_w_i': rec_w_i, 'rec_b_i': rec_b_i,
            'rec_lambda': rec_lambda, 'rec_w_out': rec_w_out, 'att_w_qkv': att_w_qkv,
            'att_sinks': att_sinks, 'att_w_o': att_w_o, 'moe_w_group': moe_w_group,
            'moe_b_group': moe_b_group, 'moe_w_expert': moe_w_expert, 'moe_b_expert': moe_b_expert,
            'moe_w1': moe_w1, 'moe_w3': moe_w3, 'moe_w2': moe_w2, 'ln_g': ln_g, 'ln_b': ln_b}


def reference(x, rec_w_in, rec_conv_w, rec_conv_b, rec_w_r, rec_b_r, rec_w_i, rec_b_i, rec_lambda,
              rec_w_out, att_w_qkv, att_sinks, att_w_o, moe_w_group, moe_b_group, moe_w_expert,
              moe_b_expert, moe_w1, moe_w3, moe_w2, ln_g, ln_b):
    for layer in range(DEPTH):
        j = layer // N_MIXERS
        if layer % N_MIXERS == 0:
            h = rglru_block(x, rec_w_in[j], rec_conv_w[j], rec_conv_b[j], rec_w_r[j], rec_b_r[j],
                            rec_w_i[j], rec_b_i[j], rec_lambda[j], rec_w_out[j])
        else:
            h = swa_sink_alibi(x, att_w_qkv[j], att_sinks[j], att_w_o[j])
        x = layer_norm(ALPHA * x + h, ln_g[layer, 0], ln_b[layer, 0])
        f = hier_moe(x, moe_w_group[layer], moe_b_group[layer], moe_w_expert[layer], moe_b_expert[layer],
                     moe_w1[layer], moe_w3[layer], moe_w2[layer])
        x = layer_norm(ALPHA * x + f, ln_g[layer, 1], ln_b[layer, 1])
    return x
```

```python
from contextlib import ExitStack

import numpy as np
import concourse.bass as bass
import concourse.mybir as mybir
from concourse.bass_utils import run_bass_kernel_spmd

F32 = mybir.dt.float32
BF16 = mybir.dt.bfloat16
I32 = mybir.dt.int32
AF = mybir.ActivationFunctionType
ALU = mybir.AluOpType
AX = mybir.AxisListType

NCORES = 8
D = 1024
SEQ = 2048
TOK = 4096
NT = TOK // 128
NE = 32
DE = 512
CAP = 384
NSLOT = NE * CAP
ALPHA = float((2 * 2) ** 0.25)
LN_EPS = 1e-5
D_RNN = 1280
RC = 80
NRC = D_RNN // RC
NH = 16
NKV = 4
HD = 64


class Buf:
    __slots__ = ("name", "writers", "readers")

    def __init__(self, name):
        self.name = name
        self.writers = {}
        self.readers = {}


def _merge(dst, src):
    for k, (s, v) in src.items():
        if k not in dst or dst[k][1] < v:
            dst[k] = (s, v)


class K:
    def __init__(self, nc, es):
        self.nc = nc
        self.es = es
        self.es_root = es
        self.engs = {"pe": nc.tensor, "dve": nc.vector, "act": nc.scalar, "pool": nc.gpsimd, "sp": nc.sync}
        self.sem = {}
        self.cnt = {}
        self.waited = {n: {} for n in self.engs}
        for n in self.engs:
            self.sem[n] = es.enter_context(nc.semaphore("s_" + n))
            self.cnt[n] = 0
        self.ring = {}
        for q, n in (("sp", 10), ("pool", 10), ("act", 4)):
            self.ring[q] = [[es.enter_context(nc.semaphore(f"d_{q}{i}")), 0] for i in range(n)]
        self.ring_pos = {q: 0 for q in self.ring}
        self.nbuf = 0

    def sb(self, name, shape, dt):
        return self.es.enter_context(self.nc.sbuf_tensor(name, list(shape), dt))

    def ps(self, name, shape, dt):
        return self.es.enter_context(self.nc.psum_tensor(name, list(shape), dt))

    def dram(self, name, shape, dt):
        return self.nc.dram_tensor(name, list(shape), dt, kind="Internal").ap()

    def buf(self, name=None):
        self.nbuf += 1
        return Buf(name or f"b{self.nbuf}")

    def _wait(self, engname, deps):
        e = self.engs[engname]
        w = self.waited[engname]
        for k, (s, v) in deps.items():
            if engname == "pe" and k == id(self.sem["pe"]):
                continue
            if w.get(k, 0) >= v:
                continue
            e.wait_ge(s, v)
            w[k] = v

    def _deps(self, reads, writes):
        deps = {}
        for b in reads:
            _merge(deps, b.writers)
        for b in writes:
            _merge(deps, b.writers)
            _merge(deps, b.readers)
        return deps

    def _record(self, tok, reads, writes):
        k, s, v = tok
        for b in reads:
            if k not in b.readers or b.readers[k][1] < v:
                b.readers[k] = (s, v)
        for b in writes:
            if b.readers:
                b.writers = {}
                b.readers = {}
            b.writers[k] = (s, v)

    def op(self, engname, fn, reads=(), writes=()):
        deps = self._deps(reads, writes)
        self._wait(engname, deps)
        inst = fn(self.engs[engname])
        s = self.sem[engname]
        self.cnt[engname] += 1
        inst.then_inc(s, 1)
        tok = (id(s), s, self.cnt[engname])
        self._record(tok, reads, writes)
        return tok

    def dma(self, q, fn, reads=(), writes=()):
        deps = self._deps(reads, writes)
        ring = self.ring[q]
        pos = self.ring_pos[q]
        self.ring_pos[q] = (pos + 1) % len(ring)
        ent = ring[pos]
        s = ent[0]
        if ent[1] > 0:
            deps[id(s)] = (s, ent[1])
        self._wait(q, deps)
        inst = fn(self.engs[q])
        ent[1] += 16
        inst.then_inc(s, 16)
        tok = (id(s), s, ent[1])
        self._record(tok, reads, writes)
        return tok

    def finish(self, bufs):
        deps = {}
        for b in bufs:
            _merge(deps, b.writers)
        self._wait("sp", deps)


C_IDENT = 0
C_UPPER = 128
C_ONES = 256
C_EBASE = 384
NCONST = 416


def make_consts():
    c = np.zeros((128, NCONST), np.float32)
    c[:, C_IDENT:C_IDENT + 128] = np.eye(128, dtype=np.float32)
    c[:, C_UPPER:C_UPPER + 128] = np.triu(np.ones((128, 128), np.float32), 1)
    c[:, C_ONES:C_ONES + 128] = 1.0
    c[:, C_EBASE:C_EBASE + NE] = (np.arange(NE, dtype=np.float32) * CAP)[None, :]
    return c


class Common:
    def __init__(self, k, consts_ap):
        self.k = k
        nc = k.nc
        self.cf = k.sb("cf", [128, NCONST], F32)
        self.b_cf = k.buf("cf")
        k.dma("sp", lambda e: e.dma_start(out=self.cf[:], in_=consts_ap), writes=[self.b_cf])
        self.cb = k.sb("cb", [128, 384], BF16)
        self.b_cb = k.buf("cb")
        k.op("dve", lambda e: e.tensor_copy(out=self.cb[:], in_=self.cf[:, 0:384]),
             reads=[self.b_cf], writes=[self.b_cb])
        self.ident_f = self.cf[:, C_IDENT:C_IDENT + 128]
        self.ident_b = self.cb[:, C_IDENT:C_IDENT + 128]
        self.upper_b = self.cb[:, C_UPPER:C_UPPER + 128]
        self.ones_b = self.cb[:, C_ONES:C_ONES + 128]
        self.ebase = self.cf[:, C_EBASE:C_EBASE + NE]
        self.bound_reg = nc.gpsimd.to_reg(NSLOT - 1)
        k.mhalf = k.sb("ln_mhalf", [128, 1], F32)
        k.op("pool", lambda q: q.memset(k.mhalf[:], -0.5))
        barrier(k)


def moe_phase(k, cm, tag, xin, b_xin, xout, b_xout, w_rg, b_rg, w_re, b_re, w1, w3, w2, ln_g, ln_b,
              xbuf, ybuf):
    nc = k.nc
    es_outer = k.es
    with ExitStack() as es:
        k.es = es
        T = tag
        wr = k.sb(T + "wr", [128, 8, 36], F32)
        b_wr = k.buf()
        k.dma("sp", lambda e: e.dma_start(out=wr[:, :, 0:4], in_=w_rg.rearrange("(c p) n -> p c n", p=128)),
              writes=[b_wr])
        k.dma("sp", lambda e: e.dma_start(out=wr[:, :, 4:36], in_=w_re.rearrange("(c p) n -> p c n", p=128)),
              writes=[b_wr])
        rbias = k.sb(T + "rbias", [128, 36], F32)
        b_rbias = k.buf()
        k.dma("sp", lambda e: e.dma_start(out=rbias[:, 0:4], in_=b_rg.partition_broadcast(128)), writes=[b_rbias])
        k.dma("sp", lambda e: e.dma_start(out=rbias[:, 4:36], in_=b_re.partition_broadcast(128)), writes=[b_rbias])
        gam = k.sb(T + "gam", [128, D], F32)
        bet = k.sb(T + "bet", [128, D], F32)
        b_gb = k.buf()
        k.dma("sp", lambda e: e.dma_start(out=gam[:], in_=ln_g.partition_broadcast(128)), writes=[b_gb])
        k.dma("sp", lambda e: e.dma_start(out=bet[:], in_=ln_b.partition_broadcast(128)), writes=[b_gb])
        idx_all = k.sb(T + "idx", [128, NT, 2], I32)
        gate_all = k.sb(T + "gate", [128, NT, 2], F32)
        b_idx = k.buf()
        b_gate = k.buf()

        NWB = 3
        w1t = [k.sb(f"{T}w1_{i}", [128, 8, DE], BF16) for i in range(NWB)]
        w3t = [k.sb(f"{T}w3_{i}", [128, 8, DE], BF16) for i in range(NWB)]
        w2t = [k.sb(f"{T}w2_{i}", [128, 4, D], BF16) for i in range(NWB)]
        b_w13 = [k.buf() for _ in range(NWB)]
        b_w2 = [k.buf() for _ in range(NWB)]

        def load_weights(e):
            s = e % NWB
            k.dma("pool", lambda g: g.dma_start(out=w1t[s][:], in_=w1[e].rearrange("(c p) n -> p c n", p=128)),
                  writes=[b_w13[s]])
            k.dma("pool", lambda g: g.dma_start(out=w3t[s][:], in_=w3[e].rearrange("(c p) n -> p c n", p=128)),
                  writes=[b_w13[s]])
            k.dma("pool", lambda g: g.dma_start(out=w2t[s][:], in_=w2[e].rearrange("(c p) n -> p c n", p=128)),
                  writes=[b_w2[s]])

        for e in range(NWB):
            load_weights(e)

        with ExitStack() as es2:
            k.es = es2
            lg_all = k.sb(T + "lg_all", [128, NT, 36], F32)
            b_lg = [k.buf() for _ in range(NT)]
            xt = [k.sb(f"{T}xt{i}", [128, D], F32) for i in range(3)]
            b_xt = [k.buf() for _ in range(3)]
            xT = [k.sb(f"{T}xT{i}", [128, 8, 128], F32) for i in range(3)]
            b_xTa = [k.buf() for _ in range(3)]
            b_xTb = [k.buf() for _ in range(3)]
            psT = [k.ps(f"{T}psT{i}", [128, 8, 128], F32) for i in range(2)]
            b_psT = [k.buf() for _ in range(2)]
            psL = [k.ps(f"{T}psL{i}", [128, 512], F32) for i in range(2)]
            b_psL = [k.buf() for _ in range(2)]

            def lg0(i):
                s = i % 3
                k.dma("sp", lambda e: e.dma_start(out=xt[s][:], in_=xin[i * 128:(i + 1) * 128, :]),
                      reads=[b_xin], writes=[b_xt[s]])

            def lg1(i):
                s, p = i % 3, i % 2
                for c in range(8):
                    k.op("pe", lambda e: e.transpose(out=psT[p][:, c, :], in_=xt[s][:, c * 128:(c + 1) * 128],
                                                     identity=cm.ident_f),
                         reads=[b_xt[s], cm.b_cf], writes=[b_psT[p]])

            def lg2(i):
                s, p = i % 3, i % 2
                k.op("act", lambda e: e.activation(out=xT[s][:, 0:4, :], in_=psT[p][:, 0:4, :], func=AF.Copy),
                     reads=[b_psT[p]], writes=[b_xTa[s]])
                k.op("dve", lambda e: e.tensor_copy(out=xT[s][:, 4:8, :], in_=psT[p][:, 4:8, :]),
                     reads=[b_psT[p]], writes=[b_xTb[s]])

            def lg3(i):
                s, p = i % 3, i % 2
                for c in range(8):
                    k.op("pe", lambda e: e.matmul(psL[p][:, 0:36], lhsT=xT[s][:, c, :], rhs=wr[:, c, :],
                                                  start=(c == 0), stop=(c == 7)),
                         reads=[b_xTa[s], b_xTb[s], b_wr], writes=[b_psL[p]])

            def lg4(i):
                p = i % 2
                k.op("dve", lambda e: e.tensor_tensor(out=lg_all[:, i, :], in0=psL[p][:, 0:36], in1=rbias[:],
                                                      op=ALU.add),
                     reads=[b_psL[p], b_rbias], writes=[b_lg[i]])

            run_pipeline(NT, [lg0, lg1, lg2, lg3, lg4])

            def st(name, shape, dt=F32):
                return k.sb(T + name, shape, dt)

            lgg = lg_all[:, :, 0:4]
            lge = lg_all[:, :, 4:36]
            gmax = st("gmax", [128, NT])
            ohg = st("ohg", [128, NT, 4])
            egs = st("egs", [128, NT, 4])
            gsum = st("gsum", [128, NT])
            ggate = st("ggate", [128, NT])
            pen = st("pen", [128, NT, 4])
            me = st("me", [128, NT, 32])
            me2 = me
            v1 = st("v1", [128, NT])
            v2 = st("v2", [128, NT])
            sel1 = st("sel1", [128, NT, 32])
            sel2 = st("sel2", [128, NT, 32])
            dd = st("dd", [128, NT])
            p1 = st("p1", [128, NT])
            g1 = st("g1", [128, NT])
            g2 = st("g2", [128, NT])
            A = st("A", [128, NT, 32], BF16)
            dest = st("dest", [128, NT, 32])
            ov = st("ov", [128, NT, 32])
            tmp = st("tmp", [128, NT, 32])
            idxf = st("idxf", [128, NT, 2])
            ovs = st("ovs", [128, NT, 2])
            b_r = k.buf()

            def dv(fn, extra_r=()):
                k.op("dve", fn, reads=[b_r, *extra_r], writes=[b_r])

            def bc(ap, n):
                return ap.unsqueeze(2).to_broadcast([128, NT, n])

            dv(lambda e: e.tensor_reduce(out=gmax[:], in_=lgg, axis=AX.X, op=ALU.max), extra_r=b_lg)
            dv(lambda e: e.tensor_tensor(out=ohg[:], in0=lgg, in1=bc(gmax[:], 4), op=ALU.is_ge))
            dv(lambda e: e.tensor_tensor(out=egs[:], in0=lgg, in1=bc(gmax[:], 4), op=ALU.subtract))
            k.op("act", lambda e: e.activation(out=egs[:], in_=egs[:], func=AF.Exp), reads=[b_r], writes=[b_r])
            dv(lambda e: e.tensor_reduce(out=gsum[:], in_=egs[:], axis=AX.X, op=ALU.add))
            dv(lambda e: e.reciprocal(out=ggate[:], in_=gsum[:]))
            dv(lambda e: e.tensor_scalar(out=pen[:], in0=ohg[:], scalar1=1e30, scalar2=-1e30,
                                         op0=ALU.mult, op1=ALU.add))
            me4 = me[:].rearrange("p t (g j) -> p t g j", g=4)
            lge4 = lge.rearrange("p t (g j) -> p t g j", g=4)
            dv(lambda e: e.tensor_tensor(out=me4, in0=lge4, in1=ohg[:].unsqueeze(3).to_broadcast([128, NT, 4, 8]),
                                         op=ALU.mult))
            dv(lambda e: e.tensor_tensor(out=me4, in0=me4, in1=pen[:].unsqueeze(3).to_broadcast([128, NT, 4, 8]),
                                         op=ALU.add))
            dv(lambda e: e.tensor_reduce(out=v1[:], in_=me[:], axis=AX.X, op=ALU.max))
            dv(lambda e: e.tensor_tensor(out=sel1[:], in0=me[:], in1=bc(v1[:], 32), op=ALU.is_equal))
            dv(lambda e: e.scalar_tensor_tensor(out=me2[:], in0=sel1[:], scalar=-2e30, in1=me[:],
                                                op0=ALU.mult, op1=ALU.add))
            dv(lambda e: e.tensor_reduce(out=v2[:], in_=me2[:], axis=AX.X, op=ALU.max))
            dv(lambda e: e.tensor_tensor(out=sel2[:], in0=me2[:], in1=bc(v2[:], 32), op=ALU.is_equal))
            dv(lambda e: e.tensor_tensor(out=dd[:], in0=v2[:], in1=v1[:], op=ALU.subtract))
            k.op("act", lambda e: e.activation(out=dd[:], in_=dd[:], func=AF.Exp), reads=[b_r], writes=[b_r])
            dv(lambda e: e.tensor_scalar(out=dd[:], in0=dd[:], scalar1=1.0, scalar2=None, op0=ALU.add))
            dv(lambda e: e.reciprocal(out=p1[:], in_=dd[:]))
            dv(lambda e: e.tensor_tensor(out=g1[:], in0=p1[:], in1=ggate[:], op=ALU.mult))
            dv(lambda e: e.tensor_tensor(out=g2[:], in0=ggate[:], in1=g1[:], op=ALU.subtract))
            dv(lambda e: e.tensor_tensor(out=A[:], in0=sel1[:], in1=sel2[:], op=ALU.add))

            psP = k.ps(T + "psP", [128, NT, 32], F32)
            b_psP = k.buf()
            for i in range(NT):
                k.op("pe", lambda e: e.matmul(psP[:, i, :], lhsT=cm.upper_b, rhs=A[:, i, :], start=True,
                                              stop=(i == 0)),
                     reads=[b_r, cm.b_cb], writes=[b_psP])
                for j in range(i):
                    k.op("pe", lambda e: e.matmul(psP[:, i, :], lhsT=cm.ones_b, rhs=A[:, j, :], start=False,
                                                  stop=(j == i - 1)),
                         reads=[b_r, cm.b_cb], writes=[b_psP])

            dv(lambda e: e.tensor_tensor(out=dest[:], in0=psP[:], in1=cm.ebase.unsqueeze(1).to_broadcast([128, NT, 32]),
                                         op=ALU.add), extra_r=[b_psP, cm.b_cf])
            dv(lambda e: e.tensor_scalar(out=ov[:], in0=psP[:], scalar1=float(CAP), scalar2=None, op0=ALU.is_ge),
               extra_r=[b_psP])
            dv(lambda e: e.scalar_tensor_tensor(out=dest[:], in0=ov[:], scalar=1.0e6, in1=dest[:],
                                                op0=ALU.mult, op1=ALU.add))
            for j, sel in enumerate((sel1, sel2)):
                dv(lambda e: e.tensor_tensor(out=tmp[:], in0=sel[:], in1=dest[:], op=ALU.mult))
                dv(lambda e: e.tensor_reduce(out=idxf[:, :, j], in_=tmp[:], axis=AX.X, op=ALU.add))
                dv(lambda e: e.tensor_tensor(out=tmp[:], in0=sel[:], in1=ov[:], op=ALU.mult))
                dv(lambda e: e.tensor_reduce(out=ovs[:, :, j], in_=tmp[:], axis=AX.X, op=ALU.add))
            k.op("dve", lambda e: e.tensor_copy(out=idx_all[:], in_=idxf[:]), reads=[b_r], writes=[b_idx])
            dv(lambda e: e.tensor_scalar(out=ovs[:], in0=ovs[:], scalar1=-1.0, scalar2=1.0, op0=ALU.mult, op1=ALU.add))
            k.op("dve", lambda e: e.tensor_tensor(out=gate_all[:, :, 0], in0=g1[:], in1=ovs[:, :, 0], op=ALU.mult),
                 reads=[b_r], writes=[b_gate])
            k.op("dve", lambda e: e.tensor_tensor(out=gate_all[:, :, 1], in0=g2[:], in1=ovs[:, :, 1], op=ALU.mult),
                 reads=[b_r], writes=[b_gate])

            b_xbuf = k.buf()
            xs = [xt[0][:], xt[1][:], xt[2][:], xT[0][:].rearrange("p c t -> p (c t)")]
            b_xs = [b_xt[0], b_xt[1], b_xt[2], k.buf()]
            barrier(k)
            for i in range(NT):
                s = i % 4
                k.dma("sp", lambda e: e.dma_start(out=xs[s], in_=xin[i * 128:(i + 1) * 128, :]),
                      reads=[b_xin], writes=[b_xs[s]])
                for j in range(2):
                    k.dma("pool", lambda g: g.indirect_dma_start(
                        out=xbuf, out_offset=bass.IndirectOffsetOnAxis(ap=idx_all[:, i, j:j + 1], axis=0),
                        in_=xs[s], in_offset=None, bounds_check=cm.bound_reg, oob_is_err=False),
                        reads=[b_xs[s], b_idx], writes=[b_xbuf])
            barrier(k)
        k.es = es

        b_ybuf = k.buf()
        with ExitStack() as es2:
            k.es = es2
            NB = CAP // 128
            NXS = 3
            xe = [k.sb(f"{T}xe{i}", [128, NB, D], BF16) for i in range(NXS)]
            b_xe = [k.buf() for _ in range(NXS)]
            xeT = [k.sb(f"{T}xeT{i}", [128, 8, CAP], BF16) for i in range(NXS)]
            b_xeT = [[k.buf() for _ in range(NB)] for _ in range(NXS)]
            hd = [k.sb(f"{T}hd{i}", [128, 4, CAP], BF16) for i in range(2)]
            b_hd = [[k.buf() for _ in range(4)] for _ in range(2)]
            sil = [k.sb(f"{T}sil{i}", [128, CAP], F32) for i in range(2)]
            b_sil = [k.buf() for _ in range(2)]
            yt = [k.sb(f"{T}yt{i}", [128, NB, D], BF16) for i in range(2)]
            b_yt = [[[k.buf() for _ in range(2)] for _ in range(NB)] for _ in range(2)]
            psX = [k.ps(f"{T}psX{i}", [128, D], BF16) for i in range(2)]
            b_psX = [k.buf() for _ in range(2)]
            psH1 = [k.ps(f"{T}psH1_{i}", [128, 512], F32) for i in range(2)]
            psH3 = [k.ps(f"{T}psH3_{i}", [128, 512], F32) for i in range(2)]
            b_psH1 = [k.buf() for _ in range(2)]
            b_psH3 = [k.buf() for _ in range(2)]
            psY = [k.ps(f"{T}psY{i}", [128, 512], F32) for i in range(2)]
            b_psY = [k.buf() for _ in range(2)]
            cntX = [0]

            def stage_load(e):
                s = e % NXS
                k.dma("sp", lambda q: q.dma_start(
                    out=xe[s][:], in_=xbuf[e * CAP:(e + 1) * CAP, :].rearrange("(b p) d -> p b d", p=128)),
                    reads=[b_xbuf], writes=[b_xe[s]])

            def grp_T(e, blk):
                def f():
                    s = e % NXS
                    px = cntX[0] % 2
                    cntX[0] += 1
                    for c in range(8):
                        k.op("pe", lambda q: q.transpose(out=psX[px][:, c * 128:(c + 1) * 128],
                                                         in_=xe[s][:, blk, c * 128:(c + 1) * 128],
                                                         identity=cm.ident_b),
                             reads=[b_xe[s], cm.b_cb], writes=[b_psX[px]])
                    src = psX[px][:].rearrange("p (c t) -> p c t", c=8)
                    if px == 0:
                        k.op("dve", lambda q: q.tensor_copy(out=xeT[s][:, :, blk * 128:(blk + 1) * 128], in_=src),
                             reads=[b_psX[px]], writes=[b_xeT[s][blk]])
                    else:
                        k.op("act", lambda q: q.activation(out=xeT[s][:, :, blk * 128:(blk + 1) * 128], in_=src,
                                                           func=AF.Copy),
                             reads=[b_psX[px]], writes=[b_xeT[s][blk]])
                return f

            def grp_H(e, m):
                def f():
                    s = e % NXS
                    hs = e % 2
                    ws = e % NWB
                    ph = m % 2
                    for c in range(8):
                        k.op("pe", lambda q: q.matmul(psH1[ph][:, 0:CAP], lhsT=w1t[ws][:, c, m * 128:(m + 1) * 128],
                                                      rhs=xeT[s][:, c, :], start=(c == 0), stop=(c == 7)),
                             reads=[*b_xeT[s], b_w13[ws]], writes=[b_psH1[ph]])
                    k.op("act", lambda q: q.activation(out=sil[ph][:], in_=psH1[ph][:, 0:CAP], func=AF.Silu),
                         reads=[b_psH1[ph]], writes=[b_sil[ph]])
                    for c in range(8):
                        k.op("pe", lambda q: q.matmul(psH3[ph][:, 0:CAP], lhsT=w3t[ws][:, c, m * 128:(m + 1) * 128],
                                                      rhs=xeT[s][:, c, :], start=(c == 0), stop=(c == 7)),
                             reads=[*b_xeT[s], b_w13[ws]], writes=[b_psH3[ph]])
                    k.op("dve", lambda q: q.tensor_tensor(out=hd[hs][:, m, :], in0=sil[ph][:], in1=psH3[ph][:, 0:CAP],
                                                          op=ALU.mult),
                         reads=[b_sil[ph], b_psH3[ph]], writes=[b_hd[hs][m]])
                return f

            def grp_Y(e, blk, half):
                def f():
                    hs = e % 2
                    ws = e % NWB
                    py = (blk * 2 + half) % 2
                    for c in range(4):
                        k.op("pe", lambda q: q.matmul(psY[py][:], lhsT=hd[hs][:, c, blk * 128:(blk + 1) * 128],
                                                      rhs=w2t[ws][:, c, half * 512:(half + 1) * 512],
                                                      start=(c == 0), stop=(c == 3)),
                             reads=[b_hd[hs][c], b_w2[ws]], writes=[b_psY[py]])
                    if half == 0:
                        k.op("act", lambda q: q.activation(out=yt[hs][:, blk, 0:512], in_=psY[py][:], func=AF.Copy),
                             reads=[b_psY[py]], writes=[b_yt[hs][blk][0]])
                    else:
                        k.op("dve", lambda q: q.tensor_copy(out=yt[hs][:, blk, 512:1024], in_=psY[py][:]),
                             reads=[b_psY[py]], writes=[b_yt[hs][blk][1]])
                return f

            def store_Y(e):
                hs = e % 2
                k.dma("sp", lambda q: q.dma_start(
                    out=ybuf[e * CAP:(e + 1) * CAP, :].rearrange("(b p) d -> p b d", p=128), in_=yt[hs][:]),
                    reads=[bb for blk in b_yt[hs] for bb in blk], writes=[b_ybuf])

            for e0 in range(min(NXS, NE)):
                stage_load(e0)
            for blk in range(NB):
                grp_T(0, blk)()
            for blk in range(NB):
                grp_T(1, blk)()
            for m in range(4):
                grp_H(0, m)()
            for e in range(NE):
                Tg = [grp_T(e + 2, blk) for blk in range(NB)] if e + 2 < NE else []
                Hg = [grp_H(e + 1, m) for m in range(4)] if e + 1 < NE else []
                Yg = [grp_Y(e, blk, half) for blk in range(NB) for half in range(2)]
                order = []
                ti = hi = 0
                for yi, yg in enumerate(Yg):
                    order.append(yg)
                    if yi % 2 == 0 and hi < len(Hg):
                        order.append(Hg[hi]); hi += 1
                    if yi % 2 == 1 and ti < len(Tg):
                        order.append(Tg[ti]); ti += 1
                order += Hg[hi:] + Tg[ti:]
                for g_ in order:
                    g_()
                store_Y(e)
                if e + NWB < NE:
                    load_weights(e + NWB)
                if e + NXS < NE:
                    stage_load(e + NXS)
            barrier(k)
        k.es = es

        with ExitStack() as es2:
            k.es = es2
            NY, NA = 3, 6
            y1 = [k.sb(f"{T}y1_{i}", [128, D], BF16) for i in range(NY)]
            y2 = [k.sb(f"{T}y2_{i}", [128, D], BF16) for i in range(NY)]
            b_y1 = [k.buf() for _ in range(NY)]
            b_y2 = [k.buf() for _ in range(NY)]
            xr = [k.sb(f"{T}xr_{i}", [128, D], F32) for i in range(NY)]
            b_xr = [k.buf() for _ in range(NY)]
            acc = [k.sb(f"{T}acc_{i}", [128, D], F32) for i in range(NA)]
            b_acc = [k.buf() for _ in range(NA)]
            ot = [k.sb(f"{T}ot_{i}", [128, D], F32) for i in range(3)]
            b_ot = [k.buf() for _ in range(3)]
            for i in range(NY):
                k.op("pool", lambda q: q.memset(y1[i][:], 0.0), writes=[b_y1[i]])
                k.op("pool", lambda q: q.memset(y2[i][:], 0.0), writes=[b_y2[i]])
            lnp = {}

            def cA(i):
                s, sa = i % NY, i % NA
                k.dma("sp", lambda q: q.dma_start(out=xr[s][:], in_=xin[i * 128:(i + 1) * 128, :]),
                      reads=[b_xin], writes=[b_xr[s]])
                k.dma("pool", lambda g: g.indirect_dma_start(
                    out=y1[s][:], out_offset=None, in_=ybuf,
                    in_offset=bass.IndirectOffsetOnAxis(ap=idx_all[:, i, 0:1], axis=0),
                    bounds_check=cm.bound_reg, oob_is_err=False), reads=[b_ybuf, b_idx], writes=[b_y1[s]])
                k.dma("pool", lambda g: g.indirect_dma_start(
                    out=y2[s][:], out_offset=None, in_=ybuf,
                    in_offset=bass.IndirectOffsetOnAxis(ap=idx_all[:, i, 1:2], axis=0),
                    bounds_check=cm.bound_reg, oob_is_err=False), reads=[b_ybuf, b_idx], writes=[b_y2[s]])

            def cB(i):
                s, sa = i % NY, i % NA
                k.op("act", lambda q: q.activation(out=acc[sa][:], in_=xr[s][:], func=AF.Copy, scale=ALPHA),
                     reads=[b_xr[s]], writes=[b_acc[sa]])
                k.op("dve", lambda q: q.scalar_tensor_tensor(out=acc[sa][:], in0=y1[s][:], scalar=gate_all[:, i, 0:1],
                                                             in1=acc[sa][:], op0=ALU.mult, op1=ALU.add),
                     reads=[b_y1[s], b_gate, b_acc[sa]], writes=[b_acc[sa]])
                k.op("dve", lambda q: q.scalar_tensor_tensor(out=acc[sa][:], in0=y2[s][:], scalar=gate_all[:, i, 1:2],
                                                             in1=acc[sa][:], op0=ALU.mult, op1=ALU.add),
                     reads=[b_y2[s], b_gate, b_acc[sa]], writes=[b_acc[sa]])
                lnp[i] = layer_norm_parts(k, T + "ln", acc[sa], b_acc[sa], ot[i % 3], b_ot[i % 3], gam, bet, b_gb, sa)

            def cC(i):
                lnp[i][0]()

            def cD(i):
                lnp[i][1]()

            def cE(i):
                lnp[i][2]()
                k.dma("sp", lambda q: q.dma_start(out=xout[i * 128:(i + 1) * 128, :], in_=ot[i % 3][:]),
                      reads=[b_ot[i % 3]], writes=[b_xout])

            run_pipeline(NT, [cA, cB, cC, cD, cE])
            barrier(k)
        k.es = es
    k.es = es_outer


_ln_scratch = {}


def layer_norm_parts(k, tag, acc, b_acc, ot, b_ot, gam, bet, b_gb, s):
    key = (tag, s, id(k))
    if key not in _ln_scratch:
        _ln_scratch[key] = (k.sb(f"{tag}st{s}", [128, 2, 6], F32), k.sb(f"{tag}mv{s}", [128, 2], F32),
                            k.sb(f"{tag}rs{s}", [128, 1], F32), k.buf())
    stt, mv, rs, b_s = _ln_scratch[key]
    mhalf = k.mhalf

    def part_a():
        for h in range(2):
            k.op("dve", lambda q: q.bn_stats(out=stt[:, h, :], in_=acc[:, h * 512:(h + 1) * 512]),
                 reads=[b_acc], writes=[b_s])
        k.op("dve", lambda q: q.bn_aggr(out=mv[:], in_=stt[:].rearrange("p a b -> p (a b)")), reads=[b_s], writes=[b_s])
        k.op("dve", lambda q: q.tensor_scalar(out=rs[:], in0=mv[:, 1:2], scalar1=LN_EPS, scalar2=None, op0=ALU.add),
             reads=[b_s], writes=[b_s])
        k.op("pool", lambda q: q.tensor_tensor(out=rs[:], in0=rs[:], in1=mhalf[:], op=ALU.pow),
             reads=[b_s], writes=[b_s])

    def part_b():
        k.op("dve", lambda q: q.tensor_scalar(out=acc[:], in0=acc[:], scalar1=mv[:, 0:1], scalar2=rs[:, 0:1],
                                              op0=ALU.subtract, op1=ALU.mult),
             reads=[b_s, b_acc], writes=[b_acc])
        k.op("dve", lambda q: q.tensor_tensor(out=acc[:], in0=acc[:], in1=gam[:], op=ALU.mult),
             reads=[b_acc, b_gb], writes=[b_acc])

    def part_c():
        k.op("dve", lambda q: q.tensor_tensor(out=ot[:], in0=acc[:], in1=bet[:], op=ALU.add),
             reads=[b_acc, b_gb], writes=[b_ot])

    return part_a, part_b, part_c


def layer_norm_tile(k, tag, acc, b_acc, ot, b_ot, gam, bet, b_gb, s):
    for part in layer_norm_parts(k, tag, acc, b_acc, ot, b_ot, gam, bet, b_gb, s):
        part()


class Deferred:
    def __init__(self):
        self.q = []
        self.it = 0

    def add(self, delay, fn):
        self.q.append((self.it + delay, fn))

    def tick(self, it):
        self.it = it + 1
        keep = []
        for due, fn in self.q:
            if due <= it:
                fn()
            else:
                keep.append((due, fn))
        self.q = keep

    def flush(self):
        while self.q:
            q, self.q = self.q, []
            for _, fn in q:
                fn()


def run_pipeline(n, stages, deferred=None):
    S = len(stages)
    for it in range(n + S - 1):
        if deferred is not None:
            deferred.it = it
        for st in range(S):
            i = it - st
            if 0 <= i < n:
                stages[st](i)
        if deferred is not None:
            deferred.tick(it)
    if deferred is not None:
        deferred.flush()


def barrier(k):
    deps = {}
    for n in k.engs:
        if k.cnt[n] > 0:
            s = k.sem[n]
            deps[id(s)] = (s, k.cnt[n])
    for q, ring in k.ring.items():
        for s, v in ring:
            if v > 0:
                deps[id(s)] = (s, v)
    for n in k.engs:
        k._wait(n, dict(deps))


def build_program(phases=("rglru", "moe0", "attn", "moe1")):
    nc = bass.Bass("TRN2", target_bir_lowering=False)

    need_moe = any(p.startswith("moe") for p in phases)

    def inp(name, shape):
        if name.startswith("moe_") and not need_moe:
            return None
        if name.startswith("rec_") and "rglru" not in phases:
            return None
        if name.startswith("att_") and "attn" not in phases:
            return None
        return nc.dram_tensor(name, list(shape), F32, kind="ExternalInput").ap()

    x = inp("x", [TOK, D])
    consts = inp("consts", [128, NCONST])
    moe_w_group = inp("moe_w_group", [2, D, 4])
    moe_b_group = inp("moe_b_group", [2, 4])
    moe_w_expert = inp("moe_w_expert", [2, D, NE])
    moe_b_expert = inp("moe_b_expert", [2, NE])
    moe_w1 = inp("moe_w1", [2, NE, D, DE])
    moe_w3 = inp("moe_w3", [2, NE, D, DE])
    moe_w2 = inp("moe_w2", [2, NE, DE, D])
    ln_g = inp("ln_g", [2, 2, D])
    ln_b = inp("ln_b", [2, 2, D])
    rec_w_in = inp("rec_w_in", [D, 2 * D_RNN])
    rec_cdiag = inp("rec_cdiag", [RC, NRC, 4, RC])
    rec_prm = inp("rec_prm", [RC, 4, NRC])
    rec_w_r = inp("rec_w_r", [NRC, RC, RC])
    rec_w_i = inp("rec_w_i", [NRC, RC, RC])
    rec_w_out = inp("rec_w_out", [D_RNN, D])
    att_w_qkv = inp("att_w_qkv", [D, 1536])
    att_sinks = inp("att_sinks", [NH])
    att_w_o = inp("att_w_o", [D, D])
    att_bias = inp("att_bias", [128, NH, 256])
    out = nc.dram_tensor("out", [TOK, D], F32, kind="ExternalOutput").ap()
    with ExitStack() as es:
        k = K(nc, es)
        cm = Common(k, consts)
        xbuf = k.dram("xbuf", [NSLOT, D], BF16)
        ybuf = k.dram("ybuf", [NSLOT, D], BF16)
        cur, b_cur = x, k.buf("x")
        b_out = k.buf("out")
        for pi, ph in enumerate(phases):
            last = pi == len(phases) - 1
            if last:
                nxt, b_nxt = out, b_out
            else:
                nxt, b_nxt = k.dram(f"act{pi}", [TOK, D], F32), k.buf(f"act{pi}")
            if ph == "rglru":
                rglru_phase(k, cm, "r0", cur, b_cur, nxt, b_nxt, rec_w_in, rec_cdiag, rec_prm, rec_w_r, rec_w_i,
                            rec_w_out, ln_g[0, 0], ln_b[0, 0])
            elif ph == "attn":
                attn_phase(k, cm, "a1", cur, b_cur, nxt, b_nxt, att_w_qkv, att_sinks, att_w_o, att_bias,
                           ln_g[1, 0], ln_b[1, 0])
            elif ph in ("moe0", "moe1"):
                l = int(ph[3])
                moe_phase(k, cm, f"m{l}", cur, b_cur, nxt, b_nxt, moe_w_group[l], moe_b_group[l], moe_w_expert[l],
                          moe_b_expert[l], moe_w1[l], moe_w3[l], moe_w2[l], ln_g[l, 1], ln_b[l, 1], xbuf, ybuf)
            cur, b_cur = nxt, b_nxt
        if DBG["stop"]:
            k.dma("sp", lambda e: e.dma_start(out=out[0:128, :], in_=x[0:128, :]), writes=[b_out])
        k.finish([b_out])
        barrier(k)
    return nc


def host_inputs(inputs):
    f = lambda a: np.ascontiguousarray(np.asarray(a, dtype=np.float32))
    d = {}
    d["consts"] = make_consts()
    for n in ("moe_w_group", "moe_b_group", "moe_w_expert", "moe_b_expert", "moe_w1", "moe_w3", "moe_w2",
              "ln_g", "ln_b"):
        d[n] = f(inputs[n])
    d["rec_w_in"] = f(inputs["rec_w_in"][0])
    cw = np.asarray(inputs["rec_conv_w"][0], np.float32)
    cdiag = np.zeros((RC, NRC, 4, RC), np.float32)
    pp = np.arange(RC)
    for c in range(NRC):
        for j in range(4):
            cdiag[pp, c, j, pp] = cw[j, c * RC:(c + 1) * RC]
    d["rec_cdiag"] = cdiag
    prm = np.stack([np.asarray(inputs[n][0], np.float32).reshape(NRC, RC).T
                    for n in ("rec_conv_b", "rec_b_r", "rec_b_i", "rec_lambda")], axis=1)
    d["rec_prm"] = f(prm)
    d["rec_w_r"] = f(inputs["rec_w_r"][0])
    d["rec_w_i"] = f(inputs["rec_w_i"][0])
    d["rec_w_out"] = f(inputs["rec_w_out"][0])
    d["att_w_qkv"] = f(inputs["att_w_qkv"][0])
    d["att_sinks"] = f(inputs["att_sinks"][0])
    d["att_w_o"] = f(inputs["att_w_o"][0])
    d["att_bias"] = make_att_bias()
    return d


def make_att_bias():
    slopes = 2.0 ** (-8.0 * np.arange(1, NH + 1, dtype=np.float32) / NH)
    qi = np.arange(128)[:, None]
    sj = np.arange(256)[None, :]
    dist = qi - sj + 128
    valid = (dist >= 0) & (dist < 128)
    b = np.where(valid[:, None, :], -slopes[None, :, None] * dist[:, None, :].astype(np.float32), -30000.0)
    return np.ascontiguousarray(b.astype(np.float32))


_PROG = {}


def kernel(**inputs):
    key = "full"
    if key not in _PROG:
        _PROG[key] = build_program()
    nc = _PROG[key]
    shared = host_inputs(inputs)
    x = np.ascontiguousarray(np.asarray(inputs["x"], np.float32)).reshape(NCORES, TOK, D)
    in_maps = [dict(shared, x=x[c]) for c in range(NCORES)]
    res = run_bass_kernel_spmd(nc, in_maps, core_ids=list(range(NCORES)))
    out = np.stack([np.asarray(r["out"]) for r in res.results], axis=0)
    return out.reshape(16, SEQ, D).astype(np.float32)


GELU_K = 0.7978845608028654
DBG = {"stop": 0}


class _Stop(Exception):
    pass

TCH = 1024


def rglru_phase(k, cm, T, xin, b_xin, xout, b_xout, w_in, cdiag, prm_d, w_r, w_i, w_out, ln_g, ln_b):
    nc = k.nc
    es_outer = k.es
    with ExitStack() as es:
        k.es = es
        P = RC
        gam = k.sb(T + "gam", [128, D], F32)
        bet = k.sb(T + "bet", [128, D], F32)
        b_gb = k.buf()
        k.dma("sp", lambda e: e.dma_start(out=gam[:], in_=ln_g.partition_broadcast(128)), writes=[b_gb])
        k.dma("sp", lambda e: e.dma_start(out=bet[:], in_=ln_b.partition_broadcast(128)), writes=[b_gb])
        wo_full = k.sb(T + "wo", [128, NRC, D], BF16)
        b_wo = k.buf()
        k.op("pool", lambda e: e.memset(wo_full[:], 0.0), writes=[b_wo])
        wo = wo_full[0:P]
        k.dma("pool", lambda g: g.dma_start(out=wo[:], in_=w_out.rearrange("(c p) n -> p c n", p=P)), writes=[b_wo])
        cd_full = k.sb(T + "cd", [128, NRC, 4, P], BF16)
        b_cd = k.buf()
        k.op("pool", lambda e: e.memset(cd_full[:], 0.0), writes=[b_cd])
        cd = cd_full[0:P]
        for c in range(NRC):
            k.dma("pool", lambda g: g.dma_start(out=cd[:, c, :, :], in_=cdiag[:, c, :, :]), writes=[b_cd])
        wrt_full = k.sb(T + "wrt", [128, NRC, P], BF16)
        b_wri = k.buf()
        k.op("pool", lambda e: e.memset(wrt_full[:], 0.0), writes=[b_wri])
        wrt = wrt_full[0:P]
        wit_full = k.sb(T + "wit", [128, NRC, P], BF16)
        k.op("pool", lambda e: e.memset(wit_full[:], 0.0), writes=[b_wri])
        wit = wit_full[0:P]
        k.dma("pool", lambda g: g.dma_start(out=wrt[:], in_=w_r.rearrange("n k j -> k n j")), writes=[b_wri])
        k.dma("pool", lambda g: g.dma_start(out=wit[:], in_=w_i.rearrange("n k j -> k n j")), writes=[b_wri])
        prm = k.sb(T + "prm", [P, 4, NRC], F32)
        b_prm = k.buf()
        k.dma("sp", lambda e: e.dma_start(out=prm[:], in_=prm_d), writes=[b_prm])
        der = k.sb(T + "der", [P, 5, NRC], F32)
        b_der = k.buf()
        k.op("dve", lambda e: e.tensor_scalar(out=der[:, 0:2, :], in0=prm[:, 1:3, :], scalar1=0.5, scalar2=None,
                                              op0=ALU.mult), reads=[b_prm], writes=[b_der])
        k.op("act", lambda e: e.activation(out=der[:, 4, :], in_=prm[:, 3, :], func=AF.Exp, scale=-1.0),
             reads=[b_prm, b_der], writes=[b_der])
        k.op("act", lambda e: e.activation(out=der[:, 4, :], in_=der[:, 4, :], func=AF.Ln, bias=1.0),
             reads=[b_der], writes=[b_der])
        k.op("dve", lambda e: e.tensor_scalar(out=der[:, 2, :], in0=der[:, 4, :], scalar1=-4.0, scalar2=None,
                                              op0=ALU.mult), reads=[b_der], writes=[b_der])
        k.op("dve", lambda e: e.tensor_scalar(out=der[:, 3, :], in0=der[:, 4, :], scalar1=-8.0, scalar2=None,
                                              op0=ALU.mult), reads=[b_der], writes=[b_der])

        if DBG["stop"] == 1:
            barrier(k); k.es = es_outer; return
        cvb = k.sb(T + "cvb", [P, NRC], F32)
        k.op("dve", lambda e: e.tensor_copy(out=cvb[:], in_=prm[:, 0, :]), reads=[b_prm], writes=[b_prm])
        xT = k.sb(T + "xT", [128, 8, TCH], BF16)
        b_xT = k.buf()
        YT_full = k.sb(T + "YT", [128, NRC, TCH], BF16)
        k.op("pool", lambda e: e.memset(YT_full[:], 0.0))
        YT = YT_full[0:P]
        b_YT = [k.buf() for _ in range(NRC)]
        xrc = k.sb(T + "xrc", [P, NRC, 4], F32)
        hc = k.sb(T + "hc", [P, NRC], F32)
        b_xrc = [k.buf() for _ in range(NRC)]
        b_hc = [k.buf() for _ in range(NRC)]
        xt = [k.sb(f"{T}xt{i}", [128, D], F32) for i in range(2)]
        b_xt = [k.buf() for _ in range(2)]
        xtb = [k.sb(f"{T}xtb{i}", [128, D], BF16) for i in range(2)]
        b_xtb = [k.buf() for _ in range(2)]
        wg = [[k.sb(f"{T}wg{i}_{j}", [128, 8, P], BF16) for j in range(2)] for i in range(2)]
        b_wg = [k.buf() for _ in range(2)]
        G = k.sb(T + "G", [P, TCH], F32)
        XC = k.sb(T + "XC", [P, TCH], F32)
        TR = k.sb(T + "TR", [P, TCH], F32)
        AA = k.sb(T + "AA", [P, TCH], F32)
        SQ = k.sb(T + "SQ", [P, TCH], F32)
        TI = k.sb(T + "TI", [P, TCH], F32)
        S2 = k.sb(T + "S2", [P, TCH], F32)
        HH = k.sb(T + "HH", [P, TCH], F32)
        XR_full = k.sb(T + "XR", [128, TCH + 4], BF16)
        k.op("pool", lambda e: e.memset(XR_full[:], 0.0))
        XR = XR_full[0:P]
        XCb_full = k.sb(T + "XCb", [128, TCH], BF16)
        k.op("pool", lambda e: e.memset(XCb_full[:], 0.0))
        XCb = XCb_full[0:P]
        XRf = k.sb(T + "XRf", [P, TCH + 4], F32)
        b_XRf = k.buf()
        XRb_full = k.sb(T + "XRb", [128, TCH + 4], BF16)
        k.op("pool", lambda e: e.memset(XRb_full[:], 0.0))
        XRb = XRb_full[0:P]
        b_XRb = k.buf()
        b_G, b_XC, b_TR, b_AA, b_SQ, b_TI, b_S2, b_HH, b_XR, b_XCb = [k.buf() for _ in range(10)]
        acc = [k.sb(f"{T}acc{i}", [128, D], F32) for i in range(2)]
        b_acc = [k.buf() for _ in range(2)]
        ot = [k.sb(f"{T}ot{i}", [128, D], F32) for i in range(2)]
        b_ot = [k.buf() for _ in range(2)]
        psTb = k.ps(T + "psTb", [128, D], BF16)
        b_psTb = k.buf()
        pA = [k.ps(f"{T}pA{i}", [128, 512], F32) for i in range(4)]
        b_pA = [k.buf() for _ in range(4)]
        pO = [k.ps(f"{T}pO{i}", [128, 512], F32) for i in range(2)]
        b_pO = [k.buf() for _ in range(2)]
        NQ = TCH // 512
        barrier(k)

        def qs(q):
            return slice(q * 512, (q + 1) * 512)

        def load_wchunk(c):
            s = c % 2
            k.dma("pool", lambda g: g.dma_start(out=wg[s][0][:],
                                                in_=w_in[:, c * P:(c + 1) * P].rearrange("(c p) n -> p c n", p=128)),
                  writes=[b_wg[s]])
            k.dma("pool", lambda g: g.dma_start(out=wg[s][1][:],
                                                in_=w_in[:, D_RNN + c * P:D_RNN + (c + 1) * P].rearrange(
                                                    "(c p) n -> p c n", p=128)),
                  writes=[b_wg[s]])

        for sq in range(2):
            for th in range(SEQ // TCH):
                tok0 = sq * SEQ + th * TCH
                load_wchunk(0)
                for tl in range(TCH // 128):
                    s = tl % 2
                    r0 = tok0 + tl * 128
                    k.dma("sp", lambda e: e.dma_start(out=xt[s][:], in_=xin[r0:r0 + 128, :]),
                          reads=[b_xin], writes=[b_xt[s]])
                    k.op("act", lambda e: e.activation(out=xtb[s][:], in_=xt[s][:], func=AF.Copy),
                         reads=[b_xt[s]], writes=[b_xtb[s]])
                    for c in range(8):
                        k.op("pe", lambda e: e.transpose(out=psTb[:, c * 128:(c + 1) * 128],
                                                         in_=xtb[s][:, c * 128:(c + 1) * 128], identity=cm.ident_b),
                             reads=[b_xtb[s], cm.b_cb], writes=[b_psTb])
                    k.op("dve", lambda e: e.tensor_copy(out=xT[:, :, tl * 128:(tl + 1) * 128],
                                                        in_=psTb[:].rearrange("p (c t) -> p c t", c=8)),
                         reads=[b_psTb], writes=[b_xT])
                if DBG["stop"] == 2:
                    barrier(k); k.es = es_outer; return
                for c in range(NRC):
                    if DBG["stop"] == 3 and c == 1:
                        barrier(k); k.es = es_outer; return
                    ws = c % 2
                    if c + 1 < NRC:
                        load_wchunk(c + 1)
                    for q in range(NQ):
                        for kk in range(8):
                            k.op("pe", lambda e: e.matmul(pA[q][0:P, :], lhsT=wg[ws][0][:, kk, :], rhs=xT[:, kk, qs(q)],
                                                          start=(kk == 0), stop=(kk == 7)),
                                 reads=[b_wg[ws], b_xT], writes=[b_pA[q]])
                        k.op("act", lambda e: e.activation(out=G[:, qs(q)], in_=pA[q][0:P, :], func=AF.Copy),
                             reads=[b_pA[q]], writes=[b_G])
                    for q in range(NQ):
                        for kk in range(8):
                            k.op("pe", lambda e: e.matmul(pA[2 + q][0:P, :], lhsT=wg[ws][1][:, kk, :],
                                                          rhs=xT[:, kk, qs(q)], start=(kk == 0), stop=(kk == 7)),
                                 reads=[b_wg[ws], b_xT], writes=[b_pA[2 + q]])
                        k.op("act", lambda e: e.activation(out=XRf[:, 4 + q * 512:4 + (q + 1) * 512], in_=pA[2 + q][0:P, :],
                                                           func=AF.Copy),
                             reads=[b_pA[2 + q]], writes=[b_XRf])
                    if DBG["stop"] == 10:
                        barrier(k); k.es = es_outer; return
                    if th == 0:
                        k.op("pool", lambda e: e.memset(XRf[:, 0:4], 0.0), writes=[b_XRf])
                    else:
                        k.op("pool", lambda e: e.tensor_copy(out=XRf[:, 0:4], in_=xrc[:, c, :]),
                             reads=[b_xrc[c]], writes=[b_XRf])
                    k.op("pool", lambda e: e.tensor_copy(out=xrc[:, c, :], in_=XRf[:, TCH:TCH + 4]),
                         reads=[b_XRf], writes=[b_xrc[c]])
                    k.op("pool", lambda e: e.tensor_copy(out=XR[:, 0:TCH + 4], in_=XRf[:, 0:TCH + 4]),
                         reads=[b_XRf], writes=[b_XR])
                    if DBG["stop"] == 11:
                        barrier(k); k.es = es_outer; return
                    k.op("dve", lambda e: e.tensor_copy(out=XRb[:, 0:TCH + 2], in_=XRf[:, 1:TCH + 3]),
                         reads=[b_XRf], writes=[b_XRb])
                    for q in range(NQ):
                        for j in range(4):
                            o = 1 + q * 512 + j
                            src = XR_full[:, o:o + 512] if o % 2 == 0 else XRb_full[:, o - 1:o - 1 + 512]
                            k.op("pe", lambda e: e.matmul(pA[q][0:P, :], lhsT=cd_full[:, c, j, :], rhs=src,
                                                          start=(j == 0), stop=(j == 3)),
                                 reads=[b_cd, b_XR, b_XRb], writes=[b_pA[q]])
                        if DBG.get("var") == "A":
                            k.op("dve", lambda e: e.tensor_copy(out=XC[:, qs(q)], in_=pA[q][0:P, :]),
                                 reads=[b_pA[q], b_prm], writes=[b_XC])
                            k.op("act", lambda e: e.activation(out=XCb[:, qs(q)], in_=pA[q][0:P, :], func=AF.Copy),
                                 reads=[b_pA[q], b_prm], writes=[b_XCb])
                        else:
                            k.op("dve", lambda e: e.tensor_scalar(out=XC[:, qs(q)], in0=pA[q][0:P, :],
                                                                  scalar1=cvb[:, c:c + 1], scalar2=None, op0=ALU.add),
                                 reads=[b_pA[q], b_prm], writes=[b_XC])
                            k.op("act", lambda e: e.activation(out=XCb[:, qs(q)], in_=XC[:, qs(q)], func=AF.Copy),
                                 reads=[b_XC], writes=[b_XCb])
                    if DBG["stop"] == 12:
                        barrier(k); k.es = es_outer; return
                    for q in range(NQ):
                        k.op("pe", lambda e: e.matmul(pA[2 + q][0:P, :], lhsT=wrt_full[:, c, :], rhs=XCb_full[:, qs(q)],
                                                      start=True, stop=True),
                             reads=[b_wri, b_XCb], writes=[b_pA[2 + q]])
                        k.op("act", lambda e: e.activation(out=TR[:, qs(q)], in_=pA[2 + q][0:P, :], func=AF.Tanh,
                                                           scale=0.5, bias=der[:, 0, c:c + 1]),
                             reads=[b_pA[2 + q], b_der], writes=[b_TR])
                    for q in range(NQ):
                        k.op("pe", lambda e: e.matmul(pA[q][0:P, :], lhsT=wit_full[:, c, :], rhs=XCb_full[:, qs(q)],
                                                      start=True, stop=True),
                             reads=[b_wri, b_XCb], writes=[b_pA[q]])
                        k.op("act", lambda e: e.activation(out=TI[:, qs(q)], in_=pA[q][0:P, :], func=AF.Tanh,
                                                           scale=0.5, bias=der[:, 1, c:c + 1]),
                             reads=[b_pA[q], b_der], writes=[b_TI])
                    if DBG["stop"] == 13:
                        barrier(k); k.es = es_outer; return
                    k.op("dve", lambda e: e.tensor_tensor(out=S2[:], in0=G[:], in1=G[:], op=ALU.mult),
                         reads=[b_G], writes=[b_S2])
                    k.op("dve", lambda e: e.tensor_scalar(out=S2[:], in0=S2[:], scalar1=0.044715, scalar2=1.0,
                                                          op0=ALU.mult, op1=ALU.add), reads=[b_S2], writes=[b_S2])
                    k.op("pool", lambda e: e.tensor_tensor(out=S2[:], in0=S2[:], in1=G[:], op=ALU.mult),
                         reads=[b_S2, b_G], writes=[b_S2])
                    k.op("act", lambda e: e.activation(out=S2[:], in_=S2[:], func=AF.Tanh, scale=GELU_K),
                         reads=[b_S2], writes=[b_S2])
                    k.op("dve", lambda e: e.scalar_tensor_tensor(out=S2[:], in0=S2[:], scalar=1.0, in1=G[:],
                                                                 op0=ALU.add, op1=ALU.mult),
                         reads=[b_S2, b_G], writes=[b_S2])
                    if DBG["stop"] == 14:
                        barrier(k); k.es = es_outer; return
                    k.op("act", lambda e: e.activation(out=AA[:], in_=TR[:], func=AF.Exp, scale=der[:, 2, c:c + 1],
                                                       bias=der[:, 2, c:c + 1]),
                         reads=[b_TR, b_der], writes=[b_AA])
                    k.op("act", lambda e: e.activation(out=SQ[:], in_=TR[:], func=AF.Exp, scale=der[:, 3, c:c + 1],
                                                       bias=der[:, 3, c:c + 1]),
                         reads=[b_TR, b_der], writes=[b_SQ])
                    k.op("act", lambda e: e.activation(out=SQ[:], in_=SQ[:], func=AF.Sqrt, scale=-1.0, bias=1.0),
                         reads=[b_SQ], writes=[b_SQ])
                    if DBG["stop"] == 15:
                        barrier(k); k.es = es_outer; return
                    k.op("dve", lambda e: e.scalar_tensor_tensor(out=TI[:], in0=TI[:], scalar=1.0, in1=XC[:],
                                                                 op0=ALU.add, op1=ALU.mult),
                         reads=[b_TI, b_XC], writes=[b_TI])
                    k.op("pool", lambda e: e.tensor_tensor(out=TI[:], in0=TI[:], in1=SQ[:], op=ALU.mult),
                         reads=[b_TI, b_SQ], writes=[b_TI])
                    if DBG["stop"] == 16:
                        barrier(k); k.es = es_outer; return
                    init = 0.0 if th == 0 else hc[:, c:c + 1]
                    k.op("dve", lambda e: e.tensor_tensor_scan(out=HH[:], data0=AA[:], data1=TI[:], initial=init,
                                                               op0=ALU.mult, op1=ALU.add),
                         reads=[b_AA, b_TI, b_hc[c]], writes=[b_HH])
                    k.op("pool", lambda e: e.tensor_copy(out=hc[:, c:c + 1], in_=HH[:, TCH - 1:TCH]),
                         reads=[b_HH], writes=[b_hc[c]])
                    if DBG["stop"] == 17:
                        barrier(k); k.es = es_outer; return
                    k.op("dve", lambda e: e.scalar_tensor_tensor(out=YT[:, c, :], in0=S2[:], scalar=0.25, in1=HH[:],
                                                                 op0=ALU.mult, op1=ALU.mult),
                         reads=[b_S2, b_HH], writes=[b_YT[c]])
                if DBG["stop"] == 4:
                    barrier(k); k.es = es_outer; return
                for tl in range(TCH // 128):
                    s = tl % 2
                    r0 = tok0 + tl * 128
                    k.dma("sp", lambda e: e.dma_start(out=xt[s][:], in_=xin[r0:r0 + 128, :]),
                          reads=[b_xin], writes=[b_xt[s]])
                    k.op("act", lambda e: e.activation(out=acc[s][:], in_=xt[s][:], func=AF.Copy, scale=ALPHA),
                         reads=[b_xt[s]], writes=[b_acc[s]])
                    for h in range(2):
                        for c in range(NRC):
                            k.op("pe", lambda e: e.matmul(pO[h][:], lhsT=YT_full[:, c, tl * 128:(tl + 1) * 128],
                                                          rhs=wo_full[:, c, h * 512:(h + 1) * 512],
                                                          start=(c == 0), stop=(c == NRC - 1)),
                                 reads=[b_YT[c], b_wo], writes=[b_pO[h]])
                        k.op("dve", lambda e: e.tensor_tensor(out=acc[s][:, h * 512:(h + 1) * 512],
                                                              in0=acc[s][:, h * 512:(h + 1) * 512], in1=pO[h][:],
                                                              op=ALU.add),
                             reads=[b_pO[h], b_acc[s]], writes=[b_acc[s]])
                    layer_norm_tile(k, T + "ln", acc[s], b_acc[s], ot[s], b_ot[s], gam, bet, b_gb, s)
                    k.dma("sp", lambda e: e.dma_start(out=xout[r0:r0 + 128, :], in_=ot[s][:]),
                          reads=[b_ot[s]], writes=[b_xout])
        barrier(k)
    k.es = es_outer


def attn_phase(k, cm, T, xin, b_xin, xout, b_xout, w_qkv, sinks, w_o, att_bias, ln_g, ln_b):
    es_outer = k.es
    HALF = 1024
    with ExitStack() as es:
        k.es = es
        gam = k.sb(T + "gam", [128, D], F32)
        bet = k.sb(T + "bet", [128, D], F32)
        b_gb = k.buf()
        k.dma("sp", lambda e: e.dma_start(out=gam[:], in_=ln_g.partition_broadcast(128)), writes=[b_gb])
        k.dma("sp", lambda e: e.dma_start(out=bet[:], in_=ln_b.partition_broadcast(128)), writes=[b_gb])
        wq = k.sb(T + "wq", [128, 8, 1024], BF16)
        wk = k.sb(T + "wk", [128, 8, 256], BF16)
        wv = k.sb(T + "wv", [128, 8, 256], BF16)
        wo = k.sb(T + "wo", [128, 8, 1024], BF16)
        b_w = k.buf()
        k.dma("pool", lambda g: g.dma_start(out=wq[:], in_=w_qkv[:, 0:1024].rearrange("(c p) n -> p c n", p=128)),
              writes=[b_w])
        k.dma("pool", lambda g: g.dma_start(out=wk[:], in_=w_qkv[:, 1024:1280].rearrange("(c p) n -> p c n", p=128)),
              writes=[b_w])
        k.dma("pool", lambda g: g.dma_start(out=wv[:], in_=w_qkv[:, 1280:1536].rearrange("(c p) n -> p c n", p=128)),
              writes=[b_w])
        k.dma("pool", lambda g: g.dma_start(out=wo[:], in_=w_o.rearrange("(c p) n -> p c n", p=128)), writes=[b_w])
        sk = k.sb(T + "sk", [128, NH], F32)
        bt = k.sb(T + "bt", [128, NH, 256], F32)
        b_c = k.buf()
        k.dma("sp", lambda e: e.dma_start(out=sk[:], in_=sinks.partition_broadcast(128)), writes=[b_c])
        k.dma("sp", lambda e: e.dma_start(out=bt[:], in_=att_bias), writes=[b_c])
        nsk = k.sb(T + "nsk", [128, NH], F32)
        k.op("dve", lambda e: e.tensor_scalar(out=nsk[:], in0=sk[:], scalar1=-1.0, scalar2=None, op0=ALU.mult),
             reads=[b_c], writes=[b_c])

        xT = k.sb(T + "xT", [128, 8, HALF], BF16)
        b_xT = k.buf()
        QT = k.sb(T + "QT", [HD, NH, HALF], BF16)
        b_QT = k.buf()
        KT = k.sb(T + "KT", [HD, NKV, SEQ], BF16)
        b_KT = k.buf()
        V = k.sb(T + "V", [128, SEQ // 128, NKV * HD], BF16)
        b_V = k.buf()
        xt = [k.sb(f"{T}xt{i}", [128, D], F32) for i in range(2)]
        b_xt = [k.buf() for _ in range(2)]
        xtb = [k.sb(f"{T}xtb{i}", [128, D], BF16) for i in range(2)]
        b_xtb = [k.buf() for _ in range(2)]
        Sb = [k.sb(f"{T}Sb{i}", [128, 2, 256], F32) for i in range(3)]
        Pm = [k.sb(f"{T}Pm{i}", [128, 2, 256], BF16) for i in range(3)]
        PT = [k.sb(f"{T}PT{i}", [128, 2, 256], BF16) for i in range(3)]
        sm = [k.sb(f"{T}sm{i}", [128, 12], F32) for i in range(4)]
        b_Sb = [k.buf() for _ in range(3)]
        b_Pm = [k.buf() for _ in range(3)]
        b_PT = [k.buf() for _ in range(3)]
        b_sm = [[k.buf() for _ in range(6)] for _ in range(4)]
        Ot = [k.sb(f"{T}Ot{i}", [128, D], BF16) for i in range(2)]
        b_Ot = [k.buf() for _ in range(2)]
        OT = [k.sb(f"{T}OT{i}", [128, 8, 128], BF16) for i in range(2)]
        b_OT = [k.buf() for _ in range(2)]
        acc = [k.sb(f"{T}acc{i}", [128, D], F32) for i in range(2)]
        b_acc = [k.buf() for _ in range(2)]
        ot = [k.sb(f"{T}ot{i}", [128, D], F32) for i in range(2)]
        b_ot = [k.buf() for _ in range(2)]
        psTb = k.ps(T + "psTb", [128, D], BF16)
        b_psTb = k.buf()
        psS_bk = [k.ps(f"{T}psS{i}", [128, 512], F32) for i in range(2)]
        _bS = [k.buf() for _ in range(2)]
        psS = [psS_bk[i % 2][:, 0:256] for i in range(4)]
        b_psS = [_bS[i % 2] for i in range(4)]
        psPT_bk = [k.ps(f"{T}psPT{i}", [128, 1024], BF16) for i in range(2)]
        _bP = [k.buf() for _ in range(2)]
        psPT = [psPT_bk[i % 2][:, 0:256] for i in range(4)]
        b_psPT = [_bP[i % 2] for i in range(4)]
        psO_bk = [k.ps(f"{T}psO{i}", [128, 512], F32) for i in range(2)]
        _bO = [k.buf() for _ in range(2)]
        psO = [psO_bk[i % 2][:, 0:HD] for i in range(8)]
        b_psO = [_bO[i % 2] for i in range(8)]
        pE = k.ps(T + "pE", [128, 512], F32)
        b_pE = k.buf()
        pA = psS_bk
        b_pA = _bS
        npa = [0]

        def next_pa():
            npa[0] += 1
            return npa[0] % 2

        def qs(q):
            return slice(q * 512, (q + 1) * 512)

        hcnt = 0
        for sq in range(2):
            for hf in range(SEQ // HALF):
                tok0 = sq * SEQ + hf * HALF
                for tl in range(HALF // 128):
                    s = tl % 2
                    r0 = tok0 + tl * 128
                    k.dma("sp", lambda e: e.dma_start(out=xt[s][:], in_=xin[r0:r0 + 128, :]),
                          reads=[b_xin], writes=[b_xt[s]])
                    k.op("act", lambda e: e.activation(out=xtb[s][:], in_=xt[s][:], func=AF.Copy),
                         reads=[b_xt[s]], writes=[b_xtb[s]])
                    for c in range(8):
                        k.op("pe", lambda e: e.transpose(out=psTb[:, c * 128:(c + 1) * 128],
                                                         in_=xtb[s][:, c * 128:(c + 1) * 128], identity=cm.ident_b),
                             reads=[b_xtb[s], cm.b_cb], writes=[b_psTb])
                    k.op("dve", lambda e: e.tensor_copy(out=xT[:, :, tl * 128:(tl + 1) * 128],
                                                        in_=psTb[:].rearrange("p (c t) -> p c t", c=8)),
                         reads=[b_psTb], writes=[b_xT])
                for h in range(NH):
                    for q in range(HALF // 512):
                        pa = next_pa()
                        for kk in range(8):
                            k.op("pe", lambda e: e.matmul(pA[pa][0:HD, :], lhsT=wq[:, kk, h * HD:(h + 1) * HD],
                                                          rhs=xT[:, kk, qs(q)], start=(kk == 0), stop=(kk == 7)),
                                 reads=[b_w, b_xT], writes=[b_pA[pa]])
                        if (h + q) % 2 == 0:
                            k.op("act", lambda e: e.activation(out=QT[:, h, qs(q)], in_=pA[pa][0:HD, :], func=AF.Copy,
                                                               scale=HD ** -0.5),
                                 reads=[b_pA[pa]], writes=[b_QT])
                        else:
                            k.op("dve", lambda e: e.tensor_scalar(out=QT[:, h, qs(q)], in0=pA[pa][0:HD, :],
                                                                  scalar1=HD ** -0.5, scalar2=None, op0=ALU.mult),
                                 reads=[b_pA[pa]], writes=[b_QT])
                for kv in range(NKV):
                    for q in range(HALF // 512):
                        pa = next_pa()
                        for kk in range(8):
                            k.op("pe", lambda e: e.matmul(pA[pa][0:HD, :], lhsT=wk[:, kk, kv * HD:(kv + 1) * HD],
                                                          rhs=xT[:, kk, qs(q)], start=(kk == 0), stop=(kk == 7)),
                                 reads=[b_w, b_xT], writes=[b_pA[pa]])
                        c0 = hf * HALF + q * 512
                        k.op("act", lambda e: e.activation(out=KT[:, kv, c0:c0 + 512], in_=pA[pa][0:HD, :], func=AF.Copy),
                             reads=[b_pA[pa]], writes=[b_KT])
                for tl in range(HALF // 128):
                    pa = next_pa()
                    for kk in range(8):
                        k.op("pe", lambda e: e.matmul(pA[pa][:, 0:256], lhsT=xT[:, kk, tl * 128:(tl + 1) * 128],
                                                      rhs=wv[:, kk, :], start=(kk == 0), stop=(kk == 7)),
                             reads=[b_w, b_xT], writes=[b_pA[pa]])
                    k.op("dve", lambda e: e.tensor_copy(out=V[:, hf * 8 + tl, :], in_=pA[pa][:, 0:256]),
                         reads=[b_pA[pa]], writes=[b_V])
                NB_ = HALF // 128
                items = [(b, hp) for b in range(NB_) for hp in range(NH // 2)]

                def geom(b):
                    g = hf * NB_ + b
                    has_prev = g > 0
                    cs = slice(0, 256) if has_prev else slice(128, 256)
                    k0 = (g - 1) * 128 if has_prev else 0
                    return g, has_prev, cs, k0, (g + 1) * 128

                def st1(n):
                    b, hp = items[n]
                    g, has_prev, cs, k0, k1 = geom(b)
                    h0 = 2 * hp
                    kv = h0 // 4
                    os_ = b % 2
                    r0 = tok0 + b * 128
                    if hp == 0:
                        k.dma("sp", lambda e: e.dma_start(out=xt[os_][:], in_=xin[r0:r0 + 128, :]),
                              reads=[b_xin], writes=[b_xt[os_]])
                        k.op("act", lambda e: e.activation(out=acc[os_][:], in_=xt[os_][:], func=AF.Copy, scale=ALPHA),
                             reads=[b_xt[os_]], writes=[b_acc[os_]])
                    s2, s3, s4 = n % 2, n % 3, n % 4
                    pS = psS_bk[s2][:].rearrange("p (j c) -> p j c", j=2)
                    for j in range(2):
                        k.op("pe", lambda e: e.matmul(pS[:, j, cs], lhsT=QT[:, h0 + j, b * 128:(b + 1) * 128],
                                                      rhs=KT[:, kv, k0:k1], start=True, stop=True),
                             reads=[b_QT, b_KT], writes=[_bS[s2]])
                    k.op("dve", lambda e: e.tensor_tensor(out=Sb[s3][:, :, cs], in0=pS[:, :, cs], in1=bt[:, h0:h0 + 2, cs],
                                                          op=ALU.add),
                         reads=[_bS[s2], b_c], writes=[b_Sb[s3]])
                    k.op("dve", lambda e: e.tensor_reduce(out=sm[s4][:, 0:2], in_=Sb[s3][:, :, cs], axis=AX.X, op=ALU.max),
                         reads=[b_Sb[s3]], writes=[b_sm[s4][0]])
                    k.op("dve", lambda e: e.scalar_tensor_tensor(out=sm[s4][:, 2:4], in0=sm[s4][:, 0:2], scalar=-1.0,
                                                                 in1=nsk[:, h0:h0 + 2], op0=ALU.mult, op1=ALU.min),
                         reads=[b_sm[s4][0], b_c], writes=[b_sm[s4][1]])
                    for j in range(2):
                        k.op("act", lambda e: e.activation(out=Pm[s3][:, j, cs], in_=Sb[s3][:, j, cs], func=AF.Exp,
                                                           bias=sm[s4][:, 2 + j:3 + j], accum_out=sm[s4][:, 4 + j:5 + j]),
                             reads=[b_Sb[s3], b_sm[s4][1]], writes=[b_Pm[s3], b_sm[s4][2]])
                    k.op("dve", lambda e: e.tensor_tensor(out=sm[s4][:, 6:8], in0=sk[:, h0:h0 + 2], in1=sm[s4][:, 2:4],
                                                          op=ALU.add),
                         reads=[b_sm[s4][1], b_c], writes=[b_sm[s4][3]])
                    k.op("act", lambda e: e.activation(out=sm[s4][:, 6:8], in_=sm[s4][:, 6:8], func=AF.Exp),
                         reads=[b_sm[s4][3]], writes=[b_sm[s4][3]])

                def st2(n):
                    b, hp = items[n]
                    g, has_prev, cs, k0, k1 = geom(b)
                    s2, s3, s4 = n % 2, n % 3, n % 4
                    k.op("dve", lambda e: e.tensor_tensor(out=sm[s4][:, 8:10], in0=sm[s4][:, 4:6], in1=sm[s4][:, 6:8],
                                                          op=ALU.add),
                         reads=[b_sm[s4][2], b_sm[s4][3]], writes=[b_sm[s4][4]])
                    k.op("dve", lambda e: e.reciprocal(out=sm[s4][:, 10:12], in_=sm[s4][:, 8:10]),
                         reads=[b_sm[s4][4]], writes=[b_sm[s4][5]])
                    pP = psPT_bk[s2][:, 0:512].rearrange("p (j c) -> p j c", j=2)
                    for j in range(2):
                        if has_prev:
                            k.op("pe", lambda e: e.transpose(out=pP[:, j, 0:128], in_=Pm[s3][:, j, 0:128],
                                                             identity=cm.ident_b),
                                 reads=[b_Pm[s3], cm.b_cb], writes=[_bP[s2]])
                        k.op("pe", lambda e: e.transpose(out=pP[:, j, 128:256], in_=Pm[s3][:, j, 128:256],
                                                         identity=cm.ident_b),
                             reads=[b_Pm[s3], cm.b_cb], writes=[_bP[s2]])
                    if n % 2 == 0:
                        k.op("act", lambda e: e.activation(out=PT[s3][:, :, cs], in_=pP[:, :, cs], func=AF.Copy),
                             reads=[_bP[s2]], writes=[b_PT[s3]])
                    else:
                        k.op("dve", lambda e: e.tensor_copy(out=PT[s3][:, :, cs], in_=pP[:, :, cs]),
                             reads=[_bP[s2]], writes=[b_PT[s3]])

                def st3(n):
                    b, hp = items[n]
                    g, has_prev, cs, k0, k1 = geom(b)
                    h0 = 2 * hp
                    kv = h0 // 4
                    os_ = b % 2
                    s2, s3, s4 = n % 2, n % 3, n % 4
                    pO_ = psO_bk[s2][:, 0:2 * HD].rearrange("p (j d) -> p j d", j=2)
                    for j in range(2):
                        if has_prev:
                            k.op("pe", lambda e: e.matmul(pO_[:, j, :], lhsT=PT[s3][:, j, 0:128],
                                                          rhs=V[:, g - 1, kv * HD:(kv + 1) * HD], start=True, stop=False),
                                 reads=[b_PT[s3], b_V], writes=[_bO[s2]])
                        k.op("pe", lambda e: e.matmul(pO_[:, j, :], lhsT=PT[s3][:, j, 128:256],
                                                      rhs=V[:, g, kv * HD:(kv + 1) * HD], start=(not has_prev), stop=True),
                             reads=[b_PT[s3], b_V], writes=[_bO[s2]])
                    k.op("dve", lambda e: e.tensor_tensor(
                        out=Ot[os_][:, h0 * HD:(h0 + 2) * HD].rearrange("p (j d) -> p j d", j=2), in0=pO_,
                        in1=sm[s4][:, 10:12].unsqueeze(2).to_broadcast([128, 2, HD]), op=ALU.mult),
                        reads=[_bO[s2], b_sm[s4][5]], writes=[b_Ot[os_]])
                    if hp == NH // 2 - 1:
                        epilogue(b)

                dfr = Deferred()

                def epilogue(b):
                    os_ = b % 2
                    r0 = tok0 + b * 128

                    def e1():
                        for c in range(8):
                            k.op("pe", lambda e: e.transpose(out=psTb[:, c * 128:(c + 1) * 128],
                                                             in_=Ot[os_][:, c * 128:(c + 1) * 128], identity=cm.ident_b),
                                 reads=[b_Ot[os_], cm.b_cb], writes=[b_psTb])
                        k.op("act", lambda e: e.activation(out=OT[os_][:], in_=psTb[:].rearrange("p (c t) -> p c t", c=8),
                                                           func=AF.Copy),
                             reads=[b_psTb], writes=[b_OT[os_]])

                    def e2(hh):
                        def f():
                            for c in range(8):
                                k.op("pe", lambda e: e.matmul(pE[:], lhsT=OT[os_][:, c, :],
                                                              rhs=wo[:, c, hh * 512:(hh + 1) * 512],
                                                              start=(c == 0), stop=(c == 7)),
                                     reads=[b_OT[os_], b_w], writes=[b_pE])
                        return f

                    def e3(hh):
                        def f():
                            k.op("dve", lambda e: e.tensor_tensor(out=acc[os_][:, hh * 512:(hh + 1) * 512],
                                                                  in0=acc[os_][:, hh * 512:(hh + 1) * 512], in1=pE[:],
                                                                  op=ALU.add),
                                 reads=[b_pE, b_acc[os_]], writes=[b_acc[os_]])
                        return f

                    pa_, pb_, pc_ = layer_norm_parts(k, T + "ln", acc[os_], b_acc[os_], ot[os_], b_ot[os_], gam, bet,
                                                     b_gb, os_)

                    def store():
                        k.dma("sp", lambda e: e.dma_start(out=xout[r0:r0 + 128, :], in_=ot[os_][:]),
                              reads=[b_ot[os_]], writes=[b_xout])

                    e1()
                    dfr.add(1, e2(0))
                    dfr.add(2, e3(0))
                    dfr.add(2, e2(1))
                    dfr.add(3, e3(1))
                    dfr.add(3, pa_)
                    dfr.add(4, pb_)
                    dfr.add(5, pc_)
                    dfr.add(6, store)

                run_pipeline(len(items), [st1, st2, st3], dfr)
        barrier(k)
    k.es = es_outer
```

```python
from contextlib import ExitStack

import numpy as np
import concourse.bass as bass
import concourse.mybir as mybir
from concourse.bass_utils import run_bass_kernel_spmd

F32 = mybir.dt.float32
BF16 = mybir.dt.bfloat16
I32 = mybir.dt.int32
AF = mybir.ActivationFunctionType
ALU = mybir.AluOpType
AX = mybir.AxisListType

NCORES = 8
D = 1024
SEQ = 2048
TOK = 4096
NT = TOK // 128
NE = 32
DE = 512
CAP = 384
NSLOT = NE * CAP
ALPHA = float((2 * 2) ** 0.25)
LN_EPS = 1e-5
D_RNN = 1280
RC = 80
NRC = D_RNN // RC
NH = 16
NKV = 4
HD = 64


class Buf:
    __slots__ = ("name", "writers", "readers")

    def __init__(self, name):
        self.name = name
        self.writers = {}
        self.readers = {}


def _merge(dst, src):
    for k, (s, v) in src.items():
        if k not in dst or dst[k][1] < v:
            dst[k] = (s, v)


class K:
    def __init__(self, nc, es):
        self.nc = nc
        self.es = es
        self.es_root = es
        self.engs = {"pe": nc.tensor, "dve": nc.vector, "act": nc.scalar, "pool": nc.gpsimd, "sp": nc.sync}
        self.sem = {}
        self.cnt = {}
        self.waited = {n: {} for n in self.engs}
        for n in self.engs:
            self.sem[n] = es.enter_context(nc.semaphore("s_" + n))
            self.cnt[n] = 0
        self.ring = {}
        for q, n in (("sp", 10), ("pool", 10), ("act", 4)):
            self.ring[q] = [[es.enter_context(nc.semaphore(f"d_{q}{i}")), 0] for i in range(n)]
        self.ring_pos = {q: 0 for q in self.ring}
        self.nbuf = 0

    def sb(self, name, shape, dt):
        return self.es.enter_context(self.nc.sbuf_tensor(name, list(shape), dt))

    def ps(self, name, shape, dt):
        return self.es.enter_context(self.nc.psum_tensor(name, list(shape), dt))

    def dram(self, name, shape, dt):
        return self.nc.dram_tensor(name, list(shape), dt, kind="Internal").ap()

    def buf(self, name=None):
        self.nbuf += 1
        return Buf(name or f"b{self.nbuf}")

    def _wait(self, engname, deps):
        e = self.engs[engname]
        w = self.waited[engname]
        for k, (s, v) in deps.items():
            if engname == "pe" and k == id(self.sem["pe"]):
                continue
            if w.get(k, 0) >= v:
                continue
            e.wait_ge(s, v)
            w[k] = v

    def _deps(self, reads, writes):
        deps = {}
        for b in reads:
            _merge(deps, b.writers)
        for b in writes:
            _merge(deps, b.writers)
            _merge(deps, b.readers)
        return deps

    def _record(self, tok, reads, writes):
        k, s, v = tok
        for b in reads:
            if k not in b.readers or b.readers[k][1] < v:
                b.readers[k] = (s, v)
        for b in writes:
            if b.readers:
                b.writers = {}
                b.readers = {}
            b.writers[k] = (s, v)

    def op(self, engname, fn, reads=(), writes=()):
        deps = self._deps(reads, writes)
        self._wait(engname, deps)
        inst = fn(self.engs[engname])
        s = self.sem[engname]
        self.cnt[engname] += 1
        inst.then_inc(s, 1)
        tok = (id(s), s, self.cnt[engname])
        self._record(tok, reads, writes)
        return tok

    def dma(self, q, fn, reads=(), writes=()):
        deps = self._deps(reads, writes)
        ring = self.ring[q]
        pos = self.ring_pos[q]
        self.ring_pos[q] = (pos + 1) % len(ring)
        ent = ring[pos]
        s = ent[0]
        if ent[1] > 0:
            deps[id(s)] = (s, ent[1])
        self._wait(q, deps)
        inst = fn(self.engs[q])
        ent[1] += 16
        inst.then_inc(s, 16)
        tok = (id(s), s, ent[1])
        self._record(tok, reads, writes)
        return tok

    def finish(self, bufs):
        deps = {}
        for b in bufs:
            _merge(deps, b.writers)
        self._wait("sp", deps)


C_IDENT = 0
C_UPPER = 128
C_ONES = 256
C_EBASE = 384
NCONST = 416


def make_consts():
    c = np.zeros((128, NCONST), np.float32)
    c[:, C_IDENT:C_IDENT + 128] = np.eye(128, dtype=np.float32)
    c[:, C_UPPER:C_UPPER + 128] = np.triu(np.ones((128, 128), np.float32), 1)
    c[:, C_ONES:C_ONES + 128] = 1.0
    c[:, C_EBASE:C_EBASE + NE] = (np.arange(NE, dtype=np.float32) * CAP)[None, :]
    return c


class Common:
    def __init__(self, k, consts_ap):
        self.k = k
        nc = k.nc
        self.cf = k.sb("cf", [128, NCONST], F32)
        self.b_cf = k.buf("cf")
        k.dma("sp", lambda e: e.dma_start(out=self.cf[:], in_=consts_ap), writes=[self.b_cf])
        self.cb = k.sb("cb", [128, 384], BF16)
        self.b_cb = k.buf("cb")
        k.op("dve", lambda e: e.tensor_copy(out=self.cb[:], in_=self.cf[:, 0:384]),
             reads=[self.b_cf], writes=[self.b_cb])
        self.ident_f = self.cf[:, C_IDENT:C_IDENT + 128]
        self.ident_b = self.cb[:, C_IDENT:C_IDENT + 128]
        self.upper_b = self.cb[:, C_UPPER:C_UPPER + 128]
        self.ones_b = self.cb[:, C_ONES:C_ONES + 128]
        self.ebase = self.cf[:, C_EBASE:C_EBASE + NE]
        self.bound_reg = nc.gpsimd.to_reg(NSLOT - 1)
        k.mhalf = k.sb("ln_mhalf", [128, 1], F32)
        k.op("pool", lambda q: q.memset(k.mhalf[:], -0.5))
        barrier(k)


def moe_phase(k, cm, tag, xin, b_xin, xout, b_xout, w_rg, b_rg, w_re, b_re, w1, w3, w2, ln_g, ln_b,
              xbuf, ybuf):
    nc = k.nc
    es_outer = k.es
    with ExitStack() as es:
        k.es = es
        T = tag
        wr = k.sb(T + "wr", [128, 8, 36], F32)
        b_wr = k.buf()
        k.dma("sp", lambda e: e.dma_start(out=wr[:, :, 0:4], in_=w_rg.rearrange("(c p) n -> p c n", p=128)),
              writes=[b_wr])
        k.dma("sp", lambda e: e.dma_start(out=wr[:, :, 4:36], in_=w_re.rearrange("(c p) n -> p c n", p=128)),
              writes=[b_wr])
        rbias = k.sb(T + "rbias", [128, 36], F32)
        b_rbias = k.buf()
        k.dma("sp", lambda e: e.dma_start(out=rbias[:, 0:4], in_=b_rg.partition_broadcast(128)), writes=[b_rbias])
        k.dma("sp", lambda e: e.dma_start(out=rbias[:, 4:36], in_=b_re.partition_broadcast(128)), writes=[b_rbias])
        gam = k.sb(T + "gam", [128, D], F32)
        bet = k.sb(T + "bet", [128, D], F32)
        b_gb = k.buf()
        k.dma("sp", lambda e: e.dma_start(out=gam[:], in_=ln_g.partition_broadcast(128)), writes=[b_gb])
        k.dma("sp", lambda e: e.dma_start(out=bet[:], in_=ln_b.partition_broadcast(128)), writes=[b_gb])
        idx_all = k.sb(T + "idx", [128, NT, 2], I32)
        gate_all = k.sb(T + "gate", [128, NT, 2], F32)
        b_idx = k.buf()
        b_gate = k.buf()

        NWB = 3
        w1t = [k.sb(f"{T}w1_{i}", [128, 8, DE], BF16) for i in range(NWB)]
        w3t = [k.sb(f"{T}w3_{i}", [128, 8, DE], BF16) for i in range(NWB)]
        w2t = [k.sb(f"{T}w2_{i}", [128, 4, D], BF16) for i in range(NWB)]
        b_w13 = [k.buf() for _ in range(NWB)]
        b_w2 = [k.buf() for _ in range(NWB)]

        def load_weights(e):
            s = e % NWB
            k.dma("pool", lambda g: g.dma_start(out=w1t[s][:], in_=w1[e].rearrange("(c p) n -> p c n", p=128)),
                  writes=[b_w13[s]])
            k.dma("pool", lambda g: g.dma_start(out=w3t[s][:], in_=w3[e].rearrange("(c p) n -> p c n", p=128)),
                  writes=[b_w13[s]])
            k.dma("pool", lambda g: g.dma_start(out=w2t[s][:], in_=w2[e].rearrange("(c p) n -> p c n", p=128)),
                  writes=[b_w2[s]])

        for e in range(NWB):
            load_weights(e)

        with ExitStack() as es2:
            k.es = es2
            lg_all = k.sb(T + "lg_all", [128, NT, 36], F32)
            b_lg = [k.buf() for _ in range(NT)]
            xt = [k.sb(f"{T}xt{i}", [128, D], F32) for i in range(3)]
            b_xt = [k.buf() for _ in range(3)]
            xT = [k.sb(f"{T}xT{i}", [128, 8, 128], F32) for i in range(3)]
            b_xTa = [k.buf() for _ in range(3)]
            b_xTb = [k.buf() for _ in range(3)]
            psT = [k.ps(f"{T}psT{i}", [128, 8, 128], F32) for i in range(2)]
            b_psT = [k.buf() for _ in range(2)]
            psL = [k.ps(f"{T}psL{i}", [128, 512], F32) for i in range(2)]
            b_psL = [k.buf() for _ in range(2)]

            def lg0(i):
                s = i % 3
                k.dma("sp", lambda e: e.dma_start(out=xt[s][:], in_=xin[i * 128:(i + 1) * 128, :]),
                      reads=[b_xin], writes=[b_xt[s]])

            def lg1(i):
                s, p = i % 3, i % 2
                for c in range(8):
                    k.op("pe", lambda e: e.transpose(out=psT[p][:, c, :], in_=xt[s][:, c * 128:(c + 1) * 128],
                                                     identity=cm.ident_f),
                         reads=[b_xt[s], cm.b_cf], writes=[b_psT[p]])

            def lg2(i):
                s, p = i % 3, i % 2
                k.op("act", lambda e: e.activation(out=xT[s][:, 0:4, :], in_=psT[p][:, 0:4, :], func=AF.Copy),
                     reads=[b_psT[p]], writes=[b_xTa[s]])
                k.op("dve", lambda e: e.tensor_copy(out=xT[s][:, 4:8, :], in_=psT[p][:, 4:8, :]),
                     reads=[b_psT[p]], writes=[b_xTb[s]])

            def lg3(i):
                s, p = i % 3, i % 2
                for c in range(8):
                    k.op("pe", lambda e: e.matmul(psL[p][:, 0:36], lhsT=xT[s][:, c, :], rhs=wr[:, c, :],
                                                  start=(c == 0), stop=(c == 7)),
                         reads=[b_xTa[s], b_xTb[s], b_wr], writes=[b_psL[p]])

            def lg4(i):
                p = i % 2
                k.op("dve", lambda e: e.tensor_tensor(out=lg_all[:, i, :], in0=psL[p][:, 0:36], in1=rbias[:],
                                                      op=ALU.add),
                     reads=[b_psL[p], b_rbias], writes=[b_lg[i]])

            run_pipeline(NT, [lg0, lg1, lg2, lg3, lg4])

            def st(name, shape, dt=F32):
                return k.sb(T + name, shape, dt)

            lgg = lg_all[:, :, 0:4]
            lge = lg_all[:, :, 4:36]
            gmax = st("gmax", [128, NT])
            ohg = st("ohg", [128, NT, 4])
            egs = st("egs", [128, NT, 4])
            gsum = st("gsum", [128, NT])
            ggate = st("ggate", [128, NT])
            pen = st("pen", [128, NT, 4])
            me = st("me", [128, NT, 32])
            me2 = me
            v1 = st("v1", [128, NT])
            v2 = st("v2", [128, NT])
            sel1 = st("sel1", [128, NT, 32])
            sel2 = st("sel2", [128, NT, 32])
            dd = st("dd", [128, NT])
            p1 = st("p1", [128, NT])
            g1 = st("g1", [128, NT])
            g2 = st("g2", [128, NT])
            A = st("A", [128, NT, 32], BF16)
            dest = st("dest", [128, NT, 32])
            ov = st("ov", [128, NT, 32])
            tmp = st("tmp", [128, NT, 32])
            idxf = st("idxf", [128, NT, 2])
            ovs = st("ovs", [128, NT, 2])
            b_r = k.buf()

            def dv(fn, extra_r=()):
                k.op("dve", fn, reads=[b_r, *extra_r], writes=[b_r])

            def bc(ap, n):
                return ap.unsqueeze(2).to_broadcast([128, NT, n])

            dv(lambda e: e.tensor_reduce(out=gmax[:], in_=lgg, axis=AX.X, op=ALU.max), extra_r=b_lg)
            dv(lambda e: e.tensor_tensor(out=ohg[:], in0=lgg, in1=bc(gmax[:], 4), op=ALU.is_ge))
            dv(lambda e: e.tensor_tensor(out=egs[:], in0=lgg, in1=bc(gmax[:], 4), op=ALU.subtract))
            k.op("act", lambda e: e.activation(out=egs[:], in_=egs[:], func=AF.Exp), reads=[b_r], writes=[b_r])
            dv(lambda e: e.tensor_reduce(out=gsum[:], in_=egs[:], axis=AX.X, op=ALU.add))
            dv(lambda e: e.reciprocal(out=ggate[:], in_=gsum[:]))
            dv(lambda e: e.tensor_scalar(out=pen[:], in0=ohg[:], scalar1=1e30, scalar2=-1e30,
                                         op0=ALU.mult, op1=ALU.add))
            me4 = me[:].rearrange("p t (g j) -> p t g j", g=4)
            lge4 = lge.rearrange("p t (g j) -> p t g j", g=4)
            dv(lambda e: e.tensor_tensor(out=me4, in0=lge4, in1=ohg[:].unsqueeze(3).to_broadcast([128, NT, 4, 8]),
                                         op=ALU.mult))
            dv(lambda e: e.tensor_tensor(out=me4, in0=me4, in1=pen[:].unsqueeze(3).to_broadcast([128, NT, 4, 8]),
                                         op=ALU.add))
            dv(lambda e: e.tensor_reduce(out=v1[:], in_=me[:], axis=AX.X, op=ALU.max))
            dv(lambda e: e.tensor_tensor(out=sel1[:], in0=me[:], in1=bc(v1[:], 32), op=ALU.is_equal))
            dv(lambda e: e.scalar_tensor_tensor(out=me2[:], in0=sel1[:], scalar=-2e30, in1=me[:],
                                                op0=ALU.mult, op1=ALU.add))
            dv(lambda e: e.tensor_reduce(out=v2[:], in_=me2[:], axis=AX.X, op=ALU.max))
            dv(lambda e: e.tensor_tensor(out=sel2[:], in0=me2[:], in1=bc(v2[:], 32), op=ALU.is_equal))
            dv(lambda e: e.tensor_tensor(out=dd[:], in0=v2[:], in1=v1[:], op=ALU.subtract))
            k.op("act", lambda e: e.activation(out=dd[:], in_=dd[:], func=AF.Exp), reads=[b_r], writes=[b_r])
            dv(lambda e: e.tensor_scalar(out=dd[:], in0=dd[:], scalar1=1.0, scalar2=None, op0=ALU.add))
            dv(lambda e: e.reciprocal(out=p1[:], in_=dd[:]))
            dv(lambda e: e.tensor_tensor(out=g1[:], in0=p1[:], in1=ggate[:], op=ALU.mult))
            dv(lambda e: e.tensor_tensor(out=g2[:], in0=ggate[:], in1=g1[:], op=ALU.subtract))
            dv(lambda e: e.tensor_tensor(out=A[:], in0=sel1[:], in1=sel2[:], op=ALU.add))

            psP = k.ps(T + "psP", [128, NT, 32], F32)
            b_psP = k.buf()
            for i in range(NT):
                k.op("pe", lambda e: e.matmul(psP[:, i, :], lhsT=cm.upper_b, rhs=A[:, i, :], start=True,
                                              stop=(i == 0)),
                     reads=[b_r, cm.b_cb], writes=[b_psP])
                for j in range(i):
                    k.op("pe", lambda e: e.matmul(psP[:, i, :], lhsT=cm.ones_b, rhs=A[:, j, :], start=False,
                                                  stop=(j == i - 1)),
                         reads=[b_r, cm.b_cb], writes=[b_psP])

            dv(lambda e: e.tensor_tensor(out=dest[:], in0=psP[:], in1=cm.ebase.unsqueeze(1).to_broadcast([128, NT, 32]),
                                         op=ALU.add), extra_r=[b_psP, cm.b_cf])
            dv(lambda e: e.tensor_scalar(out=ov[:], in0=psP[:], scalar1=float(CAP), scalar2=None, op0=ALU.is_ge),
               extra_r=[b_psP])
            dv(lambda e: e.scalar_tensor_tensor(out=dest[:], in0=ov[:], scalar=1.0e6, in1=dest[:],
                                                op0=ALU.mult, op1=ALU.add))
            for j, sel in enumerate((sel1, sel2)):
                dv(lambda e: e.tensor_tensor(out=tmp[:], in0=sel[:], in1=dest[:], op=ALU.mult))
                dv(lambda e: e.tensor_reduce(out=idxf[:, :, j], in_=tmp[:], axis=AX.X, op=ALU.add))
                dv(lambda e: e.tensor_tensor(out=tmp[:], in0=sel[:], in1=ov[:], op=ALU.mult))
                dv(lambda e: e.tensor_reduce(out=ovs[:, :, j], in_=tmp[:], axis=AX.X, op=ALU.add))
            k.op("dve", lambda e: e.tensor_copy(out=idx_all[:], in_=idxf[:]), reads=[b_r], writes=[b_idx])
            dv(lambda e: e.tensor_scalar(out=ovs[:], in0=ovs[:], scalar1=-1.0, scalar2=1.0, op0=ALU.mult, op1=ALU.add))
            k.op("dve", lambda e: e.tensor_tensor(out=gate_all[:, :, 0], in0=g1[:], in1=ovs[:, :, 0], op=ALU.mult),
                 reads=[b_r], writes=[b_gate])
            k.op("dve", lambda e: e.tensor_tensor(out=gate_all[:, :, 1], in0=g2[:], in1=ovs[:, :, 1], op=ALU.mult),
                 reads=[b_r], writes=[b_gate])

            b_xbuf = k.buf()
            xs = [xt[0][:], xt[1][:], xt[2][:], xT[0][:].rearrange("p c t -> p (c t)")]
            b_xs = [b_xt[0], b_xt[1], b_xt[2], k.buf()]
            barrier(k)
            for i in range(NT):
                s = i % 4
                k.dma("sp", lambda e: e.dma_start(out=xs[s], in_=xin[i * 128:(i + 1) * 128, :]),
                      reads=[b_xin], writes=[b_xs[s]])
                for j in range(2):
                    k.dma("pool", lambda g: g.indirect_dma_start(
                        out=xbuf, out_offset=bass.IndirectOffsetOnAxis(ap=idx_all[:, i, j:j + 1], axis=0),
                        in_=xs[s], in_offset=None, bounds_check=cm.bound_reg, oob_is_err=False),
                        reads=[b_xs[s], b_idx], writes=[b_xbuf])
            barrier(k)
        k.es = es

        b_ybuf = k.buf()
        with ExitStack() as es2:
            k.es = es2
            NB = CAP // 128
            NXS = 3
            xe = [k.sb(f"{T}xe{i}", [128, NB, D], BF16) for i in range(NXS)]
            b_xe = [k.buf() for _ in range(NXS)]
            xeT = [k.sb(f"{T}xeT{i}", [128, 8, CAP], BF16) for i in range(NXS)]
            b_xeT = [[k.buf() for _ in range(NB)] for _ in range(NXS)]
            hd = [k.sb(f"{T}hd{i}", [128, 4, CAP], BF16) for i in range(2)]
            b_hd = [[k.buf() for _ in range(4)] for _ in range(2)]
            sil = [k.sb(f"{T}sil{i}", [128, CAP], F32) for i in range(2)]
            b_sil = [k.buf() for _ in range(2)]
            yt = [k.sb(f"{T}yt{i}", [128, NB, D], BF16) for i in range(2)]
            b_yt = [[[k.buf() for _ in range(2)] for _ in range(NB)] for _ in range(2)]
            psX = [k.ps(f"{T}psX{i}", [128, D], BF16) for i in range(2)]
            b_psX = [k.buf() for _ in range(2)]
            psH1 = [k.ps(f"{T}psH1_{i}", [128, 512], F32) for i in range(2)]
            psH3 = [k.ps(f"{T}psH3_{i}", [128, 512], F32) for i in range(2)]
            b_psH1 = [k.buf() for _ in range(2)]
            b_psH3 = [k.buf() for _ in range(2)]
            psY = [k.ps(f"{T}psY{i}", [128, 512], F32) for i in range(2)]
            b_psY = [k.buf() for _ in range(2)]
            cntX = [0]

            def stage_load(e):
                s = e % NXS
                k.dma("sp", lambda q: q.dma_start(
                    out=xe[s][:], in_=xbuf[e * CAP:(e + 1) * CAP, :].rearrange("(b p) d -> p b d", p=128)),
                    reads=[b_xbuf], writes=[b_xe[s]])

            def grp_T(e, blk):
                def f():
                    s = e % NXS
                    px = cntX[0] % 2
                    cntX[0] += 1
                    for c in range(8):
                        k.op("pe", lambda q: q.transpose(out=psX[px][:, c * 128:(c + 1) * 128],
                                                         in_=xe[s][:, blk, c * 128:(c + 1) * 128],
                                                         identity=cm.ident_b),
                             reads=[b_xe[s], cm.b_cb], writes=[b_psX[px]])
                    src = psX[px][:].rearrange("p (c t) -> p c t", c=8)
                    if px == 0:
                        k.op("dve", lambda q: q.tensor_copy(out=xeT[s][:, :, blk * 128:(blk + 1) * 128], in_=src),
                             reads=[b_psX[px]], writes=[b_xeT[s][blk]])
                    else:
                        k.op("act", lambda q: q.activation(out=xeT[s][:, :, blk * 128:(blk + 1) * 128], in_=src,
                                                           func=AF.Copy),
                             reads=[b_psX[px]], writes=[b_xeT[s][blk]])
                return f

            def grp_H(e, m):
                def f():
                    s = e % NXS
                    hs = e % 2
                    ws = e % NWB
                    ph = m % 2
                    for c in range(8):
                        k.op("pe", lambda q: q.matmul(psH1[ph][:, 0:CAP], lhsT=w1t[ws][:, c, m * 128:(m + 1) * 128],
                                                      rhs=xeT[s][:, c, :], start=(c == 0), stop=(c == 7)),
                             reads=[*b_xeT[s], b_w13[ws]], writes=[b_psH1[ph]])
                    k.op("act", lambda q: q.activation(out=sil[ph][:], in_=psH1[ph][:, 0:CAP], func=AF.Silu),
                         reads=[b_psH1[ph]], writes=[b_sil[ph]])
                    for c in range(8):
                        k.op("pe", lambda q: q.matmul(psH3[ph][:, 0:CAP], lhsT=w3t[ws][:, c, m * 128:(m + 1) * 128],
                                                      rhs=xeT[s][:, c, :], start=(c == 0), stop=(c == 7)),
                             reads=[*b_xeT[s], b_w13[ws]], writes=[b_psH3[ph]])
                    k.op("dve", lambda q: q.tensor_tensor(out=hd[hs][:, m, :], in0=sil[ph][:], in1=psH3[ph][:, 0:CAP],
                                                          op=ALU.mult),
                         reads=[b_sil[ph], b_psH3[ph]], writes=[b_hd[hs][m]])
                return f

            def grp_Y(e, blk, half):
                def f():
                    hs = e % 2
                    ws = e % NWB
                    py = (blk * 2 + half) % 2
                    for c in range(4):
                        k.op("pe", lambda q: q.matmul(psY[py][:], lhsT=hd[hs][:, c, blk * 128:(blk + 1) * 128],
                                                      rhs=w2t[ws][:, c, half * 512:(half + 1) * 512],
                                                      start=(c == 0), stop=(c == 3)),
                             reads=[b_hd[hs][c], b_w2[ws]], writes=[b_psY[py]])
                    if half == 0:
                        k.op("act", lambda q: q.activation(out=yt[hs][:, blk, 0:512], in_=psY[py][:], func=AF.Copy),
                             reads=[b_psY[py]], writes=[b_yt[hs][blk][0]])
                    else:
                        k.op("dve", lambda q: q.tensor_copy(out=yt[hs][:, blk, 512:1024], in_=psY[py][:]),
                             reads=[b_psY[py]], writes=[b_yt[hs][blk][1]])
                return f

            def store_Y(e):
                hs = e % 2
                k.dma("sp", lambda q: q.dma_start(
                    out=ybuf[e * CAP:(e + 1) * CAP, :].rearrange("(b p) d -> p b d", p=128), in_=yt[hs][:]),
                    reads=[bb for blk in b_yt[hs] for bb in blk], writes=[b_ybuf])

            for e0 in range(min(NXS, NE)):
                stage_load(e0)
            for blk in range(NB):
                grp_T(0, blk)()
            for blk in range(NB):
                grp_T(1, blk)()
            for m in range(4):
                grp_H(0, m)()
            for e in range(NE):
                Tg = [grp_T(e + 2, blk) for blk in range(NB)] if e + 2 < NE else []
                Hg = [grp_H(e + 1, m) for m in range(4)] if e + 1 < NE else []
                Yg = [grp_Y(e, blk, half) for blk in range(NB) for half in range(2)]
                order = []
                ti = hi = 0
                for yi, yg in enumerate(Yg):
                    order.append(yg)
                    if yi % 2 == 0 and hi < len(Hg):
                        order.append(Hg[hi]); hi += 1
                    if yi % 2 == 1 and ti < len(Tg):
                        order.append(Tg[ti]); ti += 1
                order += Hg[hi:] + Tg[ti:]
                for g_ in order:
                    g_()
                store_Y(e)
                if e + NWB < NE:
                    load_weights(e + NWB)
                if e + NXS < NE:
                    stage_load(e + NXS)
            barrier(k)
        k.es = es

        with ExitStack() as es2:
            k.es = es2
            NY, NA = 3, 6
            y1 = [k.sb(f"{T}y1_{i}", [128, D], BF16) for i in range(NY)]
            y2 = [k.sb(f"{T}y2_{i}", [128, D], BF16) for i in range(NY)]
            b_y1 = [k.buf() for _ in range(NY)]
            b_y2 = [k.buf() for _ in range(NY)]
            xr = [k.sb(f"{T}xr_{i}", [128, D], F32) for i in range(NY)]
            b_xr = [k.buf() for _ in range(NY)]
            acc = [k.sb(f"{T}acc_{i}", [128, D], F32) for i in range(NA)]
            b_acc = [k.buf() for _ in range(NA)]
            ot = [k.sb(f"{T}ot_{i}", [128, D], F32) for i in range(3)]
            b_ot = [k.buf() for _ in range(3)]
            for i in range(NY):
                k.op("pool", lambda q: q.memset(y1[i][:], 0.0), writes=[b_y1[i]])
                k.op("pool", lambda q: q.memset(y2[i][:], 0.0), writes=[b_y2[i]])
            lnp = {}

            def cA(i):
                s, sa = i % NY, i % NA
                k.dma("sp", lambda q: q.dma_start(out=xr[s][:], in_=xin[i * 128:(i + 1) * 128, :]),
                      reads=[b_xin], writes=[b_xr[s]])
                k.dma("pool", lambda g: g.indirect_dma_start(
                    out=y1[s][:], out_offset=None, in_=ybuf,
                    in_offset=bass.IndirectOffsetOnAxis(ap=idx_all[:, i, 0:1], axis=0),
                    bounds_check=cm.bound_reg, oob_is_err=False), reads=[b_ybuf, b_idx], writes=[b_y1[s]])
                k.dma("pool", lambda g: g.indirect_dma_start(
                    out=y2[s][:], out_offset=None, in_=ybuf,
                    in_offset=bass.IndirectOffsetOnAxis(ap=idx_all[:, i, 1:2], axis=0),
                    bounds_check=cm.bound_reg, oob_is_err=False), reads=[b_ybuf, b_idx], writes=[b_y2[s]])

            def cB(i):
                s, sa = i % NY, i % NA
                k.op("act", lambda q: q.activation(out=acc[sa][:], in_=xr[s][:], func=AF.Copy, scale=ALPHA),
                     reads=[b_xr[s]], writes=[b_acc[sa]])
                k.op("dve", lambda q: q.scalar_tensor_tensor(out=acc[sa][:], in0=y1[s][:], scalar=gate_all[:, i, 0:1],
                                                             in1=acc[sa][:], op0=ALU.mult, op1=ALU.add),
                     reads=[b_y1[s], b_gate, b_acc[sa]], writes=[b_acc[sa]])
                k.op("dve", lambda q: q.scalar_tensor_tensor(out=acc[sa][:], in0=y2[s][:], scalar=gate_all[:, i, 1:2],
                                                             in1=acc[sa][:], op0=ALU.mult, op1=ALU.add),
                     reads=[b_y2[s], b_gate, b_acc[sa]], writes=[b_acc[sa]])
                lnp[i] = layer_norm_parts(k, T + "ln", acc[sa], b_acc[sa], ot[i % 3], b_ot[i % 3], gam, bet, b_gb, sa)

            def cC(i):
                lnp[i][0]()

            def cD(i):
                lnp[i][1]()

            def cE(i):
                lnp[i][2]()
                k.dma("sp", lambda q: q.dma_start(out=xout[i * 128:(i + 1) * 128, :], in_=ot[i % 3][:]),
                      reads=[b_ot[i % 3]], writes=[b_xout])

            run_pipeline(NT, [cA, cB, cC, cD, cE])
            barrier(k)
        k.es = es
    k.es = es_outer


_ln_scratch = {}


def layer_norm_parts(k, tag, acc, b_acc, ot, b_ot, gam, bet, b_gb, s):
    key = (tag, s, id(k))
    if key not in _ln_scratch:
        _ln_scratch[key] = (k.sb(f"{tag}st{s}", [128, 2, 6], F32), k.sb(f"{tag}mv{s}", [128, 2], F32),
                            k.sb(f"{tag}rs{s}", [128, 1], F32), k.buf())
    stt, mv, rs, b_s = _ln_scratch[key]
    mhalf = k.mhalf

    def part_a():
        for h in range(2):
            k.op("dve", lambda q: q.bn_stats(out=stt[:, h, :], in_=acc[:, h * 512:(h + 1) * 512]),
                 reads=[b_acc], writes=[b_s])
        k.op("dve", lambda q: q.bn_aggr(out=mv[:], in_=stt[:].rearrange("p a b -> p (a b)")), reads=[b_s], writes=[b_s])
        k.op("dve", lambda q: q.tensor_scalar(out=rs[:], in0=mv[:, 1:2], scalar1=LN_EPS, scalar2=None, op0=ALU.add),
             reads=[b_s], writes=[b_s])
        k.op("pool", lambda q: q.tensor_tensor(out=rs[:], in0=rs[:], in1=mhalf[:], op=ALU.pow),
             reads=[b_s], writes=[b_s])

    def part_b():
        k.op("dve", lambda q: q.tensor_scalar(out=acc[:], in0=acc[:], scalar1=mv[:, 0:1], scalar2=rs[:, 0:1],
                                              op0=ALU.subtract, op1=ALU.mult),
             reads=[b_s, b_acc], writes=[b_acc])
        k.op("dve", lambda q: q.tensor_tensor(out=acc[:], in0=acc[:], in1=gam[:], op=ALU.mult),
             reads=[b_acc, b_gb], writes=[b_acc])

    def part_c():
        k.op("dve", lambda q: q.tensor_tensor(out=ot[:], in0=acc[:], in1=bet[:], op=ALU.add),
             reads=[b_acc, b_gb], writes=[b_ot])

    return part_a, part_b, part_c


def layer_norm_tile(k, tag, acc, b_acc, ot, b_ot, gam, bet, b_gb, s):
    for part in layer_norm_parts(k, tag, acc, b_acc, ot, b_ot, gam, bet, b_gb, s):
        part()


class Deferred:
    def __init__(self):
        self.q = []
        self.it = 0

    def add(self, delay, fn):
        self.q.append((self.it + delay, fn))

    def tick(self, it):
        self.it = it + 1
        keep = []
        for due, fn in self.q:
            if due <= it:
                fn()
            else:
                keep.append((due, fn))
        self.q = keep

    def flush(self):
        while self.q:
            q, self.q = self.q, []
            for _, fn in q:
                fn()


def run_pipeline(n, stages, deferred=None):
    S = len(stages)
    for it in range(n + S - 1):
        if deferred is not None:
            deferred.it = it
        for st in range(S):
            i = it - st
            if 0 <= i < n:
                stages[st](i)
        if deferred is not None:
            deferred.tick(it)
    if deferred is not None:
        deferred.flush()


def barrier(k):
    deps = {}
    for n in k.engs:
        if k.cnt[n] > 0:
            s = k.sem[n]
            deps[id(s)] = (s, k.cnt[n])
    for q, ring in k.ring.items():
        for s, v in ring:
            if v > 0:
                deps[id(s)] = (s, v)
    for n in k.engs:
        k._wait(n, dict(deps))


def build_program(phases=("rglru", "moe0", "attn", "moe1")):
    nc = bass.Bass("TRN2", target_bir_lowering=False)

    need_moe = any(p.startswith("moe") for p in phases)

    def inp(name, shape):
        if name.startswith("moe_") and not need_moe:
            return None
        if name.startswith("rec_") and "rglru" not in phases:
            return None
        if name.startswith("att_") and "attn" not in phases:
            return None
        return nc.dram_tensor(name, list(shape), F32, kind="ExternalInput").ap()

    x = inp("x", [TOK, D])
    consts = inp("consts", [128, NCONST])
    moe_w_group = inp("moe_w_group", [2, D, 4])
    moe_b_group = inp("moe_b_group", [2, 4])
    moe_w_expert = inp("moe_w_expert", [2, D, NE])
    moe_b_expert = inp("moe_b_expert", [2, NE])
    moe_w1 = inp("moe_w1", [2, NE, D, DE])
    moe_w3 = inp("moe_w3", [2, NE, D, DE])
    moe_w2 = inp("moe_w2", [2, NE, DE, D])
    ln_g = inp("ln_g", [2, 2, D])
    ln_b = inp("ln_b", [2, 2, D])
    rec_w_in = inp("rec_w_in", [D, 2 * D_RNN])
    rec_cdiag = inp("rec_cdiag", [RC, NRC, 4, RC])
    rec_prm = inp("rec_prm", [RC, 4, NRC])
    rec_w_r = inp("rec_w_r", [NRC, RC, RC])
    rec_w_i = inp("rec_w_i", [NRC, RC, RC])
    rec_w_out = inp("rec_w_out", [D_RNN, D])
    att_w_qkv = inp("att_w_qkv", [D, 1536])
    att_sinks = inp("att_sinks", [NH])
    att_w_o = inp("att_w_o", [D, D])
    att_bias = inp("att_bias", [128, NH, 256])
    out = nc.dram_tensor("out", [TOK, D], F32, kind="ExternalOutput").ap()
    with ExitStack() as es:
        k = K(nc, es)
        cm = Common(k, consts)
        xbuf = k.dram("xbuf", [NSLOT, D], BF16)
        ybuf = k.dram("ybuf", [NSLOT, D], BF16)
        cur, b_cur = x, k.buf("x")
        b_out = k.buf("out")
        for pi, ph in enumerate(phases):
            last = pi == len(phases) - 1
            if last:
                nxt, b_nxt = out, b_out
            else:
                nxt, b_nxt = k.dram(f"act{pi}", [TOK, D], F32), k.buf(f"act{pi}")
            if ph == "rglru":
                rglru_phase(k, cm, "r0", cur, b_cur, nxt, b_nxt, rec_w_in, rec_cdiag, rec_prm, rec_w_r, rec_w_i,
                            rec_w_out, ln_g[0, 0], ln_b[0, 0])
            elif ph == "attn":
                attn_phase(k, cm, "a1", cur, b_cur, nxt, b_nxt, att_w_qkv, att_sinks, att_w_o, att_bias,
                           ln_g[1, 0], ln_b[1, 0])
            elif ph in ("moe0", "moe1"):
                l = int(ph[3])
                moe_phase(k, cm, f"m{l}", cur, b_cur, nxt, b_nxt, moe_w_group[l], moe_b_group[l], moe_w_expert[l],
                          moe_b_expert[l], moe_w1[l], moe_w3[l], moe_w2[l], ln_g[l, 1], ln_b[l, 1], xbuf, ybuf)
            cur, b_cur = nxt, b_nxt
        if DBG["stop"]:
            k.dma("sp", lambda e: e.dma_start(out=out[0:128, :], in_=x[0:128, :]), writes=[b_out])
        k.finish([b_out])
        barrier(k)
    return nc


def host_inputs(inputs):
    f = lambda a: np.ascontiguousarray(np.asarray(a, dtype=np.float32))
    d = {}
    d["consts"] = make_consts()
    for n in ("moe_w_group", "moe_b_group", "moe_w_expert", "moe_b_expert", "moe_w1", "moe_w3", "moe_w2",
              "ln_g", "ln_b"):
        d[n] = f(inputs[n])
    d["rec_w_in"] = f(inputs["rec_w_in"][0])
    cw = np.asarray(inputs["rec_conv_w"][0], np.float32)
    cdiag = np.zeros((RC, NRC, 4, RC), np.float32)
    pp = np.arange(RC)
    for c in range(NRC):
        for j in range(4):
            cdiag[pp, c, j, pp] = cw[j, c * RC:(c + 1) * RC]
    d["rec_cdiag"] = cdiag
    prm = np.stack([np.asarray(inputs[n][0], np.float32).reshape(NRC, RC).T
                    for n in ("rec_conv_b", "rec_b_r", "rec_b_i", "rec_lambda")], axis=1)
    d["rec_prm"] = f(prm)
    d["rec_w_r"] = f(inputs["rec_w_r"][0])
    d["rec_w_i"] = f(inputs["rec_w_i"][0])
    d["rec_w_out"] = f(inputs["rec_w_out"][0])
    d["att_w_qkv"] = f(inputs["att_w_qkv"][0])
    d["att_sinks"] = f(inputs["att_sinks"][0])
    d["att_w_o"] = f(inputs["att_w_o"][0])
    d["att_bias"] = make_att_bias()
    return d


def make_att_bias():
    slopes = 2.0 ** (-8.0 * np.arange(1, NH + 1, dtype=np.float32) / NH)
    qi = np.arange(128)[:, None]
    sj = np.arange(256)[None, :]
    dist = qi - sj + 128
    valid = (dist >= 0) & (dist < 128)
    b = np.where(valid[:, None, :], -slopes[None, :, None] * dist[:, None, :].astype(np.float32), -30000.0)
    return np.ascontiguousarray(b.astype(np.float32))


_PROG = {}


def kernel(**inputs):
    key = "full"
    if key not in _PROG:
        _PROG[key] = build_program()
    nc = _PROG[key]
    shared = host_inputs(inputs)
    x = np.ascontiguousarray(np.asarray(inputs["x"], np.float32)).reshape(NCORES, TOK, D)
    in_maps = [dict(shared, x=x[c]) for c in range(NCORES)]
    res = run_bass_kernel_spmd(nc, in_maps, core_ids=list(range(NCORES)))
    out = np.stack([np.asarray(r["out"]) for r in res.results], axis=0)
    return out.reshape(16, SEQ, D).astype(np.float32)


GELU_K = 0.7978845608028654
DBG = {"stop": 0}


class _Stop(Exception):
    pass

TCH = 1024


def rglru_phase(k, cm, T, xin, b_xin, xout, b_xout, w_in, cdiag, prm_d, w_r, w_i, w_out, ln_g, ln_b):
    nc = k.nc
    es_outer = k.es
    with ExitStack() as es:
        k.es = es
        P = RC
        gam = k.sb(T + "gam", [128, D], F32)
        bet = k.sb(T + "bet", [128, D], F32)
        b_gb = k.buf()
        k.dma("sp", lambda e: e.dma_start(out=gam[:], in_=ln_g.partition_broadcast(128)), writes=[b_gb])
        k.dma("sp", lambda e: e.dma_start(out=bet[:], in_=ln_b.partition_broadcast(128)), writes=[b_gb])
        wo_full = k.sb(T + "wo", [128, NRC, D], BF16)
        b_wo = k.buf()
        k.op("pool", lambda e: e.memset(wo_full[:], 0.0), writes=[b_wo])
        wo = wo_full[0:P]
        k.dma("pool", lambda g: g.dma_start(out=wo[:], in_=w_out.rearrange("(c p) n -> p c n", p=P)), writes=[b_wo])
        cd_full = k.sb(T + "cd", [128, NRC, 4, P], BF16)
        b_cd = k.buf()
        k.op("pool", lambda e: e.memset(cd_full[:], 0.0), writes=[b_cd])
        cd = cd_full[0:P]
        for c in range(NRC):
            k.dma("pool", lambda g: g.dma_start(out=cd[:, c, :, :], in_=cdiag[:, c, :, :]), writes=[b_cd])
        wrt_full = k.sb(T + "wrt", [128, NRC, P], BF16)
        b_wri = k.buf()
        k.op("pool", lambda e: e.memset(wrt_full[:], 0.0), writes=[b_wri])
        wrt = wrt_full[0:P]
        wit_full = k.sb(T + "wit", [128, NRC, P], BF16)
        k.op("pool", lambda e: e.memset(wit_full[:], 0.0), writes=[b_wri])
        wit = wit_full[0:P]
        k.dma("pool", lambda g: g.dma_start(out=wrt[:], in_=w_r.rearrange("n k j -> k n j")), writes=[b_wri])
        k.dma("pool", lambda g: g.dma_start(out=wit[:], in_=w_i.rearrange("n k j -> k n j")), writes=[b_wri])
        prm = k.sb(T + "prm", [P, 4, NRC], F32)
        b_prm = k.buf()
        k.dma("sp", lambda e: e.dma_start(out=prm[:], in_=prm_d), writes=[b_prm])
        der = k.sb(T + "der", [P, 5, NRC], F32)
        b_der = k.buf()
        k.op("dve", lambda e: e.tensor_scalar(out=der[:, 0:2, :], in0=prm[:, 1:3, :], scalar1=0.5, scalar2=None,
                                              op0=ALU.mult), reads=[b_prm], writes=[b_der])
        k.op("act", lambda e: e.activation(out=der[:, 4, :], in_=prm[:, 3, :], func=AF.Exp, scale=-1.0),
             reads=[b_prm, b_der], writes=[b_der])
        k.op("act", lambda e: e.activation(out=der[:, 4, :], in_=der[:, 4, :], func=AF.Ln, bias=1.0),
             reads=[b_der], writes=[b_der])
        k.op("dve", lambda e: e.tensor_scalar(out=der[:, 2, :], in0=der[:, 4, :], scalar1=-4.0, scalar2=None,
                                              op0=ALU.mult), reads=[b_der], writes=[b_der])
        k.op("dve", lambda e: e.tensor_scalar(out=der[:, 3, :], in0=der[:, 4, :], scalar1=-8.0, scalar2=None,
                                              op0=ALU.mult), reads=[b_der], writes=[b_der])

        if DBG["stop"] == 1:
            barrier(k); k.es = es_outer; return
        cvb = k.sb(T + "cvb", [P, NRC], F32)
        k.op("dve", lambda e: e.tensor_copy(out=cvb[:], in_=prm[:, 0, :]), reads=[b_prm], writes=[b_prm])
        xT = k.sb(T + "xT", [128, 8, TCH], BF16)
        b_xT = k.buf()
        YT_full = k.sb(T + "YT", [128, NRC, TCH], BF16)
        k.op("pool", lambda e: e.memset(YT_full[:], 0.0))
        YT = YT_full[0:P]
        b_YT = [k.buf() for _ in range(NRC)]
        xrc = k.sb(T + "xrc", [P, NRC, 4], F32)
        hc = k.sb(T + "hc", [P, NRC], F32)
        b_xrc = [k.buf() for _ in range(NRC)]
        b_hc = [k.buf() for _ in range(NRC)]
        xt = [k.sb(f"{T}xt{i}", [128, D], F32) for i in range(2)]
        b_xt = [k.buf() for _ in range(2)]
        xtb = [k.sb(f"{T}xtb{i}", [128, D], BF16) for i in range(2)]
        b_xtb = [k.buf() for _ in range(2)]
        wg = [[k.sb(f"{T}wg{i}_{j}", [128, 8, P], BF16) for j in range(2)] for i in range(2)]
        b_wg = [k.buf() for _ in range(2)]
        G = [k.sb(f"{T}G{i}", [P, TCH], F32) for i in range(2)]
        XC = [k.sb(f"{T}XC{i}", [P, TCH], F32) for i in range(2)]
        TR = [k.sb(f"{T}TR{i}", [P, TCH], F32) for i in range(2)]
        TI = [k.sb(f"{T}TI{i}", [P, TCH], F32) for i in range(2)]
        NQ = TCH // 512
        b_G = [[k.buf() for _ in range(NQ)] for _ in range(2)]
        b_XC = [[k.buf() for _ in range(NQ)] for _ in range(2)]
        b_TR = [[k.buf() for _ in range(NQ)] for _ in range(2)]
        b_TI = [[k.buf() for _ in range(NQ)] for _ in range(2)]
        AA = k.sb(T + "AA", [P, TCH], F32)
        SQ = k.sb(T + "SQ", [P, TCH], F32)
        S2 = k.sb(T + "S2", [P, TCH], F32)
        HH = SQ
        XR_full = k.sb(T + "XR", [128, TCH + 4], BF16)
        k.op("pool", lambda e: e.memset(XR_full[:], 0.0))
        XR = XR_full[0:P]
        XCb_full = k.sb(T + "XCb", [128, TCH], BF16)
        k.op("pool", lambda e: e.memset(XCb_full[:], 0.0))
        XCb = XCb_full[0:P]
        XRf = k.sb(T + "XRf", [P, TCH + 4], F32)
        b_XRf = k.buf()
        XRb_full = k.sb(T + "XRb", [128, TCH + 4], BF16)
        k.op("pool", lambda e: e.memset(XRb_full[:], 0.0))
        XRb = XRb_full[0:P]
        b_XRb = k.buf()
        b_AA, b_SQ, b_S2, b_XR = [k.buf() for _ in range(4)]
        b_XCb = [k.buf() for _ in range(NQ)]
        NACC = 4
        acc = [k.sb(f"{T}acc{i}", [128, D], F32) for i in range(NACC)]
        b_acc = [k.buf() for _ in range(NACC)]
        psTb = k.ps(T + "psTb", [128, D], BF16)
        b_psTb = k.buf()
        pA = [k.ps(f"{T}pA{i}", [128, 512], F32) for i in range(5)]
        b_pA = [k.buf() for _ in range(5)]
        pO = [k.ps(f"{T}pO{i}", [128, 512], F32) for i in range(2)]
        b_pO = [k.buf() for _ in range(2)]
        barrier(k)
        npa = [0]

        def nxt():
            npa[0] += 1
            return npa[0] % 5

        def qs(q):
            return slice(q * 512, (q + 1) * 512)

        def load_wchunk(c):
            s = c % 2
            k.dma("pool", lambda g: g.dma_start(out=wg[s][0][:],
                                                in_=w_in[:, c * P:(c + 1) * P].rearrange("(c p) n -> p c n", p=128)),
                  writes=[b_wg[s]])
            k.dma("pool", lambda g: g.dma_start(out=wg[s][1][:],
                                                in_=w_in[:, D_RNN + c * P:D_RNN + (c + 1) * P].rearrange(
                                                    "(c p) n -> p c n", p=128)),
                  writes=[b_wg[s]])

        for sq in range(2):
            for th in range(SEQ // TCH):
                tok0 = sq * SEQ + th * TCH
                load_wchunk(0)
                for tl in range(TCH // 128):
                    s = tl % 2
                    r0 = tok0 + tl * 128
                    k.dma("sp", lambda e: e.dma_start(out=xt[s][:], in_=xin[r0:r0 + 128, :]),
                          reads=[b_xin], writes=[b_xt[s]])
                    k.op("act", lambda e: e.activation(out=xtb[s][:], in_=xt[s][:], func=AF.Copy),
                         reads=[b_xt[s]], writes=[b_xtb[s]])
                    for c in range(8):
                        k.op("pe", lambda e: e.transpose(out=psTb[:, c * 128:(c + 1) * 128],
                                                         in_=xtb[s][:, c * 128:(c + 1) * 128], identity=cm.ident_b),
                             reads=[b_xtb[s], cm.b_cb], writes=[b_psTb])
                    k.op("dve", lambda e: e.tensor_copy(out=xT[:, :, tl * 128:(tl + 1) * 128],
                                                        in_=psTb[:].rearrange("p (c t) -> p c t", c=8)),
                         reads=[b_psTb], writes=[b_xT])

                def stA(c):
                    u = c % 2
                    ws = c % 2
                    if c + 1 < NRC:
                        load_wchunk(c + 1)
                    for q in range(NQ):
                        pa = nxt()
                        for kk in range(8):
                            k.op("pe", lambda e: e.matmul(pA[pa][0:P, :], lhsT=wg[ws][0][:, kk, :], rhs=xT[:, kk, qs(q)],
                                                          start=(kk == 0), stop=(kk == 7)),
                                 reads=[b_wg[ws], b_xT], writes=[b_pA[pa]])
                        k.op("act", lambda e: e.activation(out=G[u][:, qs(q)], in_=pA[pa][0:P, :], func=AF.Copy),
                             reads=[b_pA[pa]], writes=[b_G[u][q]])
                    for q in range(NQ):
                        pa = nxt()
                        for kk in range(8):
                            k.op("pe", lambda e: e.matmul(pA[pa][0:P, :], lhsT=wg[ws][1][:, kk, :],
                                                          rhs=xT[:, kk, qs(q)], start=(kk == 0), stop=(kk == 7)),
                                 reads=[b_wg[ws], b_xT], writes=[b_pA[pa]])
                        k.op("dve", lambda e: e.tensor_copy(out=XRf[:, 4 + q * 512:4 + (q + 1) * 512], in_=pA[pa][0:P, :]),
                             reads=[b_pA[pa]], writes=[b_XRf])
                    if th == 0:
                        k.op("pool", lambda e: e.memset(XRf[:, 0:4], 0.0), writes=[b_XRf])
                    else:
                        k.op("pool", lambda e: e.tensor_copy(out=XRf[:, 0:4], in_=xrc[:, c, :]),
                             reads=[b_xrc[c]], writes=[b_XRf])
                    k.op("pool", lambda e: e.tensor_copy(out=xrc[:, c, :], in_=XRf[:, TCH:TCH + 4]),
                         reads=[b_XRf], writes=[b_xrc[c]])
                    k.op("pool", lambda e: e.tensor_copy(out=XR[:, 0:TCH + 4], in_=XRf[:, 0:TCH + 4]),
                         reads=[b_XRf], writes=[b_XR])
                    k.op("dve", lambda e: e.tensor_copy(out=XRb[:, 0:TCH + 2], in_=XRf[:, 1:TCH + 3]),
                         reads=[b_XRf], writes=[b_XRb])
                    for q in range(NQ):
                        pa = nxt()
                        for j in range(4):
                            o = 1 + q * 512 + j
                            src = XR_full[:, o:o + 512] if o % 2 == 0 else XRb_full[:, o - 1:o - 1 + 512]
                            k.op("pe", lambda e: e.matmul(pA[pa][0:P, :], lhsT=cd_full[:, c, j, :], rhs=src,
                                                          start=(j == 0), stop=(j == 3)),
                                 reads=[b_cd, b_XR, b_XRb], writes=[b_pA[pa]])
                        k.op("dve", lambda e: e.tensor_scalar(out=XC[u][:, qs(q)], in0=pA[pa][0:P, :],
                                                              scalar1=cvb[:, c:c + 1], scalar2=None, op0=ALU.add),
                             reads=[b_pA[pa], b_prm], writes=[b_XC[u][q]])
                        k.op("act", lambda e: e.activation(out=XCb[:, qs(q)], in_=XC[u][:, qs(q)], func=AF.Copy),
                             reads=[b_XC[u][q]], writes=[b_XCb[q]])
                    for q in range(NQ):
                        pa = nxt()
                        k.op("pe", lambda e: e.matmul(pA[pa][0:P, :], lhsT=wrt_full[:, c, :], rhs=XCb_full[:, qs(q)],
                                                      start=True, stop=True),
                             reads=[b_wri, b_XCb[q]], writes=[b_pA[pa]])
                        k.op("act", lambda e: e.activation(out=TR[u][:, qs(q)], in_=pA[pa][0:P, :], func=AF.Tanh,
                                                           scale=0.5, bias=der[:, 0, c:c + 1]),
                             reads=[b_pA[pa], b_der], writes=[b_TR[u][q]])
                        pa = nxt()
                        k.op("pe", lambda e: e.matmul(pA[pa][0:P, :], lhsT=wit_full[:, c, :], rhs=XCb_full[:, qs(q)],
                                                      start=True, stop=True),
                             reads=[b_wri, b_XCb[q]], writes=[b_pA[pa]])
                        k.op("act", lambda e: e.activation(out=TI[u][:, qs(q)], in_=pA[pa][0:P, :], func=AF.Tanh,
                                                           scale=0.5, bias=der[:, 1, c:c + 1]),
                             reads=[b_pA[pa], b_der], writes=[b_TI[u][q]])

                def stB(c):
                    u = c % 2
                    k.op("dve", lambda e: e.tensor_tensor(out=S2[:], in0=G[u][:], in1=G[u][:], op=ALU.mult),
                         reads=b_G[u], writes=[b_S2])
                    k.op("dve", lambda e: e.tensor_scalar(out=S2[:], in0=S2[:], scalar1=0.044715, scalar2=1.0,
                                                          op0=ALU.mult, op1=ALU.add), reads=[b_S2], writes=[b_S2])
                    k.op("pool", lambda e: e.tensor_tensor(out=S2[:], in0=S2[:], in1=G[u][:], op=ALU.mult),
                         reads=[b_S2, *b_G[u]], writes=[b_S2])
                    k.op("act", lambda e: e.activation(out=AA[:], in_=TR[u][:], func=AF.Exp, scale=der[:, 2, c:c + 1],
                                                       bias=der[:, 2, c:c + 1]),
                         reads=[*b_TR[u], b_der], writes=[b_AA])
                    k.op("act", lambda e: e.activation(out=SQ[:], in_=TR[u][:], func=AF.Exp, scale=der[:, 3, c:c + 1],
                                                       bias=der[:, 3, c:c + 1]),
                         reads=[*b_TR[u], b_der], writes=[b_SQ])
                    k.op("act", lambda e: e.activation(out=S2[:], in_=S2[:], func=AF.Tanh, scale=GELU_K),
                         reads=[b_S2], writes=[b_S2])
                    k.op("act", lambda e: e.activation(out=SQ[:], in_=SQ[:], func=AF.Sqrt, scale=-1.0, bias=1.0),
                         reads=[b_SQ], writes=[b_SQ])
                    k.op("dve", lambda e: e.scalar_tensor_tensor(out=S2[:], in0=S2[:], scalar=1.0, in1=G[u][:],
                                                                 op0=ALU.add, op1=ALU.mult),
                         reads=[b_S2, *b_G[u]], writes=[b_S2])
                    k.op("dve", lambda e: e.scalar_tensor_tensor(out=TI[u][:], in0=TI[u][:], scalar=1.0, in1=XC[u][:],
                                                                 op0=ALU.add, op1=ALU.mult),
                         reads=[*b_TI[u], *b_XC[u]], writes=b_TI[u])
                    k.op("pool", lambda e: e.tensor_tensor(out=TI[u][:], in0=TI[u][:], in1=SQ[:], op=ALU.mult),
                         reads=[*b_TI[u], b_SQ], writes=b_TI[u])
                    init = 0.0 if th == 0 else hc[:, c:c + 1]
                    k.op("dve", lambda e: e.tensor_tensor_scan(out=HH[:], data0=AA[:], data1=TI[u][:], initial=init,
                                                               op0=ALU.mult, op1=ALU.add),
                         reads=[b_AA, *b_TI[u], b_hc[c], b_SQ], writes=[b_SQ])
                    k.op("pool", lambda e: e.tensor_copy(out=hc[:, c:c + 1], in_=HH[:, TCH - 1:TCH]),
                         reads=[b_SQ], writes=[b_hc[c]])
                    k.op("dve", lambda e: e.scalar_tensor_tensor(out=YT[:, c, :], in0=S2[:], scalar=0.25, in1=HH[:],
                                                                 op0=ALU.mult, op1=ALU.mult),
                         reads=[b_S2, b_SQ], writes=[b_YT[c]])

                run_pipeline(NRC, [lambda c: (stA(c), stB(c))] if DBG.get("var") != "skew" else [stA, stB])

                lnp = {}

                def p0(tl):
                    s, sa = tl % 2, tl % NACC
                    r0 = tok0 + tl * 128
                    k.dma("sp", lambda e: e.dma_start(out=xt[s][:], in_=xin[r0:r0 + 128, :]),
                          reads=[b_xin], writes=[b_xt[s]])
                    k.op("act", lambda e: e.activation(out=acc[sa][:], in_=xt[s][:], func=AF.Copy, scale=ALPHA),
                         reads=[b_xt[s]], writes=[b_acc[sa]])

                def p1(tl):
                    sa = tl % NACC
                    for h in range(2):
                        for c in range(NRC):
                            k.op("pe", lambda e: e.matmul(pO[h][:], lhsT=YT_full[:, c, tl * 128:(tl + 1) * 128],
                                                          rhs=wo_full[:, c, h * 512:(h + 1) * 512],
                                                          start=(c == 0), stop=(c == NRC - 1)),
                                 reads=[b_YT[c], b_wo], writes=[b_pO[h]])
                        k.op("dve", lambda e: e.tensor_tensor(out=acc[sa][:, h * 512:(h + 1) * 512],
                                                              in0=acc[sa][:, h * 512:(h + 1) * 512], in1=pO[h][:],
                                                              op=ALU.add),
                             reads=[b_pO[h], b_acc[sa]], writes=[b_acc[sa]])
                    lnp[tl] = layer_norm_parts(k, T + "ln", acc[sa], b_acc[sa], acc[sa], b_acc[sa], gam, bet, b_gb, sa)

                def p2(tl):
                    lnp[tl][0]()

                def p3(tl):
                    sa = tl % NACC
                    r0 = tok0 + tl * 128
                    lnp[tl][1]()
                    lnp[tl][2]()
                    k.dma("sp", lambda e: e.dma_start(out=xout[r0:r0 + 128, :], in_=acc[sa][:]),
                          reads=[b_acc[sa]], writes=[b_xout])

                run_pipeline(TCH // 128, [p0, p1, p2, p3])
        barrier(k)
    k.es = es_outer


def attn_phase(k, cm, T, xin, b_xin, xout, b_xout, w_qkv, sinks, w_o, att_bias, ln_g, ln_b):
    es_outer = k.es
    HALF = 1024
    with ExitStack() as es:
        k.es = es
        gam = k.sb(T + "gam", [128, D], F32)
        bet = k.sb(T + "bet", [128, D], F32)
        b_gb = k.buf()
        k.dma("sp", lambda e: e.dma_start(out=gam[:], in_=ln_g.partition_broadcast(128)), writes=[b_gb])
        k.dma("sp", lambda e: e.dma_start(out=bet[:], in_=ln_b.partition_broadcast(128)), writes=[b_gb])
        wq = k.sb(T + "wq", [128, 8, 1024], BF16)
        wk = k.sb(T + "wk", [128, 8, 256], BF16)
        wv = k.sb(T + "wv", [128, 8, 256], BF16)
        wo = k.sb(T + "wo", [128, 8, 1024], BF16)
        b_w = k.buf()
        k.dma("pool", lambda g: g.dma_start(out=wq[:], in_=w_qkv[:, 0:1024].rearrange("(c p) n -> p c n", p=128)),
              writes=[b_w])
        k.dma("pool", lambda g: g.dma_start(out=wk[:], in_=w_qkv[:, 1024:1280].rearrange("(c p) n -> p c n", p=128)),
              writes=[b_w])
        k.dma("pool", lambda g: g.dma_start(out=wv[:], in_=w_qkv[:, 1280:1536].rearrange("(c p) n -> p c n", p=128)),
              writes=[b_w])
        k.dma("pool", lambda g: g.dma_start(out=wo[:], in_=w_o.rearrange("(c p) n -> p c n", p=128)), writes=[b_w])
        sk = k.sb(T + "sk", [128, NH], F32)
        bt = k.sb(T + "bt", [128, NH, 256], F32)
        b_c = k.buf()
        k.dma("sp", lambda e: e.dma_start(out=sk[:], in_=sinks.partition_broadcast(128)), writes=[b_c])
        k.dma("sp", lambda e: e.dma_start(out=bt[:], in_=att_bias), writes=[b_c])
        nsk = k.sb(T + "nsk", [128, NH], F32)
        k.op("dve", lambda e: e.tensor_scalar(out=nsk[:], in0=sk[:], scalar1=-1.0, scalar2=None, op0=ALU.mult),
             reads=[b_c], writes=[b_c])

        xT = k.sb(T + "xT", [128, 8, HALF], BF16)
        b_xT = k.buf()
        QT = k.sb(T + "QT", [HD, NH, HALF], BF16)
        b_QT = k.buf()
        KT = k.sb(T + "KT", [HD, NKV, SEQ], BF16)
        b_KT = k.buf()
        V = k.sb(T + "V", [128, SEQ // 128, NKV * HD], BF16)
        b_V = k.buf()
        xt = [k.sb(f"{T}xt{i}", [128, D], F32) for i in range(2)]
        b_xt = [k.buf() for _ in range(2)]
        xtb = [k.sb(f"{T}xtb{i}", [128, D], BF16) for i in range(2)]
        b_xtb = [k.buf() for _ in range(2)]
        Sb = [k.sb(f"{T}Sb{i}", [128, 2, 256], F32) for i in range(3)]
        Pm = [k.sb(f"{T}Pm{i}", [128, 2, 256], BF16) for i in range(3)]
        PT = [k.sb(f"{T}PT{i}", [128, 2, 256], BF16) for i in range(3)]
        sm = [k.sb(f"{T}sm{i}", [128, 12], F32) for i in range(4)]
        b_Sb = [k.buf() for _ in range(3)]
        b_Pm = [k.buf() for _ in range(3)]
        b_PT = [k.buf() for _ in range(3)]
        b_sm = [[k.buf() for _ in range(6)] for _ in range(4)]
        Ot = [k.sb(f"{T}Ot{i}", [128, D], BF16) for i in range(2)]
        b_Ot = [k.buf() for _ in range(2)]
        OT = [k.sb(f"{T}OT{i}", [128, 8, 128], BF16) for i in range(2)]
        b_OT = [k.buf() for _ in range(2)]
        acc = [k.sb(f"{T}acc{i}", [128, D], F32) for i in range(2)]
        b_acc = [k.buf() for _ in range(2)]
        ot = [k.sb(f"{T}ot{i}", [128, D], F32) for i in range(2)]
        b_ot = [k.buf() for _ in range(2)]
        psTb = k.ps(T + "psTb", [128, D], BF16)
        b_psTb = k.buf()
        psS_bk = [k.ps(f"{T}psS{i}", [128, 512], F32) for i in range(2)]
        _bS = [k.buf() for _ in range(2)]
        psS = [psS_bk[i % 2][:, 0:256] for i in range(4)]
        b_psS = [_bS[i % 2] for i in range(4)]
        psPT_bk = [k.ps(f"{T}psPT{i}", [128, 1024], BF16) for i in range(2)]
        _bP = [k.buf() for _ in range(2)]
        psPT = [psPT_bk[i % 2][:, 0:256] for i in range(4)]
        b_psPT = [_bP[i % 2] for i in range(4)]
        psO_bk = [k.ps(f"{T}psO{i}", [128, 512], F32) for i in range(2)]
        _bO = [k.buf() for _ in range(2)]
        psO = [psO_bk[i % 2][:, 0:HD] for i in range(8)]
        b_psO = [_bO[i % 2] for i in range(8)]
        pE = k.ps(T + "pE", [128, 512], F32)
        b_pE = k.buf()
        pA = psS_bk
        b_pA = _bS
        npa = [0]

        def next_pa():
            npa[0] += 1
            return npa[0] % 2

        def qs(q):
            return slice(q * 512, (q + 1) * 512)

        hcnt = 0
        for sq in range(2):
            for hf in range(SEQ // HALF):
                tok0 = sq * SEQ + hf * HALF
                for tl in range(HALF // 128):
                    s = tl % 2
                    r0 = tok0 + tl * 128
                    k.dma("sp", lambda e: e.dma_start(out=xt[s][:], in_=xin[r0:r0 + 128, :]),
                          reads=[b_xin], writes=[b_xt[s]])
                    k.op("act", lambda e: e.activation(out=xtb[s][:], in_=xt[s][:], func=AF.Copy),
                         reads=[b_xt[s]], writes=[b_xtb[s]])
                    for c in range(8):
                        k.op("pe", lambda e: e.transpose(out=psTb[:, c * 128:(c + 1) * 128],
                                                         in_=xtb[s][:, c * 128:(c + 1) * 128], identity=cm.ident_b),
                             reads=[b_xtb[s], cm.b_cb], writes=[b_psTb])
                    k.op("dve", lambda e: e.tensor_copy(out=xT[:, :, tl * 128:(tl + 1) * 128],
                                                        in_=psTb[:].rearrange("p (c t) -> p c t", c=8)),
                         reads=[b_psTb], writes=[b_xT])
                for h in range(NH):
                    for q in range(HALF // 512):
                        pa = next_pa()
                        for kk in range(8):
                            k.op("pe", lambda e: e.matmul(pA[pa][0:HD, :], lhsT=wq[:, kk, h * HD:(h + 1) * HD],
                                                          rhs=xT[:, kk, qs(q)], start=(kk == 0), stop=(kk == 7)),
                                 reads=[b_w, b_xT], writes=[b_pA[pa]])
                        if (h + q) % 2 == 0:
                            k.op("act", lambda e: e.activation(out=QT[:, h, qs(q)], in_=pA[pa][0:HD, :], func=AF.Copy,
                                                               scale=HD ** -0.5),
                                 reads=[b_pA[pa]], writes=[b_QT])
                        else:
                            k.op("dve", lambda e: e.tensor_scalar(out=QT[:, h, qs(q)], in0=pA[pa][0:HD, :],
                                                                  scalar1=HD ** -0.5, scalar2=None, op0=ALU.mult),
                                 reads=[b_pA[pa]], writes=[b_QT])
                for kv in range(NKV):
                    for q in range(HALF // 512):
                        pa = next_pa()
                        for kk in range(8):
                            k.op("pe", lambda e: e.matmul(pA[pa][0:HD, :], lhsT=wk[:, kk, kv * HD:(kv + 1) * HD],
                                                          rhs=xT[:, kk, qs(q)], start=(kk == 0), stop=(kk == 7)),
                                 reads=[b_w, b_xT], writes=[b_pA[pa]])
                        c0 = hf * HALF + q * 512
                        k.op("act", lambda e: e.activation(out=KT[:, kv, c0:c0 + 512], in_=pA[pa][0:HD, :], func=AF.Copy),
                             reads=[b_pA[pa]], writes=[b_KT])
                for tl in range(HALF // 128):
                    pa = next_pa()
                    for kk in range(8):
                        k.op("pe", lambda e: e.matmul(pA[pa][:, 0:256], lhsT=xT[:, kk, tl * 128:(tl + 1) * 128],
                                                      rhs=wv[:, kk, :], start=(kk == 0), stop=(kk == 7)),
                             reads=[b_w, b_xT], writes=[b_pA[pa]])
                    k.op("dve", lambda e: e.tensor_copy(out=V[:, hf * 8 + tl, :], in_=pA[pa][:, 0:256]),
                         reads=[b_pA[pa]], writes=[b_V])
                NB_ = HALF // 128
                items = [(b, hp) for b in range(NB_) for hp in range(NH // 2)]

                def geom(b):
                    g = hf * NB_ + b
                    has_prev = g > 0
                    cs = slice(0, 256) if has_prev else slice(128, 256)
                    k0 = (g - 1) * 128 if has_prev else 0
                    return g, has_prev, cs, k0, (g + 1) * 128

                def st1(n):
                    b, hp = items[n]
                    g, has_prev, cs, k0, k1 = geom(b)
                    h0 = 2 * hp
                    kv = h0 // 4
                    os_ = b % 2
                    r0 = tok0 + b * 128
                    if hp == 0:
                        k.dma("sp", lambda e: e.dma_start(out=xt[os_][:], in_=xin[r0:r0 + 128, :]),
                              reads=[b_xin], writes=[b_xt[os_]])
                        k.op("act", lambda e: e.activation(out=acc[os_][:], in_=xt[os_][:], func=AF.Copy, scale=ALPHA),
                             reads=[b_xt[os_]], writes=[b_acc[os_]])
                    s2, s3, s4 = n % 2, n % 3, n % 4
                    pS = psS_bk[s2][:].rearrange("p (j c) -> p j c", j=2)
                    for j in range(2):
                        k.op("pe", lambda e: e.matmul(pS[:, j, cs], lhsT=QT[:, h0 + j, b * 128:(b + 1) * 128],
                                                      rhs=KT[:, kv, k0:k1], start=True, stop=True),
                             reads=[b_QT, b_KT], writes=[_bS[s2]])
                    k.op("dve", lambda e: e.tensor_tensor(out=Sb[s3][:, :, cs], in0=pS[:, :, cs], in1=bt[:, h0:h0 + 2, cs],
                                                          op=ALU.add),
                         reads=[_bS[s2], b_c], writes=[b_Sb[s3]])
                    k.op("dve", lambda e: e.tensor_reduce(out=sm[s4][:, 0:2], in_=Sb[s3][:, :, cs], axis=AX.X, op=ALU.max),
                         reads=[b_Sb[s3]], writes=[b_sm[s4][0]])
                    k.op("dve", lambda e: e.scalar_tensor_tensor(out=sm[s4][:, 2:4], in0=sm[s4][:, 0:2], scalar=-1.0,
                                                                 in1=nsk[:, h0:h0 + 2], op0=ALU.mult, op1=ALU.min),
                         reads=[b_sm[s4][0], b_c], writes=[b_sm[s4][1]])
                    for j in range(2):
                        k.op("act", lambda e: e.activation(out=Pm[s3][:, j, cs], in_=Sb[s3][:, j, cs], func=AF.Exp,
                                                           bias=sm[s4][:, 2 + j:3 + j], accum_out=sm[s4][:, 4 + j:5 + j]),
                             reads=[b_Sb[s3], b_sm[s4][1]], writes=[b_Pm[s3], b_sm[s4][2]])
                    k.op("dve", lambda e: e.tensor_tensor(out=sm[s4][:, 6:8], in0=sk[:, h0:h0 + 2], in1=sm[s4][:, 2:4],
                                                          op=ALU.add),
                         reads=[b_sm[s4][1], b_c], writes=[b_sm[s4][3]])
                    k.op("act", lambda e: e.activation(out=sm[s4][:, 6:8], in_=sm[s4][:, 6:8], func=AF.Exp),
                         reads=[b_sm[s4][3]], writes=[b_sm[s4][3]])

                def st2(n):
                    b, hp = items[n]
                    g, has_prev, cs, k0, k1 = geom(b)
                    s2, s3, s4 = n % 2, n % 3, n % 4
                    k.op("dve", lambda e: e.tensor_tensor(out=sm[s4][:, 8:10], in0=sm[s4][:, 4:6], in1=sm[s4][:, 6:8],
                                                          op=ALU.add),
                         reads=[b_sm[s4][2], b_sm[s4][3]], writes=[b_sm[s4][4]])
                    k.op("dve", lambda e: e.reciprocal(out=sm[s4][:, 10:12], in_=sm[s4][:, 8:10]),
                         reads=[b_sm[s4][4]], writes=[b_sm[s4][5]])
                    pP = psPT_bk[s2][:, 0:512].rearrange("p (j c) -> p j c", j=2)
                    for j in range(2):
                        if has_prev:
                            k.op("pe", lambda e: e.transpose(out=pP[:, j, 0:128], in_=Pm[s3][:, j, 0:128],
                                                             identity=cm.ident_b),
                                 reads=[b_Pm[s3], cm.b_cb], writes=[_bP[s2]])
                        k.op("pe", lambda e: e.transpose(out=pP[:, j, 128:256], in_=Pm[s3][:, j, 128:256],
                                                         identity=cm.ident_b),
                             reads=[b_Pm[s3], cm.b_cb], writes=[_bP[s2]])
                    if n % 2 == 0:
                        k.op("act", lambda e: e.activation(out=PT[s3][:, :, cs], in_=pP[:, :, cs], func=AF.Copy),
                             reads=[_bP[s2]], writes=[b_PT[s3]])
                    else:
                        k.op("dve", lambda e: e.tensor_copy(out=PT[s3][:, :, cs], in_=pP[:, :, cs]),
                             reads=[_bP[s2]], writes=[b_PT[s3]])

                def st3(n):
                    b, hp = items[n]
                    g, has_prev, cs, k0, k1 = geom(b)
                    h0 = 2 * hp
                    kv = h0 // 4
                    os_ = b % 2
                    s2, s3, s4 = n % 2, n % 3, n % 4
                    pO_ = psO_bk[s2][:, 0:2 * HD].rearrange("p (j d) -> p j d", j=2)
                    for j in range(2):
                        if has_prev:
                            k.op("pe", lambda e: e.matmul(pO_[:, j, :], lhsT=PT[s3][:, j, 0:128],
                                                          rhs=V[:, g - 1, kv * HD:(kv + 1) * HD], start=True, stop=False),
                                 reads=[b_PT[s3], b_V], writes=[_bO[s2]])
                        k.op("pe", lambda e: e.matmul(pO_[:, j, :], lhsT=PT[s3][:, j, 128:256],
                                                      rhs=V[:, g, kv * HD:(kv + 1) * HD], start=(not has_prev), stop=True),
                             reads=[b_PT[s3], b_V], writes=[_bO[s2]])
                    k.op("dve", lambda e: e.tensor_tensor(
                        out=Ot[os_][:, h0 * HD:(h0 + 2) * HD].rearrange("p (j d) -> p j d", j=2), in0=pO_,
                        in1=sm[s4][:, 10:12].unsqueeze(2).to_broadcast([128, 2, HD]), op=ALU.mult),
                        reads=[_bO[s2], b_sm[s4][5]], writes=[b_Ot[os_]])
                    if hp == NH // 2 - 1:
                        epilogue(b)

                dfr = Deferred()

                def epilogue(b):
                    os_ = b % 2
                    r0 = tok0 + b * 128

                    def e1():
                        for c in range(8):
                            k.op("pe", lambda e: e.transpose(out=psTb[:, c * 128:(c + 1) * 128],
                                                             in_=Ot[os_][:, c * 128:(c + 1) * 128], identity=cm.ident_b),
                                 reads=[b_Ot[os_], cm.b_cb], writes=[b_psTb])
                        k.op("act", lambda e: e.activation(out=OT[os_][:], in_=psTb[:].rearrange("p (c t) -> p c t", c=8),
                                                           func=AF.Copy),
                             reads=[b_psTb], writes=[b_OT[os_]])

                    def e2(hh):
                        def f():
                            for c in range(8):
                                k.op("pe", lambda e: e.matmul(pE[:], lhsT=OT[os_][:, c, :],
                                                              rhs=wo[:, c, hh * 512:(hh + 1) * 512],
                                                              start=(c == 0), stop=(c == 7)),
                                     reads=[b_OT[os_], b_w], writes=[b_pE])
                        return f

                    def e3(hh):
                        def f():
                            k.op("dve", lambda e: e.tensor_tensor(out=acc[os_][:, hh * 512:(hh + 1) * 512],
                                                                  in0=acc[os_][:, hh * 512:(hh + 1) * 512], in1=pE[:],
                                                                  op=ALU.add),
                                 reads=[b_pE, b_acc[os_]], writes=[b_acc[os_]])
                        return f

                    pa_, pb_, pc_ = layer_norm_parts(k, T + "ln", acc[os_], b_acc[os_], ot[os_], b_ot[os_], gam, bet,
                                                     b_gb, os_)

                    def store():
                        k.dma("sp", lambda e: e.dma_start(out=xout[r0:r0 + 128, :], in_=ot[os_][:]),
                              reads=[b_ot[os_]], writes=[b_xout])

                    e1()
                    dfr.add(1, e2(0))
                    dfr.add(2, e3(0))
                    dfr.add(2, e2(1))
                    dfr.add(3, e3(1))
                    dfr.add(3, pa_)
                    dfr.add(4, pb_)
                    dfr.add(5, pc_)
                    dfr.add(6, store)

                run_pipeline(len(items), [st1, st2, st3], dfr)
        barrier(k)
    k.es = es_outer
```

```python
from contextlib import ExitStack

import numpy as np
import concourse.bass as bass
import concourse.mybir as mybir
from concourse.bass_utils import run_bass_kernel_spmd

F32 = mybir.dt.float32
BF16 = mybir.dt.bfloat16
I32 = mybir.dt.int32
AF = mybir.ActivationFunctionType
ALU = mybir.AluOpType
AX = mybir.AxisListType

NCORES = 8
D = 1024
SEQ = 2048
TOK = 4096
NT = TOK // 128
NE = 32
DE = 512
CAP = 384
NSLOT = NE * CAP
ALPHA = float((2 * 2) ** 0.25)
LN_EPS = 1e-5
D_RNN = 1280
RC = 80
NRC = D_RNN // RC
NH = 16
NKV = 4
HD = 64


class Buf:
    __slots__ = ("name", "writers", "readers", "nowaw")

    def __init__(self, name, nowaw=False):
        self.name = name
        self.writers = {}
        self.readers = {}
        self.nowaw = nowaw


def _merge(dst, src):
    for k, (s, v) in src.items():
        if k not in dst or dst[k][1] < v:
            dst[k] = (s, v)


class K:
    def __init__(self, nc, es):
        self.nc = nc
        self.es = es
        self.es_root = es
        self.engs = {"pe": nc.tensor, "dve": nc.vector, "act": nc.scalar, "pool": nc.gpsimd, "sp": nc.sync}
        self.sem = {}
        self.cnt = {}
        self.waited = {n: {} for n in self.engs}
        for n in self.engs:
            self.sem[n] = es.enter_context(nc.semaphore("s_" + n))
            self.cnt[n] = 0
        self.ring = {}
        for q, n in (("sp", 10), ("pool", 10), ("act", 4)):
            self.ring[q] = [[es.enter_context(nc.semaphore(f"d_{q}{i}")), 0] for i in range(n)]
        self.ring_pos = {q: 0 for q in self.ring}
        self.nbuf = 0

    def sb(self, name, shape, dt):
        return self.es.enter_context(self.nc.sbuf_tensor(name, list(shape), dt))

    def ps(self, name, shape, dt):
        return self.es.enter_context(self.nc.psum_tensor(name, list(shape), dt))

    def dram(self, name, shape, dt):
        return self.nc.dram_tensor(name, list(shape), dt, kind="Internal").ap()

    def buf(self, name=None, nowaw=False):
        self.nbuf += 1
        return Buf(name or f"b{self.nbuf}", nowaw)

    def _wait(self, engname, deps):
        e = self.engs[engname]
        w = self.waited[engname]
        for k, (s, v) in deps.items():
            if engname == "pe" and k == id(self.sem["pe"]):
                continue
            if w.get(k, 0) >= v:
                continue
            e.wait_ge(s, v)
            w[k] = v

    def _deps(self, reads, writes):
        deps = {}
        for b in reads:
            _merge(deps, b.writers)
        for b in writes:
            if not b.nowaw:
                _merge(deps, b.writers)
            _merge(deps, b.readers)
        return deps

    def _record(self, tok, reads, writes):
        k, s, v = tok
        for b in reads:
            if k not in b.readers or b.readers[k][1] < v:
                b.readers[k] = (s, v)
        for b in writes:
            if b.readers:
                b.writers = {}
                b.readers = {}
            b.writers[k] = (s, v)

    def op(self, engname, fn, reads=(), writes=()):
        deps = self._deps(reads, writes)
        self._wait(engname, deps)
        inst = fn(self.engs[engname])
        s = self.sem[engname]
        self.cnt[engname] += 1
        inst.then_inc(s, 1)
        tok = (id(s), s, self.cnt[engname])
        self._record(tok, reads, writes)
        return tok

    def dma(self, q, fn, reads=(), writes=()):
        deps = self._deps(reads, writes)
        ring = self.ring[q]
        pos = self.ring_pos[q]
        self.ring_pos[q] = (pos + 1) % len(ring)
        ent = ring[pos]
        s = ent[0]
        if ent[1] > 0:
            deps[id(s)] = (s, ent[1])
        self._wait(q, deps)
        inst = fn(self.engs[q])
        ent[1] += 16
        inst.then_inc(s, 16)
        tok = (id(s), s, ent[1])
        self._record(tok, reads, writes)
        return tok

    def finish(self, bufs):
        deps = {}
        for b in bufs:
            _merge(deps, b.writers)
        self._wait("sp", deps)


C_IDENT = 0
C_UPPER = 128
C_ONES = 256
C_EBASE = 384
NCONST = 416


def make_consts():
    c = np.zeros((128, NCONST), np.float32)
    c[:, C_IDENT:C_IDENT + 128] = np.eye(128, dtype=np.float32)
    c[:, C_UPPER:C_UPPER + 128] = np.triu(np.ones((128, 128), np.float32), 1)
    c[:, C_ONES:C_ONES + 128] = 1.0
    c[:, C_EBASE:C_EBASE + NE] = (np.arange(NE, dtype=np.float32) * CAP)[None, :]
    return c


class Common:
    def __init__(self, k, consts_ap):
        self.k = k
        nc = k.nc
        self.cf = k.sb("cf", [128, NCONST], F32)
        self.b_cf = k.buf("cf")
        k.dma("sp", lambda e: e.dma_start(out=self.cf[:], in_=consts_ap), writes=[self.b_cf])
        self.cb = k.sb("cb", [128, 384], BF16)
        self.b_cb = k.buf("cb")
        k.op("dve", lambda e: e.tensor_copy(out=self.cb[:], in_=self.cf[:, 0:384]),
             reads=[self.b_cf], writes=[self.b_cb])
        self.ident_f = self.cf[:, C_IDENT:C_IDENT + 128]
        self.ident_b = self.cb[:, C_IDENT:C_IDENT + 128]
        self.upper_b = self.cb[:, C_UPPER:C_UPPER + 128]
        self.ones_b = self.cb[:, C_ONES:C_ONES + 128]
        self.ebase = self.cf[:, C_EBASE:C_EBASE + NE]
        self.bound_reg = nc.gpsimd.to_reg(NSLOT - 1)
        k.mhalf = k.sb("ln_mhalf", [128, 1], F32)
        k.op("pool", lambda q: q.memset(k.mhalf[:], -0.5))
        barrier(k)


def moe_phase(k, cm, tag, xin, b_xin, xout, b_xout, w_rg, b_rg, w_re, b_re, w1, w3, w2, ln_g, ln_b,
              xbuf, ybuf):
    nc = k.nc
    es_outer = k.es
    with ExitStack() as es:
        k.es = es
        T = tag
        wr = k.sb(T + "wr", [128, 8, 36], F32)
        b_wr = k.buf()
        k.dma("sp", lambda e: e.dma_start(out=wr[:, :, 0:4], in_=w_rg.rearrange("(c p) n -> p c n", p=128)),
              writes=[b_wr])
        k.dma("sp", lambda e: e.dma_start(out=wr[:, :, 4:36], in_=w_re.rearrange("(c p) n -> p c n", p=128)),
              writes=[b_wr])
        rbias = k.sb(T + "rbias", [128, 36], F32)
        b_rbias = k.buf()
        k.dma("sp", lambda e: e.dma_start(out=rbias[:, 0:4], in_=b_rg.partition_broadcast(128)), writes=[b_rbias])
        k.dma("sp", lambda e: e.dma_start(out=rbias[:, 4:36], in_=b_re.partition_broadcast(128)), writes=[b_rbias])
        gam = k.sb(T + "gam", [128, D], F32)
        bet = k.sb(T + "bet", [128, D], F32)
        b_gb = k.buf()
        k.dma("sp", lambda e: e.dma_start(out=gam[:], in_=ln_g.partition_broadcast(128)), writes=[b_gb])
        k.dma("sp", lambda e: e.dma_start(out=bet[:], in_=ln_b.partition_broadcast(128)), writes=[b_gb])
        idx_all = k.sb(T + "idx", [128, NT, 2], I32)
        gate_all = k.sb(T + "gate", [128, NT, 2], F32)
        b_idx = k.buf()
        b_gate = k.buf()

        NWB = 4
        w1t = [k.sb(f"{T}w1_{i}", [128, 8, DE], BF16) for i in range(NWB)]
        w3t = [k.sb(f"{T}w3_{i}", [128, 8, DE], BF16) for i in range(NWB)]
        w2t = [k.sb(f"{T}w2_{i}", [128, 4, D], BF16) for i in range(NWB)]
        b_w13 = [k.buf(nowaw=True) for _ in range(NWB)]
        b_w2 = [k.buf() for _ in range(NWB)]

        def load_weights(e):
            s = e % NWB
            k.dma("pool", lambda g: g.dma_start(out=w1t[s][:], in_=w1[e].rearrange("(c p) n -> p c n", p=128)),
                  writes=[b_w13[s]])
            k.dma("pool", lambda g: g.dma_start(out=w3t[s][:], in_=w3[e].rearrange("(c p) n -> p c n", p=128)),
                  writes=[b_w13[s]])
            k.dma("pool", lambda g: g.dma_start(out=w2t[s][:], in_=w2[e].rearrange("(c p) n -> p c n", p=128)),
                  writes=[b_w2[s]])

        for e in range(NWB):
            load_weights(e)

        with ExitStack() as es2:
            k.es = es2
            lg_all = k.sb(T + "lg_all", [128, NT, 36], F32)
            b_lg = [k.buf() for _ in range(NT)]
            xt = [k.sb(f"{T}xt{i}", [128, D], F32) for i in range(3)]
            b_xt = [k.buf() for _ in range(3)]
            xT = [k.sb(f"{T}xT{i}", [128, 8, 128], F32) for i in range(3)]
            b_xTa = [k.buf() for _ in range(3)]
            b_xTb = [k.buf() for _ in range(3)]
            psT = [k.ps(f"{T}psT{i}", [128, 8, 128], F32) for i in range(2)]
            b_psT = [k.buf() for _ in range(2)]
            psL = [k.ps(f"{T}psL{i}", [128, 512], F32) for i in range(2)]
            b_psL = [k.buf() for _ in range(2)]

            def lg0(i):
                s = i % 3
                k.dma("sp", lambda e: e.dma_start(out=xt[s][:], in_=xin[i * 128:(i + 1) * 128, :]),
                      reads=[b_xin], writes=[b_xt[s]])

            def lg1(i):
                s, p = i % 3, i % 2
                for c in range(8):
                    k.op("pe", lambda e: e.transpose(out=psT[p][:, c, :], in_=xt[s][:, c * 128:(c + 1) * 128],
                                                     identity=cm.ident_f),
                         reads=[b_xt[s], cm.b_cf], writes=[b_psT[p]])

            def lg2(i):
                s, p = i % 3, i % 2
                k.op("act", lambda e: e.activation(out=xT[s][:, 0:4, :], in_=psT[p][:, 0:4, :], func=AF.Copy),
                     reads=[b_psT[p]], writes=[b_xTa[s]])
                k.op("dve", lambda e: e.tensor_copy(out=xT[s][:, 4:8, :], in_=psT[p][:, 4:8, :]),
                     reads=[b_psT[p]], writes=[b_xTb[s]])

            def lg3(i):
                s, p = i % 3, i % 2
                for c in range(8):
                    k.op("pe", lambda e: e.matmul(psL[p][:, 0:36], lhsT=xT[s][:, c, :], rhs=wr[:, c, :],
                                                  start=(c == 0), stop=(c == 7)),
                         reads=[b_xTa[s], b_xTb[s], b_wr], writes=[b_psL[p]])

            def lg4(i):
                p = i % 2
                k.op("dve", lambda e: e.tensor_tensor(out=lg_all[:, i, :], in0=psL[p][:, 0:36], in1=rbias[:],
                                                      op=ALU.add),
                     reads=[b_psL[p], b_rbias], writes=[b_lg[i]])

            run_pipeline(NT, [lg0, lg1, lg2, lg3, lg4])

            def st(name, shape, dt=F32):
                return k.sb(T + name, shape, dt)

            lgg = lg_all[:, :, 0:4]
            lge = lg_all[:, :, 4:36]
            gmax = st("gmax", [128, NT])
            ohg = st("ohg", [128, NT, 4])
            egs = st("egs", [128, NT, 4])
            gsum = st("gsum", [128, NT])
            ggate = st("ggate", [128, NT])
            pen = st("pen", [128, NT, 4])
            me = st("me", [128, NT, 32])
            me2 = me
            v1 = st("v1", [128, NT])
            v2 = st("v2", [128, NT])
            sel1 = st("sel1", [128, NT, 32])
            sel2 = st("sel2", [128, NT, 32])
            dd = st("dd", [128, NT])
            p1 = st("p1", [128, NT])
            g1 = st("g1", [128, NT])
            g2 = st("g2", [128, NT])
            A = st("A", [128, NT, 32], BF16)
            dest = st("dest", [128, NT, 32])
            ov = st("ov", [128, NT, 32])
            tmp = st("tmp", [128, NT, 32])
            idxf = st("idxf", [128, NT, 2])
            ovs = st("ovs", [128, NT, 2])
            b_r = k.buf()

            def dv(fn, extra_r=()):
                k.op("dve", fn, reads=[b_r, *extra_r], writes=[b_r])

            def bc(ap, n):
                return ap.unsqueeze(2).to_broadcast([128, NT, n])

            dv(lambda e: e.tensor_reduce(out=gmax[:], in_=lgg, axis=AX.X, op=ALU.max), extra_r=b_lg)
            dv(lambda e: e.tensor_tensor(out=ohg[:], in0=lgg, in1=bc(gmax[:], 4), op=ALU.is_ge))
            dv(lambda e: e.tensor_tensor(out=egs[:], in0=lgg, in1=bc(gmax[:], 4), op=ALU.subtract))
            k.op("act", lambda e: e.activation(out=egs[:], in_=egs[:], func=AF.Exp), reads=[b_r], writes=[b_r])
            dv(lambda e: e.tensor_reduce(out=gsum[:], in_=egs[:], axis=AX.X, op=ALU.add))
            dv(lambda e: e.reciprocal(out=ggate[:], in_=gsum[:]))
            dv(lambda e: e.tensor_scalar(out=pen[:], in0=ohg[:], scalar1=1e30, scalar2=-1e30,
                                         op0=ALU.mult, op1=ALU.add))
            me4 = me[:].rearrange("p t (g j) -> p t g j", g=4)
            lge4 = lge.rearrange("p t (g j) -> p t g j", g=4)
            dv(lambda e: e.tensor_tensor(out=me4, in0=lge4, in1=ohg[:].unsqueeze(3).to_broadcast([128, NT, 4, 8]),
                                         op=ALU.mult))
            dv(lambda e: e.tensor_tensor(out=me4, in0=me4, in1=pen[:].unsqueeze(3).to_broadcast([128, NT, 4, 8]),
                                         op=ALU.add))
            dv(lambda e: e.tensor_reduce(out=v1[:], in_=me[:], axis=AX.X, op=ALU.max))
            dv(lambda e: e.tensor_tensor(out=sel1[:], in0=me[:], in1=bc(v1[:], 32), op=ALU.is_equal))
            dv(lambda e: e.scalar_tensor_tensor(out=me2[:], in0=sel1[:], scalar=-2e30, in1=me[:],
                                                op0=ALU.mult, op1=ALU.add))
            dv(lambda e: e.tensor_reduce(out=v2[:], in_=me2[:], axis=AX.X, op=ALU.max))
            dv(lambda e: e.tensor_tensor(out=sel2[:], in0=me2[:], in1=bc(v2[:], 32), op=ALU.is_equal))
            dv(lambda e: e.tensor_tensor(out=dd[:], in0=v2[:], in1=v1[:], op=ALU.subtract))
            k.op("act", lambda e: e.activation(out=dd[:], in_=dd[:], func=AF.Exp), reads=[b_r], writes=[b_r])
            dv(lambda e: e.tensor_scalar(out=dd[:], in0=dd[:], scalar1=1.0, scalar2=None, op0=ALU.add))
            dv(lambda e: e.reciprocal(out=p1[:], in_=dd[:]))
            dv(lambda e: e.tensor_tensor(out=g1[:], in0=p1[:], in1=ggate[:], op=ALU.mult))
            dv(lambda e: e.tensor_tensor(out=g2[:], in0=ggate[:], in1=g1[:], op=ALU.subtract))
            dv(lambda e: e.tensor_tensor(out=A[:], in0=sel1[:], in1=sel2[:], op=ALU.add))

            psP = k.ps(T + "psP", [128, NT, 32], F32)
            b_psP = k.buf()
            for i in range(NT):
                k.op("pe", lambda e: e.matmul(psP[:, i, :], lhsT=cm.upper_b, rhs=A[:, i, :], start=True,
                                              stop=(i == 0)),
                     reads=[b_r, cm.b_cb], writes=[b_psP])
                for j in range(i):
                    k.op("pe", lambda e: e.matmul(psP[:, i, :], lhsT=cm.ones_b, rhs=A[:, j, :], start=False,
                                                  stop=(j == i - 1)),
                         reads=[b_r, cm.b_cb], writes=[b_psP])

            dv(lambda e: e.tensor_tensor(out=dest[:], in0=psP[:], in1=cm.ebase.unsqueeze(1).to_broadcast([128, NT, 32]),
                                         op=ALU.add), extra_r=[b_psP, cm.b_cf])
            dv(lambda e: e.tensor_scalar(out=ov[:], in0=psP[:], scalar1=float(CAP), scalar2=None, op0=ALU.is_ge),
               extra_r=[b_psP])
            dv(lambda e: e.scalar_tensor_tensor(out=dest[:], in0=ov[:], scalar=1.0e6, in1=dest[:],
                                                op0=ALU.mult, op1=ALU.add))
            for j, sel in enumerate((sel1, sel2)):
                dv(lambda e: e.tensor_tensor(out=tmp[:], in0=sel[:], in1=dest[:], op=ALU.mult))
                dv(lambda e: e.tensor_reduce(out=idxf[:, :, j], in_=tmp[:], axis=AX.X, op=ALU.add))
                dv(lambda e: e.tensor_tensor(out=tmp[:], in0=sel[:], in1=ov[:], op=ALU.mult))
                dv(lambda e: e.tensor_reduce(out=ovs[:, :, j], in_=tmp[:], axis=AX.X, op=ALU.add))
            k.op("dve", lambda e: e.tensor_copy(out=idx_all[:], in_=idxf[:]), reads=[b_r], writes=[b_idx])
            dv(lambda e: e.tensor_scalar(out=ovs[:], in0=ovs[:], scalar1=-1.0, scalar2=1.0, op0=ALU.mult, op1=ALU.add))
            k.op("dve", lambda e: e.tensor_tensor(out=gate_all[:, :, 0], in0=g1[:], in1=ovs[:, :, 0], op=ALU.mult),
                 reads=[b_r], writes=[b_gate])
            k.op("dve", lambda e: e.tensor_tensor(out=gate_all[:, :, 1], in0=g2[:], in1=ovs[:, :, 1], op=ALU.mult),
                 reads=[b_r], writes=[b_gate])

            b_xbuf = k.buf(nowaw=True)
            xs = [xt[0][:], xt[1][:], xt[2][:], xT[0][:].rearrange("p c t -> p (c t)")]
            b_xs = [b_xt[0], b_xt[1], b_xt[2], k.buf()]
            barrier(k)
            for i in range(NT):
                s = i % 4
                k.dma("sp", lambda e: e.dma_start(out=xs[s], in_=xin[i * 128:(i + 1) * 128, :]),
                      reads=[b_xin], writes=[b_xs[s]])
                for j in range(2):
                    k.dma("pool", lambda g: g.indirect_dma_start(
                        out=xbuf, out_offset=bass.IndirectOffsetOnAxis(ap=idx_all[:, i, j:j + 1], axis=0),
                        in_=xs[s], in_offset=None, bounds_check=cm.bound_reg, oob_is_err=False),
                        reads=[b_xs[s], b_idx], writes=[b_xbuf])
            barrier(k)
        k.es = es

        b_ybuf = k.buf(nowaw=True)
        with ExitStack() as es2:
            k.es = es2
            NB = CAP // 128
            NXS = 3
            xe = [k.sb(f"{T}xe{i}", [128, NB, D], BF16) for i in range(NXS)]
            b_xe = [k.buf() for _ in range(NXS)]
            xeT = [k.sb(f"{T}xeT{i}", [128, 8, CAP], BF16) for i in range(NXS)]
            b_xeT = [[k.buf() for _ in range(NB)] for _ in range(NXS)]
            hd = [k.sb(f"{T}hd{i}", [128, 4, CAP], BF16) for i in range(2)]
            b_hd = [[k.buf() for _ in range(4)] for _ in range(2)]
            sil = [k.sb(f"{T}sil{i}", [128, CAP], F32) for i in range(2)]
            b_sil = [k.buf() for _ in range(2)]
            yt = [k.sb(f"{T}yt{i}", [128, NB, D], BF16) for i in range(2)]
            b_yt = [[[k.buf() for _ in range(2)] for _ in range(NB)] for _ in range(2)]
            psX = [k.ps(f"{T}psX{i}", [128, D], BF16) for i in range(2)]
            b_psX = [k.buf() for _ in range(2)]
            psH1 = [k.ps(f"{T}psH1_{i}", [128, 512], F32) for i in range(2)]
            psH3 = [k.ps(f"{T}psH3_{i}", [128, 512], F32) for i in range(2)]
            b_psH1 = [k.buf() for _ in range(2)]
            b_psH3 = [k.buf() for _ in range(2)]
            psY = [k.ps(f"{T}psY{i}", [128, 512], F32) for i in range(2)]
            b_psY = [k.buf() for _ in range(2)]
            cntX = [0]

            def stage_load(e):
                s = e % NXS
                k.dma("sp", lambda q: q.dma_start(
                    out=xe[s][:], in_=xbuf[e * CAP:(e + 1) * CAP, :].rearrange("(b p) d -> p b d", p=128)),
                    reads=[b_xbuf], writes=[b_xe[s]])

            def grp_T(e, blk):
                def f():
                    s = e % NXS
                    px = cntX[0] % 2
                    cntX[0] += 1
                    for c in range(8):
                        k.op("pe", lambda q: q.transpose(out=psX[px][:, c * 128:(c + 1) * 128],
                                                         in_=xe[s][:, blk, c * 128:(c + 1) * 128],
                                                         identity=cm.ident_b),
                             reads=[b_xe[s], cm.b_cb], writes=[b_psX[px]])
                    src = psX[px][:].rearrange("p (c t) -> p c t", c=8)
                    if px == 0:
                        k.op("dve", lambda q: q.tensor_copy(out=xeT[s][:, :, blk * 128:(blk + 1) * 128], in_=src),
                             reads=[b_psX[px]], writes=[b_xeT[s][blk]])
                    else:
                        k.op("act", lambda q: q.activation(out=xeT[s][:, :, blk * 128:(blk + 1) * 128], in_=src,
                                                           func=AF.Copy),
                             reads=[b_psX[px]], writes=[b_xeT[s][blk]])
                return f

            def grp_H(e, m):
                def f():
                    s = e % NXS
                    hs = e % 2
                    ws = e % NWB
                    ph = m % 2
                    for c in range(8):
                        k.op("pe", lambda q: q.matmul(psH1[ph][:, 0:CAP], lhsT=w1t[ws][:, c, m * 128:(m + 1) * 128],
                                                      rhs=xeT[s][:, c, :], start=(c == 0), stop=(c == 7)),
                             reads=[*b_xeT[s], b_w13[ws]], writes=[b_psH1[ph]])
                    k.op("act", lambda q: q.activation(out=sil[ph][:], in_=psH1[ph][:, 0:CAP], func=AF.Silu),
                         reads=[b_psH1[ph]], writes=[b_sil[ph]])
                    for c in range(8):
                        k.op("pe", lambda q: q.matmul(psH3[ph][:, 0:CAP], lhsT=w3t[ws][:, c, m * 128:(m + 1) * 128],
                                                      rhs=xeT[s][:, c, :], start=(c == 0), stop=(c == 7)),
                             reads=[*b_xeT[s], b_w13[ws]], writes=[b_psH3[ph]])
                    k.op("dve", lambda q: q.tensor_tensor(out=hd[hs][:, m, :], in0=sil[ph][:], in1=psH3[ph][:, 0:CAP],
                                                          op=ALU.mult),
                         reads=[b_sil[ph], b_psH3[ph]], writes=[b_hd[hs][m]])
                return f

            def grp_Y(e, blk, half):
                def f():
                    hs = e % 2
                    ws = e % NWB
                    py = (blk * 2 + half) % 2
                    for c in range(4):
                        k.op("pe", lambda q: q.matmul(psY[py][:], lhsT=hd[hs][:, c, blk * 128:(blk + 1) * 128],
                                                      rhs=w2t[ws][:, c, half * 512:(half + 1) * 512],
                                                      start=(c == 0), stop=(c == 3)),
                             reads=[b_hd[hs][c], b_w2[ws]], writes=[b_psY[py]])
                    if half == 0:
                        k.op("act", lambda q: q.activation(out=yt[hs][:, blk, 0:512], in_=psY[py][:], func=AF.Copy),
                             reads=[b_psY[py]], writes=[b_yt[hs][blk][0]])
                    else:
                        k.op("dve", lambda q: q.tensor_copy(out=yt[hs][:, blk, 512:1024], in_=psY[py][:]),
                             reads=[b_psY[py]], writes=[b_yt[hs][blk][1]])
                return f

            def store_Y(e):
                hs = e % 2
                k.dma("sp", lambda q: q.dma_start(
                    out=ybuf[e * CAP:(e + 1) * CAP, :].rearrange("(b p) d -> p b d", p=128), in_=yt[hs][:]),
                    reads=[bb for blk in b_yt[hs] for bb in blk], writes=[b_ybuf])

            for e0 in range(min(NXS, NE)):
                stage_load(e0)
            for blk in range(NB):
                grp_T(0, blk)()
            for blk in range(NB):
                grp_T(1, blk)()
            for m in range(4):
                grp_H(0, m)()
            for e in range(NE):
                Tg = [grp_T(e + 2, blk) for blk in range(NB)] if e + 2 < NE else []
                Hg = [grp_H(e + 1, m) for m in range(4)] if e + 1 < NE else []
                Yg = [grp_Y(e, blk, half) for blk in range(NB) for half in range(2)]
                order = []
                ti = hi = 0
                for yi, yg in enumerate(Yg):
                    order.append(yg)
                    if yi % 2 == 0 and hi < len(Hg):
                        order.append(Hg[hi]); hi += 1
                    if yi % 2 == 1 and ti < len(Tg):
                        order.append(Tg[ti]); ti += 1
                order += Hg[hi:] + Tg[ti:]
                for g_ in order:
                    g_()
                store_Y(e)
                if e + NWB < NE:
                    load_weights(e + NWB)
                if e + NXS < NE:
                    stage_load(e + NXS)
            barrier(k)
        k.es = es

        with ExitStack() as es2:
            k.es = es2
            NY, NA = 3, 6
            y1 = [k.sb(f"{T}y1_{i}", [128, D], BF16) for i in range(NY)]
            y2 = [k.sb(f"{T}y2_{i}", [128, D], BF16) for i in range(NY)]
            b_y1 = [k.buf() for _ in range(NY)]
            b_y2 = [k.buf() for _ in range(NY)]
            xr = [k.sb(f"{T}xr_{i}", [128, D], F32) for i in range(NY)]
            b_xr = [k.buf() for _ in range(NY)]
            acc = [k.sb(f"{T}acc_{i}", [128, D], F32) for i in range(NA)]
            b_acc = [k.buf() for _ in range(NA)]
            ot = [k.sb(f"{T}ot_{i}", [128, D], F32) for i in range(3)]
            b_ot = [k.buf() for _ in range(3)]
            for i in range(NY):
                k.op("pool", lambda q: q.memset(y1[i][:], 0.0), writes=[b_y1[i]])
                k.op("pool", lambda q: q.memset(y2[i][:], 0.0), writes=[b_y2[i]])
            lnp = {}

            def cA(i):
                s, sa = i % NY, i % NA
                k.dma("sp", lambda q: q.dma_start(out=xr[s][:], in_=xin[i * 128:(i + 1) * 128, :]),
                      reads=[b_xin], writes=[b_xr[s]])
                k.dma("pool", lambda g: g.indirect_dma_start(
                    out=y1[s][:], out_offset=None, in_=ybuf,
                    in_offset=bass.IndirectOffsetOnAxis(ap=idx_all[:, i, 0:1], axis=0),
                    bounds_check=cm.bound_reg, oob_is_err=False), reads=[b_ybuf, b_idx], writes=[b_y1[s]])
                k.dma("pool", lambda g: g.indirect_dma_start(
                    out=y2[s][:], out_offset=None, in_=ybuf,
                    in_offset=bass.IndirectOffsetOnAxis(ap=idx_all[:, i, 1:2], axis=0),
                    bounds_check=cm.bound_reg, oob_is_err=False), reads=[b_ybuf, b_idx], writes=[b_y2[s]])

            def cB(i):
                s, sa = i % NY, i % NA
                k.op("act", lambda q: q.activation(out=acc[sa][:], in_=xr[s][:], func=AF.Copy, scale=ALPHA),
                     reads=[b_xr[s]], writes=[b_acc[sa]])
                k.op("dve", lambda q: q.scalar_tensor_tensor(out=acc[sa][:], in0=y1[s][:], scalar=gate_all[:, i, 0:1],
                                                             in1=acc[sa][:], op0=ALU.mult, op1=ALU.add),
                     reads=[b_y1[s], b_gate, b_acc[sa]], writes=[b_acc[sa]])
                k.op("dve", lambda q: q.scalar_tensor_tensor(out=acc[sa][:], in0=y2[s][:], scalar=gate_all[:, i, 1:2],
                                                             in1=acc[sa][:], op0=ALU.mult, op1=ALU.add),
                     reads=[b_y2[s], b_gate, b_acc[sa]], writes=[b_acc[sa]])
                lnp[i] = layer_norm_parts(k, T + "ln", acc[sa], b_acc[sa], ot[i % 3], b_ot[i % 3], gam, bet, b_gb, sa)

            def cC(i):
                lnp[i][0]()

            def cD(i):
                lnp[i][1]()

            def cE(i):
                lnp[i][2]()
                k.dma("sp", lambda q: q.dma_start(out=xout[i * 128:(i + 1) * 128, :], in_=ot[i % 3][:]),
                      reads=[b_ot[i % 3]], writes=[b_xout])

            run_pipeline(NT, [cA, cB, cC, cD, cE])
            barrier(k)
        k.es = es
    k.es = es_outer


_ln_scratch = {}


def layer_norm_parts(k, tag, acc, b_acc, ot, b_ot, gam, bet, b_gb, s):
    key = (tag, s, id(k))
    if key not in _ln_scratch:
        _ln_scratch[key] = (k.sb(f"{tag}st{s}", [128, 2, 6], F32), k.sb(f"{tag}mv{s}", [128, 2], F32),
                            k.sb(f"{tag}rs{s}", [128, 1], F32), k.buf())
    stt, mv, rs, b_s = _ln_scratch[key]
    mhalf = k.mhalf

    def part_a():
        for h in range(2):
            k.op("dve", lambda q: q.bn_stats(out=stt[:, h, :], in_=acc[:, h * 512:(h + 1) * 512]),
                 reads=[b_acc], writes=[b_s])
        k.op("dve", lambda q: q.bn_aggr(out=mv[:], in_=stt[:].rearrange("p a b -> p (a b)")), reads=[b_s], writes=[b_s])
        k.op("dve", lambda q: q.tensor_scalar(out=rs[:], in0=mv[:, 1:2], scalar1=LN_EPS, scalar2=None, op0=ALU.add),
             reads=[b_s], writes=[b_s])
        k.op("pool", lambda q: q.tensor_tensor(out=rs[:], in0=rs[:], in1=mhalf[:], op=ALU.pow),
             reads=[b_s], writes=[b_s])

    def part_b():
        k.op("dve", lambda q: q.tensor_scalar(out=acc[:], in0=acc[:], scalar1=mv[:, 0:1], scalar2=rs[:, 0:1],
                                              op0=ALU.subtract, op1=ALU.mult),
             reads=[b_s, b_acc], writes=[b_acc])
        k.op("dve", lambda q: q.tensor_tensor(out=acc[:], in0=acc[:], in1=gam[:], op=ALU.mult),
             reads=[b_acc, b_gb], writes=[b_acc])

    def part_c():
        k.op("dve", lambda q: q.tensor_tensor(out=ot[:], in0=acc[:], in1=bet[:], op=ALU.add),
             reads=[b_acc, b_gb], writes=[b_ot])

    return part_a, part_b, part_c


def layer_norm_tile(k, tag, acc, b_acc, ot, b_ot, gam, bet, b_gb, s):
    for part in layer_norm_parts(k, tag, acc, b_acc, ot, b_ot, gam, bet, b_gb, s):
        part()


class Deferred:
    def __init__(self):
        self.q = []
        self.it = 0

    def add(self, delay, fn):
        self.q.append((self.it + delay, fn))

    def tick(self, it):
        self.it = it + 1
        keep = []
        for due, fn in self.q:
            if due <= it:
                fn()
            else:
                keep.append((due, fn))
        self.q = keep

    def flush(self):
        while self.q:
            q, self.q = self.q, []
            for _, fn in q:
                fn()


def run_pipeline(n, stages, deferred=None):
    S = len(stages)
    for it in range(n + S - 1):
        if deferred is not None:
            deferred.it = it
        for st in range(S):
            i = it - st
            if 0 <= i < n:
                stages[st](i)
        if deferred is not None:
            deferred.tick(it)
    if deferred is not None:
        deferred.flush()


def barrier(k):
    deps = {}
    for n in k.engs:
        if k.cnt[n] > 0:
            s = k.sem[n]
            deps[id(s)] = (s, k.cnt[n])
    for q, ring in k.ring.items():
        for s, v in ring:
            if v > 0:
                deps[id(s)] = (s, v)
    for n in k.engs:
        k._wait(n, dict(deps))


def build_program(phases=("rglru", "moe0", "attn", "moe1")):
    nc = bass.Bass("TRN2", target_bir_lowering=False)

    need_moe = any(p.startswith("moe") for p in phases)

    def inp(name, shape):
        if name.startswith("moe_") and not need_moe:
            return None
        if name.startswith("rec_") and "rglru" not in phases:
            return None
        if name.startswith("att_") and "attn" not in phases:
            return None
        return nc.dram_tensor(name, list(shape), F32, kind="ExternalInput").ap()

    x = inp("x", [TOK, D])
    consts = inp("consts", [128, NCONST])
    moe_w_group = inp("moe_w_group", [2, D, 4])
    moe_b_group = inp("moe_b_group", [2, 4])
    moe_w_expert = inp("moe_w_expert", [2, D, NE])
    moe_b_expert = inp("moe_b_expert", [2, NE])
    moe_w1 = inp("moe_w1", [2, NE, D, DE])
    moe_w3 = inp("moe_w3", [2, NE, D, DE])
    moe_w2 = inp("moe_w2", [2, NE, DE, D])
    ln_g = inp("ln_g", [2, 2, D])
    ln_b = inp("ln_b", [2, 2, D])
    rec_w_in = inp("rec_w_in", [D, 2 * D_RNN])
    rec_cdiag = inp("rec_cdiag", [RC, NRC, 4, RC])
    rec_prm = inp("rec_prm", [RC, 4, NRC])
    rec_w_r = inp("rec_w_r", [NRC, RC, RC])
    rec_w_i = inp("rec_w_i", [NRC, RC, RC])
    rec_w_out = inp("rec_w_out", [D_RNN, D])
    att_w_qkv = inp("att_w_qkv", [D, 1536])
    att_sinks = inp("att_sinks", [NH])
    att_w_o = inp("att_w_o", [D, D])
    att_bias = inp("att_bias", [128, NH, 256])
    out = nc.dram_tensor("out", [TOK, D], F32, kind="ExternalOutput").ap()
    with ExitStack() as es:
        k = K(nc, es)
        cm = Common(k, consts)
        xbuf = k.dram("xbuf", [NSLOT, D], BF16)
        ybuf = k.dram("ybuf", [NSLOT, D], BF16)
        cur, b_cur = x, k.buf("x")
        b_out = k.buf("out", nowaw=True)
        for pi, ph in enumerate(phases):
            last = pi == len(phases) - 1
            if last:
                nxt, b_nxt = out, b_out
            else:
                nxt, b_nxt = k.dram(f"act{pi}", [TOK, D], F32), k.buf(f"act{pi}", nowaw=True)
            if ph == "rglru":
                rglru_phase(k, cm, "r0", cur, b_cur, nxt, b_nxt, rec_w_in, rec_cdiag, rec_prm, rec_w_r, rec_w_i,
                            rec_w_out, ln_g[0, 0], ln_b[0, 0])
            elif ph == "attn":
                attn_phase(k, cm, "a1", cur, b_cur, nxt, b_nxt, att_w_qkv, att_sinks, att_w_o, att_bias,
                           ln_g[1, 0], ln_b[1, 0])
            elif ph in ("moe0", "moe1"):
                l = int(ph[3])
                moe_phase(k, cm, f"m{l}", cur, b_cur, nxt, b_nxt, moe_w_group[l], moe_b_group[l], moe_w_expert[l],
                          moe_b_expert[l], moe_w1[l], moe_w3[l], moe_w2[l], ln_g[l, 1], ln_b[l, 1], xbuf, ybuf)
            cur, b_cur = nxt, b_nxt
        if DBG["stop"]:
            k.dma("sp", lambda e: e.dma_start(out=out[0:128, :], in_=x[0:128, :]), writes=[b_out])
        k.finish([b_out])
        barrier(k)
    return nc


def host_inputs(inputs):
    f = lambda a: np.ascontiguousarray(np.asarray(a, dtype=np.float32))
    d = {}
    d["consts"] = make_consts()
    for n in ("moe_w_group", "moe_b_group", "moe_w_expert", "moe_b_expert", "moe_w1", "moe_w3", "moe_w2",
              "ln_g", "ln_b"):
        d[n] = f(inputs[n])
    d["rec_w_in"] = f(inputs["rec_w_in"][0])
    cw = np.asarray(inputs["rec_conv_w"][0], np.float32)
    cdiag = np.zeros((RC, NRC, 4, RC), np.float32)
    pp = np.arange(RC)
    for c in range(NRC):
        for j in range(4):
            cdiag[pp, c, j, pp] = cw[j, c * RC:(c + 1) * RC]
    d["rec_cdiag"] = cdiag
    prm = np.stack([np.asarray(inputs[n][0], np.float32).reshape(NRC, RC).T
                    for n in ("rec_conv_b", "rec_b_r", "rec_b_i", "rec_lambda")], axis=1)
    d["rec_prm"] = f(prm)
    d["rec_w_r"] = f(inputs["rec_w_r"][0])
    d["rec_w_i"] = f(inputs["rec_w_i"][0])
    d["rec_w_out"] = f(inputs["rec_w_out"][0])
    d["att_w_qkv"] = f(inputs["att_w_qkv"][0])
    d["att_sinks"] = f(inputs["att_sinks"][0])
    d["att_w_o"] = f(inputs["att_w_o"][0])
    d["att_bias"] = make_att_bias()
    return d


def make_att_bias():
    slopes = 2.0 ** (-8.0 * np.arange(1, NH + 1, dtype=np.float32) / NH)
    qi = np.arange(128)[:, None]
    sj = np.arange(256)[None, :]
    dist = qi - sj + 128
    valid = (dist >= 0) & (dist < 128)
    b = np.where(valid[:, None, :], -slopes[None, :, None] * dist[:, None, :].astype(np.float32), -30000.0)
    return np.ascontiguousarray(b.astype(np.float32))


_PROG = {}


def kernel(**inputs):
    key = "full"
    if key not in _PROG:
        _PROG[key] = build_program()
    nc = _PROG[key]
    shared = host_inputs(inputs)
    x = np.ascontiguousarray(np.asarray(inputs["x"], np.float32)).reshape(NCORES, TOK, D)
    in_maps = [dict(shared, x=x[c]) for c in range(NCORES)]
    res = run_bass_kernel_spmd(nc, in_maps, core_ids=list(range(NCORES)))
    out = np.stack([np.asarray(r["out"]) for r in res.results], axis=0)
    return out.reshape(16, SEQ, D).astype(np.float32)


GELU_K = 0.7978845608028654
DBG = {"stop": 0}


class _Stop(Exception):
    pass

TCH = 1024


def rglru_phase(k, cm, T, xin, b_xin, xout, b_xout, w_in, cdiag, prm_d, w_r, w_i, w_out, ln_g, ln_b):
    nc = k.nc
    es_outer = k.es
    with ExitStack() as es:
        k.es = es
        P = RC
        gam = k.sb(T + "gam", [128, D], F32)
        bet = k.sb(T + "bet", [128, D], F32)
        b_gb = k.buf()
        k.dma("sp", lambda e: e.dma_start(out=gam[:], in_=ln_g.partition_broadcast(128)), writes=[b_gb])
        k.dma("sp", lambda e: e.dma_start(out=bet[:], in_=ln_b.partition_broadcast(128)), writes=[b_gb])
        wo_full = k.sb(T + "wo", [128, NRC, D], BF16)
        b_wo = k.buf()
        k.op("pool", lambda e: e.memset(wo_full[:], 0.0), writes=[b_wo])
        wo = wo_full[0:P]
        k.dma("pool", lambda g: g.dma_start(out=wo[:], in_=w_out.rearrange("(c p) n -> p c n", p=P)), writes=[b_wo])
        cd_full = k.sb(T + "cd", [128, NRC, 4, P], BF16)
        b_cd = k.buf()
        k.op("pool", lambda e: e.memset(cd_full[:], 0.0), writes=[b_cd])
        cd = cd_full[0:P]
        for c in range(NRC):
            k.dma("pool", lambda g: g.dma_start(out=cd[:, c, :, :], in_=cdiag[:, c, :, :]), writes=[b_cd])
        wrt_full = k.sb(T + "wrt", [128, NRC, P], BF16)
        b_wri = k.buf()
        k.op("pool", lambda e: e.memset(wrt_full[:], 0.0), writes=[b_wri])
        wrt = wrt_full[0:P]
        wit_full = k.sb(T + "wit", [128, NRC, P], BF16)
        k.op("pool", lambda e: e.memset(wit_full[:], 0.0), writes=[b_wri])
        wit = wit_full[0:P]
        k.dma("pool", lambda g: g.dma_start(out=wrt[:], in_=w_r.rearrange("n k j -> k n j")), writes=[b_wri])
        k.dma("pool", lambda g: g.dma_start(out=wit[:], in_=w_i.rearrange("n k j -> k n j")), writes=[b_wri])
        prm = k.sb(T + "prm", [P, 4, NRC], F32)
        b_prm = k.buf()
        k.dma("sp", lambda e: e.dma_start(out=prm[:], in_=prm_d), writes=[b_prm])
        der = k.sb(T + "der", [P, 5, NRC], F32)
        b_der = k.buf()
        k.op("dve", lambda e: e.tensor_scalar(out=der[:, 0:2, :], in0=prm[:, 1:3, :], scalar1=0.5, scalar2=None,
                                              op0=ALU.mult), reads=[b_prm], writes=[b_der])
        k.op("act", lambda e: e.activation(out=der[:, 4, :], in_=prm[:, 3, :], func=AF.Exp, scale=-1.0),
             reads=[b_prm, b_der], writes=[b_der])
        k.op("act", lambda e: e.activation(out=der[:, 4, :], in_=der[:, 4, :], func=AF.Ln, bias=1.0),
             reads=[b_der], writes=[b_der])
        k.op("dve", lambda e: e.tensor_scalar(out=der[:, 2, :], in0=der[:, 4, :], scalar1=-4.0, scalar2=None,
                                              op0=ALU.mult), reads=[b_der], writes=[b_der])
        k.op("dve", lambda e: e.tensor_scalar(out=der[:, 3, :], in0=der[:, 4, :], scalar1=-8.0, scalar2=None,
                                              op0=ALU.mult), reads=[b_der], writes=[b_der])

        if DBG["stop"] == 1:
            barrier(k); k.es = es_outer; return
        cvb = k.sb(T + "cvb", [P, NRC], F32)
        k.op("dve", lambda e: e.tensor_copy(out=cvb[:], in_=prm[:, 0, :]), reads=[b_prm], writes=[b_prm])
        xT = k.sb(T + "xT", [128, 8, TCH], BF16)
        b_xT = k.buf()
        YT_full = k.sb(T + "YT", [128, NRC, TCH], BF16)
        k.op("pool", lambda e: e.memset(YT_full[:], 0.0))
        YT = YT_full[0:P]
        b_YT = [k.buf() for _ in range(NRC)]
        xrc = k.sb(T + "xrc", [P, NRC, 4], F32)
        hc = k.sb(T + "hc", [P, NRC], F32)
        b_xrc = [k.buf() for _ in range(NRC)]
        b_hc = [k.buf() for _ in range(NRC)]
        xt = [k.sb(f"{T}xt{i}", [128, D], F32) for i in range(2)]
        b_xt = [k.buf() for _ in range(2)]
        xtb = [k.sb(f"{T}xtb{i}", [128, D], BF16) for i in range(2)]
        b_xtb = [k.buf() for _ in range(2)]
        wg = [[k.sb(f"{T}wg{i}_{j}", [128, 8, P], BF16) for j in range(2)] for i in range(2)]
        b_wg = [k.buf(nowaw=True) for _ in range(2)]
        G = [k.sb(f"{T}G{i}", [P, TCH], F32) for i in range(2)]
        XC = [k.sb(f"{T}XC{i}", [P, TCH], F32) for i in range(2)]
        TR = [k.sb(f"{T}TR{i}", [P, TCH], F32) for i in range(2)]
        TI = [k.sb(f"{T}TI{i}", [P, TCH], F32) for i in range(2)]
        NQ = TCH // 512
        b_G = [[k.buf() for _ in range(NQ)] for _ in range(2)]
        b_XC = [[k.buf() for _ in range(NQ)] for _ in range(2)]
        b_TR = [[k.buf() for _ in range(NQ)] for _ in range(2)]
        b_TI = [[k.buf() for _ in range(NQ)] for _ in range(2)]
        AA = k.sb(T + "AA", [P, TCH], F32)
        SQ = k.sb(T + "SQ", [P, TCH], F32)
        S2 = k.sb(T + "S2", [P, TCH], F32)
        HH = SQ
        XR_full = k.sb(T + "XR", [128, TCH + 4], BF16)
        k.op("pool", lambda e: e.memset(XR_full[:], 0.0))
        XR = XR_full[0:P]
        XCb_full = k.sb(T + "XCb", [128, TCH], BF16)
        k.op("pool", lambda e: e.memset(XCb_full[:], 0.0))
        XCb = XCb_full[0:P]
        XRf = k.sb(T + "XRf", [P, TCH + 4], F32)
        b_XRf = k.buf()
        XRb_full = k.sb(T + "XRb", [128, TCH + 4], BF16)
        k.op("pool", lambda e: e.memset(XRb_full[:], 0.0))
        XRb = XRb_full[0:P]
        b_XRb = k.buf()
        b_AA, b_SQ, b_S2, b_XR = [k.buf() for _ in range(4)]
        b_XCb = [k.buf() for _ in range(NQ)]
        NACC = 4
        acc = [k.sb(f"{T}acc{i}", [128, D], F32) for i in range(NACC)]
        b_acc = [k.buf() for _ in range(NACC)]
        psTb = k.ps(T + "psTb", [128, D], BF16)
        b_psTb = k.buf()
        pA = [k.ps(f"{T}pA{i}", [128, 512], F32) for i in range(5)]
        b_pA = [k.buf() for _ in range(5)]
        pO = [k.ps(f"{T}pO{i}", [128, 512], F32) for i in range(2)]
        b_pO = [k.buf() for _ in range(2)]
        barrier(k)
        npa = [0]

        def nxt():
            npa[0] += 1
            return npa[0] % 5

        def qs(q):
            return slice(q * 512, (q + 1) * 512)

        def load_wchunk(c):
            s = c % 2
            k.dma("pool", lambda g: g.dma_start(out=wg[s][0][:],
                                                in_=w_in[:, c * P:(c + 1) * P].rearrange("(c p) n -> p c n", p=128)),
                  writes=[b_wg[s]])
            k.dma("pool", lambda g: g.dma_start(out=wg[s][1][:],
                                                in_=w_in[:, D_RNN + c * P:D_RNN + (c + 1) * P].rearrange(
                                                    "(c p) n -> p c n", p=128)),
                  writes=[b_wg[s]])

        for sq in range(2):
            for th in range(SEQ // TCH):
                tok0 = sq * SEQ + th * TCH
                load_wchunk(0)
                for tl in range(TCH // 128):
                    s = tl % 2
                    r0 = tok0 + tl * 128
                    k.dma("sp", lambda e: e.dma_start(out=xt[s][:], in_=xin[r0:r0 + 128, :]),
                          reads=[b_xin], writes=[b_xt[s]])
                    k.op("act", lambda e: e.activation(out=xtb[s][:], in_=xt[s][:], func=AF.Copy),
                         reads=[b_xt[s]], writes=[b_xtb[s]])
                    for c in range(8):
                        k.op("pe", lambda e: e.transpose(out=psTb[:, c * 128:(c + 1) * 128],
                                                         in_=xtb[s][:, c * 128:(c + 1) * 128], identity=cm.ident_b),
                             reads=[b_xtb[s], cm.b_cb], writes=[b_psTb])
                    k.op("dve", lambda e: e.tensor_copy(out=xT[:, :, tl * 128:(tl + 1) * 128],
                                                        in_=psTb[:].rearrange("p (c t) -> p c t", c=8)),
                         reads=[b_psTb], writes=[b_xT])

                def stA(c):
                    u = c % 2
                    ws = c % 2
                    if c + 1 < NRC:
                        load_wchunk(c + 1)
                    for q in range(NQ):
                        pa = nxt()
                        for kk in range(8):
                            k.op("pe", lambda e: e.matmul(pA[pa][0:P, :], lhsT=wg[ws][0][:, kk, :], rhs=xT[:, kk, qs(q)],
                                                          start=(kk == 0), stop=(kk == 7)),
                                 reads=[b_wg[ws], b_xT], writes=[b_pA[pa]])
                        k.op("act", lambda e: e.activation(out=G[u][:, qs(q)], in_=pA[pa][0:P, :], func=AF.Copy),
                             reads=[b_pA[pa]], writes=[b_G[u][q]])
                    for q in range(NQ):
                        pa = nxt()
                        for kk in range(8):
                            k.op("pe", lambda e: e.matmul(pA[pa][0:P, :], lhsT=wg[ws][1][:, kk, :],
                                                          rhs=xT[:, kk, qs(q)], start=(kk == 0), stop=(kk == 7)),
                                 reads=[b_wg[ws], b_xT], writes=[b_pA[pa]])
                        k.op("dve", lambda e: e.tensor_copy(out=XRf[:, 4 + q * 512:4 + (q + 1) * 512], in_=pA[pa][0:P, :]),
                             reads=[b_pA[pa]], writes=[b_XRf])
                    yield
                    if th == 0:
                        k.op("dve", lambda e: e.memset(XRf[:, 0:4], 0.0), writes=[b_XRf])
                        yield
                    else:
                        k.op("dve", lambda e: e.tensor_copy(out=XRf[:, 0:4], in_=xrc[:, c, :]),
                             reads=[b_xrc[c]], writes=[b_XRf])
                        yield
                    k.op("dve", lambda e: e.tensor_copy(out=xrc[:, c, :], in_=XRf[:, TCH:TCH + 4]),
                         reads=[b_XRf], writes=[b_xrc[c]])
                    yield
                    k.op("dve", lambda e: e.tensor_copy(out=XR[:, 0:TCH + 4], in_=XRf[:, 0:TCH + 4]),
                         reads=[b_XRf], writes=[b_XR])
                    yield
                    k.op("act", lambda e: e.activation(out=XRb[:, 0:TCH + 2], in_=XRf[:, 1:TCH + 3], func=AF.Copy),
                         reads=[b_XRf], writes=[b_XRb])
                    yield
                    for q in range(NQ):
                        pa = nxt()
                        for j in range(4):
                            o = 1 + q * 512 + j
                            src = XR_full[:, o:o + 512] if o % 2 == 0 else XRb_full[:, o - 1:o - 1 + 512]
                            k.op("pe", lambda e: e.matmul(pA[pa][0:P, :], lhsT=cd_full[:, c, j, :], rhs=src,
                                                          start=(j == 0), stop=(j == 3)),
                                 reads=[b_cd, b_XR, b_XRb], writes=[b_pA[pa]])
                            yield
                        k.op("dve", lambda e: e.tensor_scalar(out=XC[u][:, qs(q)], in0=pA[pa][0:P, :],
                                                              scalar1=cvb[:, c:c + 1], scalar2=None, op0=ALU.add),
                             reads=[b_pA[pa], b_prm], writes=[b_XC[u][q]])
                        yield
                        k.op("act", lambda e: e.activation(out=XCb[:, qs(q)], in_=XC[u][:, qs(q)], func=AF.Copy),
                             reads=[b_XC[u][q]], writes=[b_XCb[q]])
                        yield
                    for q in range(NQ):
                        pa = nxt()
                        k.op("pe", lambda e: e.matmul(pA[pa][0:P, :], lhsT=wrt_full[:, c, :], rhs=XCb_full[:, qs(q)],
                                                      start=True, stop=True),
                             reads=[b_wri, b_XCb[q]], writes=[b_pA[pa]])
                        yield
                        k.op("act", lambda e: e.activation(out=TR[u][:, qs(q)], in_=pA[pa][0:P, :], func=AF.Tanh,
                                                           scale=0.5, bias=der[:, 0, c:c + 1]),
                             reads=[b_pA[pa], b_der], writes=[b_TR[u][q]])
                        yield
                        pa = nxt()
                        k.op("pe", lambda e: e.matmul(pA[pa][0:P, :], lhsT=wit_full[:, c, :], rhs=XCb_full[:, qs(q)],
                                                      start=True, stop=True),
                             reads=[b_wri, b_XCb[q]], writes=[b_pA[pa]])
                        yield
                        k.op("act", lambda e: e.activation(out=TI[u][:, qs(q)], in_=pA[pa][0:P, :], func=AF.Tanh,
                                                           scale=0.5, bias=der[:, 1, c:c + 1]),
                             reads=[b_pA[pa], b_der], writes=[b_TI[u][q]])
                        yield

                def stB(c):
                    u = c % 2
                    k.op("act", lambda e: e.activation(out=S2[:], in_=G[u][:], func=AF.Square),
                         reads=b_G[u], writes=[b_S2])
                    yield
                    k.op("dve", lambda e: e.tensor_scalar(out=S2[:], in0=S2[:], scalar1=0.044715, scalar2=1.0,
                                                          op0=ALU.mult, op1=ALU.add), reads=[b_S2], writes=[b_S2])
                    yield
                    k.op("pool", lambda e: e.tensor_tensor(out=S2[:], in0=S2[:], in1=G[u][:], op=ALU.mult),
                         reads=[b_S2, *b_G[u]], writes=[b_S2])
                    yield
                    k.op("act", lambda e: e.activation(out=AA[:], in_=TR[u][:], func=AF.Exp, scale=der[:, 2, c:c + 1],
                                                       bias=der[:, 2, c:c + 1]),
                         reads=[*b_TR[u], b_der], writes=[b_AA])
                    yield
                    k.op("act", lambda e: e.activation(out=SQ[:], in_=TR[u][:], func=AF.Exp, scale=der[:, 3, c:c + 1],
                                                       bias=der[:, 3, c:c + 1]),
                         reads=[*b_TR[u], b_der], writes=[b_SQ])
                    yield
                    k.op("act", lambda e: e.activation(out=S2[:], in_=S2[:], func=AF.Tanh, scale=GELU_K),
                         reads=[b_S2], writes=[b_S2])
                    yield
                    k.op("act", lambda e: e.activation(out=SQ[:], in_=SQ[:], func=AF.Sqrt, scale=-1.0, bias=1.0),
                         reads=[b_SQ], writes=[b_SQ])
                    yield
                    k.op("dve", lambda e: e.scalar_tensor_tensor(out=S2[:], in0=S2[:], scalar=1.0, in1=G[u][:],
                                                                 op0=ALU.add, op1=ALU.mult),
                         reads=[b_S2, *b_G[u]], writes=[b_S2])
                    yield
                    k.op("dve", lambda e: e.scalar_tensor_tensor(out=TI[u][:], in0=TI[u][:], scalar=1.0, in1=XC[u][:],
                                                                 op0=ALU.add, op1=ALU.mult),
                         reads=[*b_TI[u], *b_XC[u]], writes=b_TI[u])
                    yield
                    k.op("pool", lambda e: e.tensor_tensor(out=TI[u][:], in0=TI[u][:], in1=SQ[:], op=ALU.mult),
                         reads=[*b_TI[u], b_SQ], writes=b_TI[u])
                    yield
                    init = 0.0 if th == 0 else hc[:, c:c + 1]
                    k.op("dve", lambda e: e.tensor_tensor_scan(out=HH[:], data0=AA[:], data1=TI[u][:], initial=init,
                                                               op0=ALU.mult, op1=ALU.add),
                         reads=[b_AA, *b_TI[u], b_hc[c], b_SQ], writes=[b_SQ])
                    yield
                    k.op("dve", lambda e: e.tensor_copy(out=hc[:, c:c + 1], in_=HH[:, TCH - 1:TCH]),
                         reads=[b_SQ], writes=[b_hc[c]])
                    yield
                    k.op("dve", lambda e: e.scalar_tensor_tensor(out=YT[:, c, :], in0=S2[:], scalar=0.25, in1=HH[:],
                                                                 op0=ALU.mult, op1=ALU.mult),
                         reads=[b_S2, b_SQ], writes=[b_YT[c]])
                    yield

                def drive(gens):
                    live = list(gens)
                    while live:
                        for g_ in list(live):
                            try:
                                next(g_)
                            except StopIteration:
                                live.remove(g_)

                if DBG.get("var") == "seq":
                    for c in range(NRC):
                        drive([stA(c)])
                        drive([stB(c)])
                else:
                    for t in range(NRC + 1):
                        gens = []
                        if t < NRC:
                            ga = stA(t)
                            next(ga)
                            gens.append(ga)
                        if t >= 1:
                            gens.append(stB(t - 1))
                        drive(gens)

                lnp = {}

                def p0(tl):
                    s, sa = tl % 2, tl % NACC
                    r0 = tok0 + tl * 128
                    k.dma("sp", lambda e: e.dma_start(out=xt[s][:], in_=xin[r0:r0 + 128, :]),
                          reads=[b_xin], writes=[b_xt[s]])
                    k.op("act", lambda e: e.activation(out=acc[sa][:], in_=xt[s][:], func=AF.Copy, scale=ALPHA),
                         reads=[b_xt[s]], writes=[b_acc[sa]])

                def p1(tl):
                    sa = tl % NACC
                    for h in range(2):
                        for c in range(NRC):
                            k.op("pe", lambda e: e.matmul(pO[h][:], lhsT=YT_full[:, c, tl * 128:(tl + 1) * 128],
                                                          rhs=wo_full[:, c, h * 512:(h + 1) * 512],
                                                          start=(c == 0), stop=(c == NRC - 1)),
                                 reads=[b_YT[c], b_wo], writes=[b_pO[h]])
                        k.op("dve", lambda e: e.tensor_tensor(out=acc[sa][:, h * 512:(h + 1) * 512],
                                                              in0=acc[sa][:, h * 512:(h + 1) * 512], in1=pO[h][:],
                                                              op=ALU.add),
                             reads=[b_pO[h], b_acc[sa]], writes=[b_acc[sa]])
                    lnp[tl] = layer_norm_parts(k, T + "ln", acc[sa], b_acc[sa], acc[sa], b_acc[sa], gam, bet, b_gb, sa)

                def p2(tl):
                    lnp[tl][0]()

                def p3(tl):
                    sa = tl % NACC
                    r0 = tok0 + tl * 128
                    lnp[tl][1]()
                    lnp[tl][2]()
                    k.dma("sp", lambda e: e.dma_start(out=xout[r0:r0 + 128, :], in_=acc[sa][:]),
                          reads=[b_acc[sa]], writes=[b_xout])

                run_pipeline(TCH // 128, [p0, p1, p2, p3])
        barrier(k)
    k.es = es_outer


def attn_phase(k, cm, T, xin, b_xin, xout, b_xout, w_qkv, sinks, w_o, att_bias, ln_g, ln_b):
    es_outer = k.es
    HALF = 1024
    with ExitStack() as es:
        k.es = es
        gam = k.sb(T + "gam", [128, D], F32)
        bet = k.sb(T + "bet", [128, D], F32)
        b_gb = k.buf()
        k.dma("sp", lambda e: e.dma_start(out=gam[:], in_=ln_g.partition_broadcast(128)), writes=[b_gb])
        k.dma("sp", lambda e: e.dma_start(out=bet[:], in_=ln_b.partition_broadcast(128)), writes=[b_gb])
        wq = k.sb(T + "wq", [128, 8, 1024], BF16)
        wk = k.sb(T + "wk", [128, 8, 256], BF16)
        wv = k.sb(T + "wv", [128, 8, 256], BF16)
        wo = k.sb(T + "wo", [128, 8, 1024], BF16)
        b_w = k.buf(nowaw=True)
        k.dma("pool", lambda g: g.dma_start(out=wq[:], in_=w_qkv[:, 0:1024].rearrange("(c p) n -> p c n", p=128)),
              writes=[b_w])
        k.dma("pool", lambda g: g.dma_start(out=wk[:], in_=w_qkv[:, 1024:1280].rearrange("(c p) n -> p c n", p=128)),
              writes=[b_w])
        k.dma("pool", lambda g: g.dma_start(out=wv[:], in_=w_qkv[:, 1280:1536].rearrange("(c p) n -> p c n", p=128)),
              writes=[b_w])
        k.dma("pool", lambda g: g.dma_start(out=wo[:], in_=w_o.rearrange("(c p) n -> p c n", p=128)), writes=[b_w])
        sk = k.sb(T + "sk", [128, NH], F32)
        bt = k.sb(T + "bt", [128, NH, 256], F32)
        b_c = k.buf()
        k.dma("sp", lambda e: e.dma_start(out=sk[:], in_=sinks.partition_broadcast(128)), writes=[b_c])
        k.dma("sp", lambda e: e.dma_start(out=bt[:], in_=att_bias), writes=[b_c])
        nsk = k.sb(T + "nsk", [128, NH], F32)
        k.op("dve", lambda e: e.tensor_scalar(out=nsk[:], in0=sk[:], scalar1=-1.0, scalar2=None, op0=ALU.mult),
             reads=[b_c], writes=[b_c])

        xT = k.sb(T + "xT", [128, 8, HALF], BF16)
        b_xT = k.buf()
        QT = k.sb(T + "QT", [HD, NH, HALF], BF16)
        b_QT = k.buf()
        KT = k.sb(T + "KT", [HD, NKV, SEQ], BF16)
        b_KT = k.buf()
        V = k.sb(T + "V", [128, SEQ // 128, NKV * HD], BF16)
        b_V = k.buf()
        xt = [k.sb(f"{T}xt{i}", [128, D], F32) for i in range(2)]
        b_xt = [k.buf() for _ in range(2)]
        xtb = [k.sb(f"{T}xtb{i}", [128, D], BF16) for i in range(2)]
        b_xtb = [k.buf() for _ in range(2)]
        Sb = [k.sb(f"{T}Sb{i}", [128, 2, 256], F32) for i in range(3)]
        Pm = [k.sb(f"{T}Pm{i}", [128, 2, 256], BF16) for i in range(3)]
        PT = [k.sb(f"{T}PT{i}", [128, 2, 256], BF16) for i in range(3)]
        sm = [k.sb(f"{T}sm{i}", [128, 12], F32) for i in range(4)]
        b_Sb = [k.buf() for _ in range(3)]
        b_Pm = [k.buf() for _ in range(3)]
        b_PT = [k.buf() for _ in range(3)]
        b_sm = [[k.buf() for _ in range(6)] for _ in range(4)]
        Ot = [k.sb(f"{T}Ot{i}", [128, D], BF16) for i in range(2)]
        b_Ot = [k.buf() for _ in range(2)]
        OT = [k.sb(f"{T}OT{i}", [128, 8, 128], BF16) for i in range(2)]
        b_OT = [k.buf() for _ in range(2)]
        acc = [k.sb(f"{T}acc{i}", [128, D], F32) for i in range(2)]
        b_acc = [k.buf() for _ in range(2)]
        ot = [k.sb(f"{T}ot{i}", [128, D], F32) for i in range(2)]
        b_ot = [k.buf() for _ in range(2)]
        psTb = k.ps(T + "psTb", [128, D], BF16)
        b_psTb = k.buf()
        psS_bk = [k.ps(f"{T}psS{i}", [128, 512], F32) for i in range(2)]
        _bS = [k.buf() for _ in range(2)]
        psS = [psS_bk[i % 2][:, 0:256] for i in range(4)]
        b_psS = [_bS[i % 2] for i in range(4)]
        psPT_bk = [k.ps(f"{T}psPT{i}", [128, 1024], BF16) for i in range(2)]
        _bP = [k.buf() for _ in range(2)]
        psPT = [psPT_bk[i % 2][:, 0:256] for i in range(4)]
        b_psPT = [_bP[i % 2] for i in range(4)]
        psO_bk = [k.ps(f"{T}psO{i}", [128, 512], F32) for i in range(2)]
        _bO = [k.buf() for _ in range(2)]
        psO = [psO_bk[i % 2][:, 0:HD] for i in range(8)]
        b_psO = [_bO[i % 2] for i in range(8)]
        pE = k.ps(T + "pE", [128, 512], F32)
        b_pE = k.buf()
        pA = psS_bk
        b_pA = _bS
        npa = [0]

        def next_pa():
            npa[0] += 1
            return npa[0] % 2

        def qs(q):
            return slice(q * 512, (q + 1) * 512)

        hcnt = 0
        for sq in range(2):
            for hf in range(SEQ // HALF):
                tok0 = sq * SEQ + hf * HALF
                for tl in range(HALF // 128):
                    s = tl % 2
                    r0 = tok0 + tl * 128
                    k.dma("sp", lambda e: e.dma_start(out=xt[s][:], in_=xin[r0:r0 + 128, :]),
                          reads=[b_xin], writes=[b_xt[s]])
                    k.op("act", lambda e: e.activation(out=xtb[s][:], in_=xt[s][:], func=AF.Copy),
                         reads=[b_xt[s]], writes=[b_xtb[s]])
                    for c in range(8):
                        k.op("pe", lambda e: e.transpose(out=psTb[:, c * 128:(c + 1) * 128],
                                                         in_=xtb[s][:, c * 128:(c + 1) * 128], identity=cm.ident_b),
                             reads=[b_xtb[s], cm.b_cb], writes=[b_psTb])
                    k.op("dve", lambda e: e.tensor_copy(out=xT[:, :, tl * 128:(tl + 1) * 128],
                                                        in_=psTb[:].rearrange("p (c t) -> p c t", c=8)),
                         reads=[b_psTb], writes=[b_xT])
                for h in range(NH):
                    for q in range(HALF // 512):
                        pa = next_pa()
                        for kk in range(8):
                            k.op("pe", lambda e: e.matmul(pA[pa][0:HD, :], lhsT=wq[:, kk, h * HD:(h + 1) * HD],
                                                          rhs=xT[:, kk, qs(q)], start=(kk == 0), stop=(kk == 7)),
                                 reads=[b_w, b_xT], writes=[b_pA[pa]])
                        if (h + q) % 2 == 0:
                            k.op("act", lambda e: e.activation(out=QT[:, h, qs(q)], in_=pA[pa][0:HD, :], func=AF.Copy,
                                                               scale=HD ** -0.5),
                                 reads=[b_pA[pa]], writes=[b_QT])
                        else:
                            k.op("dve", lambda e: e.tensor_scalar(out=QT[:, h, qs(q)], in0=pA[pa][0:HD, :],
                                                                  scalar1=HD ** -0.5, scalar2=None, op0=ALU.mult),
                                 reads=[b_pA[pa]], writes=[b_QT])
                for kv in range(NKV):
                    for q in range(HALF // 512):
                        pa = next_pa()
                        for kk in range(8):
                            k.op("pe", lambda e: e.matmul(pA[pa][0:HD, :], lhsT=wk[:, kk, kv * HD:(kv + 1) * HD],
                                                          rhs=xT[:, kk, qs(q)], start=(kk == 0), stop=(kk == 7)),
                                 reads=[b_w, b_xT], writes=[b_pA[pa]])
                        c0 = hf * HALF + q * 512
                        k.op("act", lambda e: e.activation(out=KT[:, kv, c0:c0 + 512], in_=pA[pa][0:HD, :], func=AF.Copy),
                             reads=[b_pA[pa]], writes=[b_KT])
                for tl in range(HALF // 128):
                    pa = next_pa()
                    for kk in range(8):
                        k.op("pe", lambda e: e.matmul(pA[pa][:, 0:256], lhsT=xT[:, kk, tl * 128:(tl + 1) * 128],
                                                      rhs=wv[:, kk, :], start=(kk == 0), stop=(kk == 7)),
                             reads=[b_w, b_xT], writes=[b_pA[pa]])
                    k.op("dve", lambda e: e.tensor_copy(out=V[:, hf * 8 + tl, :], in_=pA[pa][:, 0:256]),
                         reads=[b_pA[pa]], writes=[b_V])
                NB_ = HALF // 128
                items = [(b, hp) for b in range(NB_) for hp in range(NH // 2)]

                def geom(b):
                    g = hf * NB_ + b
                    has_prev = g > 0
                    cs = slice(0, 256) if has_prev else slice(128, 256)
                    k0 = (g - 1) * 128 if has_prev else 0
                    return g, has_prev, cs, k0, (g + 1) * 128

                def st1(n):
                    b, hp = items[n]
                    g, has_prev, cs, k0, k1 = geom(b)
                    h0 = 2 * hp
                    kv = h0 // 4
                    os_ = b % 2
                    r0 = tok0 + b * 128
                    if hp == 0:
                        k.dma("sp", lambda e: e.dma_start(out=xt[os_][:], in_=xin[r0:r0 + 128, :]),
                              reads=[b_xin], writes=[b_xt[os_]])
                        k.op("act", lambda e: e.activation(out=acc[os_][:], in_=xt[os_][:], func=AF.Copy, scale=ALPHA),
                             reads=[b_xt[os_]], writes=[b_acc[os_]])
                    s2, s3, s4 = n % 2, n % 3, n % 4
                    pS = psS_bk[s2][:].rearrange("p (j c) -> p j c", j=2)
                    for j in range(2):
                        k.op("pe", lambda e: e.matmul(pS[:, j, cs], lhsT=QT[:, h0 + j, b * 128:(b + 1) * 128],
                                                      rhs=KT[:, kv, k0:k1], start=True, stop=True),
                             reads=[b_QT, b_KT], writes=[_bS[s2]])
                    k.op("dve", lambda e: e.tensor_tensor(out=Sb[s3][:, :, cs], in0=pS[:, :, cs], in1=bt[:, h0:h0 + 2, cs],
                                                          op=ALU.add),
                         reads=[_bS[s2], b_c], writes=[b_Sb[s3]])
                    k.op("dve", lambda e: e.tensor_reduce(out=sm[s4][:, 0:2], in_=Sb[s3][:, :, cs], axis=AX.X, op=ALU.max),
                         reads=[b_Sb[s3]], writes=[b_sm[s4][0]])
                    k.op("dve", lambda e: e.scalar_tensor_tensor(out=sm[s4][:, 2:4], in0=sm[s4][:, 0:2], scalar=-1.0,
                                                                 in1=nsk[:, h0:h0 + 2], op0=ALU.mult, op1=ALU.min),
                         reads=[b_sm[s4][0], b_c], writes=[b_sm[s4][1]])
                    for j in range(2):
                        k.op("act", lambda e: e.activation(out=Pm[s3][:, j, cs], in_=Sb[s3][:, j, cs], func=AF.Exp,
                                                           bias=sm[s4][:, 2 + j:3 + j], accum_out=sm[s4][:, 4 + j:5 + j]),
                             reads=[b_Sb[s3], b_sm[s4][1]], writes=[b_Pm[s3], b_sm[s4][2]])
                    k.op("dve", lambda e: e.tensor_tensor(out=sm[s4][:, 6:8], in0=sk[:, h0:h0 + 2], in1=sm[s4][:, 2:4],
                                                          op=ALU.add),
                         reads=[b_sm[s4][1], b_c], writes=[b_sm[s4][3]])
                    k.op("act", lambda e: e.activation(out=sm[s4][:, 6:8], in_=sm[s4][:, 6:8], func=AF.Exp),
                         reads=[b_sm[s4][3]], writes=[b_sm[s4][3]])

                def st2(n):
                    b, hp = items[n]
                    g, has_prev, cs, k0, k1 = geom(b)
                    s2, s3, s4 = n % 2, n % 3, n % 4
                    k.op("dve", lambda e: e.tensor_tensor(out=sm[s4][:, 8:10], in0=sm[s4][:, 4:6], in1=sm[s4][:, 6:8],
                                                          op=ALU.add),
                         reads=[b_sm[s4][2], b_sm[s4][3]], writes=[b_sm[s4][4]])
                    k.op("dve", lambda e: e.reciprocal(out=sm[s4][:, 10:12], in_=sm[s4][:, 8:10]),
                         reads=[b_sm[s4][4]], writes=[b_sm[s4][5]])
                    pP = psPT_bk[s2][:, 0:512].rearrange("p (j c) -> p j c", j=2)
                    for j in range(2):
                        if has_prev:
                            k.op("pe", lambda e: e.transpose(out=pP[:, j, 0:128], in_=Pm[s3][:, j, 0:128],
                                                             identity=cm.ident_b),
                                 reads=[b_Pm[s3], cm.b_cb], writes=[_bP[s2]])
                        k.op("pe", lambda e: e.transpose(out=pP[:, j, 128:256], in_=Pm[s3][:, j, 128:256],
                                                         identity=cm.ident_b),
                             reads=[b_Pm[s3], cm.b_cb], writes=[_bP[s2]])
                    if n % 2 == 0:
                        k.op("act", lambda e: e.activation(out=PT[s3][:, :, cs], in_=pP[:, :, cs], func=AF.Copy),
                             reads=[_bP[s2]], writes=[b_PT[s3]])
                    else:
                        k.op("dve", lambda e: e.tensor_copy(out=PT[s3][:, :, cs], in_=pP[:, :, cs]),
                             reads=[_bP[s2]], writes=[b_PT[s3]])

                def st3(n):
                    b, hp = items[n]
                    g, has_prev, cs, k0, k1 = geom(b)
                    h0 = 2 * hp
                    kv = h0 // 4
                    os_ = b % 2
                    s2, s3, s4 = n % 2, n % 3, n % 4
                    pO_ = psO_bk[s2][:, 0:2 * HD].rearrange("p (j d) -> p j d", j=2)
                    for j in range(2):
                        if has_prev:
                            k.op("pe", lambda e: e.matmul(pO_[:, j, :], lhsT=PT[s3][:, j, 0:128],
                                                          rhs=V[:, g - 1, kv * HD:(kv + 1) * HD], start=True, stop=False),
                                 reads=[b_PT[s3], b_V], writes=[_bO[s2]])
                        k.op("pe", lambda e: e.matmul(pO_[:, j, :], lhsT=PT[s3][:, j, 128:256],
                                                      rhs=V[:, g, kv * HD:(kv + 1) * HD], start=(not has_prev), stop=True),
                             reads=[b_PT[s3], b_V], writes=[_bO[s2]])
                    k.op("dve", lambda e: e.tensor_tensor(
                        out=Ot[os_][:, h0 * HD:(h0 + 2) * HD].rearrange("p (j d) -> p j d", j=2), in0=pO_,
                        in1=sm[s4][:, 10:12].unsqueeze(2).to_broadcast([128, 2, HD]), op=ALU.mult),
                        reads=[_bO[s2], b_sm[s4][5]], writes=[b_Ot[os_]])
                    if hp == NH // 2 - 1:
                        epilogue(b)

                dfr = Deferred()

                def epilogue(b):
                    os_ = b % 2
                    r0 = tok0 + b * 128

                    def e1():
                        for c in range(8):
                            k.op("pe", lambda e: e.transpose(out=psTb[:, c * 128:(c + 1) * 128],
                                                             in_=Ot[os_][:, c * 128:(c + 1) * 128], identity=cm.ident_b),
                                 reads=[b_Ot[os_], cm.b_cb], writes=[b_psTb])
                        k.op("act", lambda e: e.activation(out=OT[os_][:], in_=psTb[:].rearrange("p (c t) -> p c t", c=8),
                                                           func=AF.Copy),
                             reads=[b_psTb], writes=[b_OT[os_]])

                    def e2(hh):
                        def f():
                            for c in range(8):
                                k.op("pe", lambda e: e.matmul(pE[:], lhsT=OT[os_][:, c, :],
                                                              rhs=wo[:, c, hh * 512:(hh + 1) * 512],
                                                              start=(c == 0), stop=(c == 7)),
                                     reads=[b_OT[os_], b_w], writes=[b_pE])
                        return f

                    def e3(hh):
                        def f():
                            k.op("dve", lambda e: e.tensor_tensor(out=acc[os_][:, hh * 512:(hh + 1) * 512],
                                                                  in0=acc[os_][:, hh * 512:(hh + 1) * 512], in1=pE[:],
                                                                  op=ALU.add),
                                 reads=[b_pE, b_acc[os_]], writes=[b_acc[os_]])
                        return f

                    pa_, pb_, pc_ = layer_norm_parts(k, T + "ln", acc[os_], b_acc[os_], ot[os_], b_ot[os_], gam, bet,
                                                     b_gb, os_)

                    def store():
                        k.dma("sp", lambda e: e.dma_start(out=xout[r0:r0 + 128, :], in_=ot[os_][:]),
                              reads=[b_ot[os_]], writes=[b_xout])

                    e1()
                    dfr.add(1, e2(0))
                    dfr.add(2, e3(0))
                    dfr.add(2, e2(1))
                    dfr.add(3, e3(1))
                    dfr.add(3, pa_)
                    dfr.add(4, pb_)
                    dfr.add(5, pc_)
                    dfr.add(6, store)

                run_pipeline(len(items), [st1, st2, st3], dfr)
        barrier(k)
    k.es = es_outer
```

```python
from contextlib import ExitStack

import numpy as np
import concourse.bass as bass
import concourse.mybir as mybir
from concourse.bass_utils import run_bass_kernel_spmd

F32 = mybir.dt.float32
BF16 = mybir.dt.bfloat16
I32 = mybir.dt.int32
AF = mybir.ActivationFunctionType
ALU = mybir.AluOpType
AX = mybir.AxisListType

NCORES = 8
D = 1024
SEQ = 2048
TOK = 4096
NT = TOK // 128
NE = 32
DE = 512
CAP = 384
NSLOT = NE * CAP
ALPHA = float((2 * 2) ** 0.25)
LN_EPS = 1e-5
D_RNN = 1280
RC = 80
NRC = D_RNN // RC
NH = 16
NKV = 4
HD = 64


class Buf:
    __slots__ = ("name", "writers", "readers", "nowaw")

    def __init__(self, name, nowaw=False):
        self.name = name
        self.writers = {}
        self.readers = {}
        self.nowaw = nowaw


def _merge(dst, src):
    for k, (s, v) in src.items():
        if k not in dst or dst[k][1] < v:
            dst[k] = (s, v)


class K:
    def __init__(self, nc, es):
        self.nc = nc
        self.es = es
        self.es_root = es
        self.engs = {"pe": nc.tensor, "dve": nc.vector, "act": nc.scalar, "pool": nc.gpsimd, "sp": nc.sync}
        self.sem = {}
        self.cnt = {}
        self.waited = {n: {} for n in self.engs}
        for n in self.engs:
            self.sem[n] = es.enter_context(nc.semaphore("s_" + n))
            self.cnt[n] = 0
        self.ring = {}
        for q, n in (("sp", 10), ("pool", 10), ("act", 4)):
            self.ring[q] = [[es.enter_context(nc.semaphore(f"d_{q}{i}")), 0] for i in range(n)]
        self.ring_pos = {q: 0 for q in self.ring}
        self.nbuf = 0

    def sb(self, name, shape, dt):
        return self.es.enter_context(self.nc.sbuf_tensor(name, list(shape), dt))

    def ps(self, name, shape, dt):
        return self.es.enter_context(self.nc.psum_tensor(name, list(shape), dt))

    def dram(self, name, shape, dt):
        return self.nc.dram_tensor(name, list(shape), dt, kind="Internal").ap()

    def buf(self, name=None, nowaw=False):
        self.nbuf += 1
        return Buf(name or f"b{self.nbuf}", nowaw)

    def _wait(self, engname, deps):
        e = self.engs[engname]
        w = self.waited[engname]
        for k, (s, v) in deps.items():
            if engname == "pe" and k == id(self.sem["pe"]):
                continue
            if w.get(k, 0) >= v:
                continue
            e.wait_ge(s, v)
            w[k] = v

    def _deps(self, reads, writes):
        deps = {}
        for b in reads:
            _merge(deps, b.writers)
        for b in writes:
            if not b.nowaw:
                _merge(deps, b.writers)
            _merge(deps, b.readers)
        return deps

    def _record(self, tok, reads, writes):
        k, s, v = tok
        for b in reads:
            if k not in b.readers or b.readers[k][1] < v:
                b.readers[k] = (s, v)
        for b in writes:
            if b.readers:
                b.writers = {}
                b.readers = {}
            b.writers[k] = (s, v)

    def op(self, engname, fn, reads=(), writes=()):
        deps = self._deps(reads, writes)
        self._wait(engname, deps)
        inst = fn(self.engs[engname])
        s = self.sem[engname]
        self.cnt[engname] += 1
        inst.then_inc(s, 1)
        tok = (id(s), s, self.cnt[engname])
        self._record(tok, reads, writes)
        return tok

    def dma(self, q, fn, reads=(), writes=()):
        deps = self._deps(reads, writes)
        ring = self.ring[q]
        pos = self.ring_pos[q]
        self.ring_pos[q] = (pos + 1) % len(ring)
        ent = ring[pos]
        s = ent[0]
        if ent[1] > 0:
            deps[id(s)] = (s, ent[1])
        self._wait(q, deps)
        inst = fn(self.engs[q])
        ent[1] += 16
        inst.then_inc(s, 16)
        tok = (id(s), s, ent[1])
        self._record(tok, reads, writes)
        return tok

    def finish(self, bufs):
        deps = {}
        for b in bufs:
            _merge(deps, b.writers)
        self._wait("sp", deps)


C_IDENT = 0
C_UPPER = 128
C_ONES = 256
C_EBASE = 384
NCONST = 416


def make_consts():
    c = np.zeros((128, NCONST), np.float32)
    c[:, C_IDENT:C_IDENT + 128] = np.eye(128, dtype=np.float32)
    c[:, C_UPPER:C_UPPER + 128] = np.triu(np.ones((128, 128), np.float32), 1)
    c[:, C_ONES:C_ONES + 128] = 1.0
    c[:, C_EBASE:C_EBASE + NE] = (np.arange(NE, dtype=np.float32) * CAP)[None, :]
    return c


class Common:
    def __init__(self, k, consts_ap):
        self.k = k
        nc = k.nc
        self.cf = k.sb("cf", [128, NCONST], F32)
        self.b_cf = k.buf("cf")
        k.dma("sp", lambda e: e.dma_start(out=self.cf[:], in_=consts_ap), writes=[self.b_cf])
        self.cb = k.sb("cb", [128, 384], BF16)
        self.b_cb = k.buf("cb")
        k.op("dve", lambda e: e.tensor_copy(out=self.cb[:], in_=self.cf[:, 0:384]),
             reads=[self.b_cf], writes=[self.b_cb])
        self.ident_f = self.cf[:, C_IDENT:C_IDENT + 128]
        self.ident_b = self.cb[:, C_IDENT:C_IDENT + 128]
        self.upper_b = self.cb[:, C_UPPER:C_UPPER + 128]
        self.ones_b = self.cb[:, C_ONES:C_ONES + 128]
        self.ebase = self.cf[:, C_EBASE:C_EBASE + NE]
        self.bound_reg = nc.gpsimd.to_reg(NSLOT - 1)
        k.mhalf = k.sb("ln_mhalf", [128, 1], F32)
        k.op("pool", lambda q: q.memset(k.mhalf[:], -0.5))
        barrier(k)


def moe_phase(k, cm, tag, xin, b_xin, xout, b_xout, w_rg, b_rg, w_re, b_re, w1, w3, w2, ln_g, ln_b,
              xbuf, ybuf):
    nc = k.nc
    es_outer = k.es
    with ExitStack() as es:
        k.es = es
        T = tag
        wr = k.sb(T + "wr", [128, 8, 36], F32)
        b_wr = k.buf()
        k.dma("sp", lambda e: e.dma_start(out=wr[:, :, 0:4], in_=w_rg.rearrange("(c p) n -> p c n", p=128)),
              writes=[b_wr])
        k.dma("sp", lambda e: e.dma_start(out=wr[:, :, 4:36], in_=w_re.rearrange("(c p) n -> p c n", p=128)),
              writes=[b_wr])
        rbias = k.sb(T + "rbias", [128, 36], F32)
        b_rbias = k.buf()
        k.dma("sp", lambda e: e.dma_start(out=rbias[:, 0:4], in_=b_rg.partition_broadcast(128)), writes=[b_rbias])
        k.dma("sp", lambda e: e.dma_start(out=rbias[:, 4:36], in_=b_re.partition_broadcast(128)), writes=[b_rbias])
        gam = k.sb(T + "gam", [128, D], F32)
        bet = k.sb(T + "bet", [128, D], F32)
        b_gb = k.buf()
        k.dma("sp", lambda e: e.dma_start(out=gam[:], in_=ln_g.partition_broadcast(128)), writes=[b_gb])
        k.dma("sp", lambda e: e.dma_start(out=bet[:], in_=ln_b.partition_broadcast(128)), writes=[b_gb])
        idx_all = k.sb(T + "idx", [128, NT, 2], I32)
        gate_all = k.sb(T + "gate", [128, NT, 2], F32)
        b_idx = k.buf()
        b_gate = k.buf()

        NWB = 4
        w1t = [k.sb(f"{T}w1_{i}", [128, 8, DE], BF16) for i in range(NWB)]
        w3t = [k.sb(f"{T}w3_{i}", [128, 8, DE], BF16) for i in range(NWB)]
        w2t = [k.sb(f"{T}w2_{i}", [128, 4, D], BF16) for i in range(NWB)]
        b_w13 = [k.buf(nowaw=True) for _ in range(NWB)]
        b_w2 = [k.buf() for _ in range(NWB)]

        def load_weights(e):
            s = e % NWB
            k.dma("pool", lambda g: g.dma_start(out=w1t[s][:], in_=w1[e].rearrange("(c p) n -> p c n", p=128)),
                  writes=[b_w13[s]])
            k.dma("pool", lambda g: g.dma_start(out=w3t[s][:], in_=w3[e].rearrange("(c p) n -> p c n", p=128)),
                  writes=[b_w13[s]])
            k.dma("pool", lambda g: g.dma_start(out=w2t[s][:], in_=w2[e].rearrange("(c p) n -> p c n", p=128)),
                  writes=[b_w2[s]])

        for e in range(NWB):
            load_weights(e)

        with ExitStack() as es2:
            k.es = es2
            lg_all = k.sb(T + "lg_all", [128, NT, 36], F32)
            b_lg = [k.buf() for _ in range(NT)]
            xt = [k.sb(f"{T}xt{i}", [128, D], F32) for i in range(3)]
            b_xt = [k.buf() for _ in range(3)]
            xT = [k.sb(f"{T}xT{i}", [128, 8, 128], F32) for i in range(3)]
            b_xTa = [k.buf() for _ in range(3)]
            b_xTb = [k.buf() for _ in range(3)]
            psT = [k.ps(f"{T}psT{i}", [128, 8, 128], F32) for i in range(2)]
            b_psT = [k.buf() for _ in range(2)]
            psL = [k.ps(f"{T}psL{i}", [128, 512], F32) for i in range(2)]
            b_psL = [k.buf() for _ in range(2)]

            def lg0(i):
                s = i % 3
                k.dma("sp", lambda e: e.dma_start(out=xt[s][:], in_=xin[i * 128:(i + 1) * 128, :]),
                      reads=[b_xin], writes=[b_xt[s]])

            def lg1(i):
                s, p = i % 3, i % 2
                for c in range(8):
                    k.op("pe", lambda e: e.transpose(out=psT[p][:, c, :], in_=xt[s][:, c * 128:(c + 1) * 128],
                                                     identity=cm.ident_f),
                         reads=[b_xt[s], cm.b_cf], writes=[b_psT[p]])

            def lg2(i):
                s, p = i % 3, i % 2
                k.op("act", lambda e: e.activation(out=xT[s][:, 0:4, :], in_=psT[p][:, 0:4, :], func=AF.Copy),
                     reads=[b_psT[p]], writes=[b_xTa[s]])
                k.op("dve", lambda e: e.tensor_copy(out=xT[s][:, 4:8, :], in_=psT[p][:, 4:8, :]),
                     reads=[b_psT[p]], writes=[b_xTb[s]])

            def lg3(i):
                s, p = i % 3, i % 2
                for c in range(8):
                    k.op("pe", lambda e: e.matmul(psL[p][:, 0:36], lhsT=xT[s][:, c, :], rhs=wr[:, c, :],
                                                  start=(c == 0), stop=(c == 7)),
                         reads=[b_xTa[s], b_xTb[s], b_wr], writes=[b_psL[p]])

            def lg4(i):
                p = i % 2
                k.op("dve", lambda e: e.tensor_tensor(out=lg_all[:, i, :], in0=psL[p][:, 0:36], in1=rbias[:],
                                                      op=ALU.add),
                     reads=[b_psL[p], b_rbias], writes=[b_lg[i]])

            run_pipeline(NT, [lg0, lg1, lg2, lg3, lg4])

            def st(name, shape, dt=F32):
                return k.sb(T + name, shape, dt)

            lgg = lg_all[:, :, 0:4]
            lge = lg_all[:, :, 4:36]
            gmax = st("gmax", [128, NT])
            ohg = st("ohg", [128, NT, 4])
            egs = st("egs", [128, NT, 4])
            gsum = st("gsum", [128, NT])
            ggate = st("ggate", [128, NT])
            pen = st("pen", [128, NT, 4])
            me = st("me", [128, NT, 32])
            me2 = me
            v1 = st("v1", [128, NT])
            v2 = st("v2", [128, NT])
            sel1 = st("sel1", [128, NT, 32])
            sel2 = st("sel2", [128, NT, 32])
            dd = st("dd", [128, NT])
            p1 = st("p1", [128, NT])
            g1 = st("g1", [128, NT])
            g2 = st("g2", [128, NT])
            A = st("A", [128, NT, 32], BF16)
            dest = st("dest", [128, NT, 32])
            ov = st("ov", [128, NT, 32])
            tmp = st("tmp", [128, NT, 32])
            idxf = st("idxf", [128, NT, 2])
            ovs = st("ovs", [128, NT, 2])
            b_r = k.buf()

            def dv(fn, extra_r=()):
                k.op("dve", fn, reads=[b_r, *extra_r], writes=[b_r])

            def bc(ap, n):
                return ap.unsqueeze(2).to_broadcast([128, NT, n])

            dv(lambda e: e.tensor_reduce(out=gmax[:], in_=lgg, axis=AX.X, op=ALU.max), extra_r=b_lg)
            dv(lambda e: e.tensor_tensor(out=ohg[:], in0=lgg, in1=bc(gmax[:], 4), op=ALU.is_ge))
            dv(lambda e: e.tensor_tensor(out=egs[:], in0=lgg, in1=bc(gmax[:], 4), op=ALU.subtract))
            k.op("act", lambda e: e.activation(out=egs[:], in_=egs[:], func=AF.Exp), reads=[b_r], writes=[b_r])
            dv(lambda e: e.tensor_reduce(out=gsum[:], in_=egs[:], axis=AX.X, op=ALU.add))
            dv(lambda e: e.reciprocal(out=ggate[:], in_=gsum[:]))
            dv(lambda e: e.tensor_scalar(out=pen[:], in0=ohg[:], scalar1=1e30, scalar2=-1e30,
                                         op0=ALU.mult, op1=ALU.add))
            me4 = me[:].rearrange("p t (g j) -> p t g j", g=4)
            lge4 = lge.rearrange("p t (g j) -> p t g j", g=4)
            dv(lambda e: e.tensor_tensor(out=me4, in0=lge4, in1=ohg[:].unsqueeze(3).to_broadcast([128, NT, 4, 8]),
                                         op=ALU.mult))
            dv(lambda e: e.tensor_tensor(out=me4, in0=me4, in1=pen[:].unsqueeze(3).to_broadcast([128, NT, 4, 8]),
                                         op=ALU.add))
            dv(lambda e: e.tensor_reduce(out=v1[:], in_=me[:], axis=AX.X, op=ALU.max))
            dv(lambda e: e.tensor_tensor(out=sel1[:], in0=me[:], in1=bc(v1[:], 32), op=ALU.is_equal))
            dv(lambda e: e.scalar_tensor_tensor(out=me2[:], in0=sel1[:], scalar=-2e30, in1=me[:],
                                                op0=ALU.mult, op1=ALU.add))
            dv(lambda e: e.tensor_reduce(out=v2[:], in_=me2[:], axis=AX.X, op=ALU.max))
            dv(lambda e: e.tensor_tensor(out=sel2[:], in0=me2[:], in1=bc(v2[:], 32), op=ALU.is_equal))
            dv(lambda e: e.tensor_tensor(out=dd[:], in0=v2[:], in1=v1[:], op=ALU.subtract))
            k.op("act", lambda e: e.activation(out=dd[:], in_=dd[:], func=AF.Exp), reads=[b_r], writes=[b_r])
            dv(lambda e: e.tensor_scalar(out=dd[:], in0=dd[:], scalar1=1.0, scalar2=None, op0=ALU.add))
            dv(lambda e: e.reciprocal(out=p1[:], in_=dd[:]))
            dv(lambda e: e.tensor_tensor(out=g1[:], in0=p1[:], in1=ggate[:], op=ALU.mult))
            dv(lambda e: e.tensor_tensor(out=g2[:], in0=ggate[:], in1=g1[:], op=ALU.subtract))
            dv(lambda e: e.tensor_tensor(out=A[:], in0=sel1[:], in1=sel2[:], op=ALU.add))

            psP = k.ps(T + "psP", [128, NT, 32], F32)
            b_psP = k.buf()
            for i in range(NT):
                k.op("pe", lambda e: e.matmul(psP[:, i, :], lhsT=cm.upper_b, rhs=A[:, i, :], start=True,
                                              stop=(i == 0)),
                     reads=[b_r, cm.b_cb], writes=[b_psP])
                for j in range(i):
                    k.op("pe", lambda e: e.matmul(psP[:, i, :], lhsT=cm.ones_b, rhs=A[:, j, :], start=False,
                                                  stop=(j == i - 1)),
                         reads=[b_r, cm.b_cb], writes=[b_psP])

            dv(lambda e: e.tensor_tensor(out=dest[:], in0=psP[:], in1=cm.ebase.unsqueeze(1).to_broadcast([128, NT, 32]),
                                         op=ALU.add), extra_r=[b_psP, cm.b_cf])
            dv(lambda e: e.tensor_scalar(out=ov[:], in0=psP[:], scalar1=float(CAP), scalar2=None, op0=ALU.is_ge),
               extra_r=[b_psP])
            dv(lambda e: e.scalar_tensor_tensor(out=dest[:], in0=ov[:], scalar=1.0e6, in1=dest[:],
                                                op0=ALU.mult, op1=ALU.add))
            for j, sel in enumerate((sel1, sel2)):
                dv(lambda e: e.tensor_tensor(out=tmp[:], in0=sel[:], in1=dest[:], op=ALU.mult))
                dv(lambda e: e.tensor_reduce(out=idxf[:, :, j], in_=tmp[:], axis=AX.X, op=ALU.add))
                dv(lambda e: e.tensor_tensor(out=tmp[:], in0=sel[:], in1=ov[:], op=ALU.mult))
                dv(lambda e: e.tensor_reduce(out=ovs[:, :, j], in_=tmp[:], axis=AX.X, op=ALU.add))
            k.op("dve", lambda e: e.tensor_copy(out=idx_all[:], in_=idxf[:]), reads=[b_r], writes=[b_idx])
            dv(lambda e: e.tensor_scalar(out=ovs[:], in0=ovs[:], scalar1=-1.0, scalar2=1.0, op0=ALU.mult, op1=ALU.add))
            k.op("dve", lambda e: e.tensor_tensor(out=gate_all[:, :, 0], in0=g1[:], in1=ovs[:, :, 0], op=ALU.mult),
                 reads=[b_r], writes=[b_gate])
            k.op("dve", lambda e: e.tensor_tensor(out=gate_all[:, :, 1], in0=g2[:], in1=ovs[:, :, 1], op=ALU.mult),
                 reads=[b_r], writes=[b_gate])

            b_xbuf = k.buf(nowaw=True)
            xs = [xt[0][:], xt[1][:], xt[2][:], xT[0][:].rearrange("p c t -> p (c t)")]
            b_xs = [b_xt[0], b_xt[1], b_xt[2], k.buf()]
            barrier(k)
            for i in range(NT):
                s = i % 4
                k.dma("sp", lambda e: e.dma_start(out=xs[s], in_=xin[i * 128:(i + 1) * 128, :]),
                      reads=[b_xin], writes=[b_xs[s]])
                for j in range(2):
                    k.dma("pool", lambda g: g.indirect_dma_start(
                        out=xbuf, out_offset=bass.IndirectOffsetOnAxis(ap=idx_all[:, i, j:j + 1], axis=0),
                        in_=xs[s], in_offset=None, bounds_check=cm.bound_reg, oob_is_err=False),
                        reads=[b_xs[s], b_idx], writes=[b_xbuf])
            barrier(k)
        k.es = es

        b_ybuf = k.buf(nowaw=True)
        with ExitStack() as es2:
            k.es = es2
            NB = CAP // 128
            NXS = 3
            xe = [k.sb(f"{T}xe{i}", [128, NB, D], BF16) for i in range(NXS)]
            b_xe = [k.buf() for _ in range(NXS)]
            xeT = [k.sb(f"{T}xeT{i}", [128, 8, CAP], BF16) for i in range(NXS)]
            b_xeT = [[k.buf() for _ in range(NB)] for _ in range(NXS)]
            hd = [k.sb(f"{T}hd{i}", [128, 4, CAP], BF16) for i in range(2)]
            b_hd = [[k.buf() for _ in range(4)] for _ in range(2)]
            sil = [k.sb(f"{T}sil{i}", [128, CAP], F32) for i in range(2)]
            b_sil = [k.buf() for _ in range(2)]
            yt = [k.sb(f"{T}yt{i}", [128, NB, D], BF16) for i in range(2)]
            b_yt = [[[k.buf() for _ in range(2)] for _ in range(NB)] for _ in range(2)]
            psX = [k.ps(f"{T}psX{i}", [128, D], BF16) for i in range(2)]
            b_psX = [k.buf() for _ in range(2)]
            psH1 = [k.ps(f"{T}psH1_{i}", [128, 512], F32) for i in range(2)]
            psH3 = [k.ps(f"{T}psH3_{i}", [128, 512], F32) for i in range(2)]
            b_psH1 = [k.buf() for _ in range(2)]
            b_psH3 = [k.buf() for _ in range(2)]
            psY = [k.ps(f"{T}psY{i}", [128, 512], F32) for i in range(2)]
            b_psY = [k.buf() for _ in range(2)]
            cntX = [0]

            def stage_load(e):
                s = e % NXS
                k.dma("sp", lambda q: q.dma_start(
                    out=xe[s][:], in_=xbuf[e * CAP:(e + 1) * CAP, :].rearrange("(b p) d -> p b d", p=128)),
                    reads=[b_xbuf], writes=[b_xe[s]])

            def grp_T(e, blk):
                def f():
                    s = e % NXS
                    px = cntX[0] % 2
                    cntX[0] += 1
                    for c in range(8):
                        k.op("pe", lambda q: q.transpose(out=psX[px][:, c * 128:(c + 1) * 128],
                                                         in_=xe[s][:, blk, c * 128:(c + 1) * 128],
                                                         identity=cm.ident_b),
                             reads=[b_xe[s], cm.b_cb], writes=[b_psX[px]])
                    src = psX[px][:].rearrange("p (c t) -> p c t", c=8)
                    if px == 0:
                        k.op("dve", lambda q: q.tensor_copy(out=xeT[s][:, :, blk * 128:(blk + 1) * 128], in_=src),
                             reads=[b_psX[px]], writes=[b_xeT[s][blk]])
                    else:
                        k.op("act", lambda q: q.activation(out=xeT[s][:, :, blk * 128:(blk + 1) * 128], in_=src,
                                                           func=AF.Copy),
                             reads=[b_psX[px]], writes=[b_xeT[s][blk]])
                return f

            def grp_H(e, m):
                def f():
                    s = e % NXS
                    hs = e % 2
                    ws = e % NWB
                    ph = m % 2
                    for c in range(8):
                        k.op("pe", lambda q: q.matmul(psH1[ph][:, 0:CAP], lhsT=w1t[ws][:, c, m * 128:(m + 1) * 128],
                                                      rhs=xeT[s][:, c, :], start=(c == 0), stop=(c == 7)),
                             reads=[*b_xeT[s], b_w13[ws]], writes=[b_psH1[ph]])
                    k.op("act", lambda q: q.activation(out=sil[ph][:], in_=psH1[ph][:, 0:CAP], func=AF.Silu),
                         reads=[b_psH1[ph]], writes=[b_sil[ph]])
                    for c in range(8):
                        k.op("pe", lambda q: q.matmul(psH3[ph][:, 0:CAP], lhsT=w3t[ws][:, c, m * 128:(m + 1) * 128],
                                                      rhs=xeT[s][:, c, :], start=(c == 0), stop=(c == 7)),
                             reads=[*b_xeT[s], b_w13[ws]], writes=[b_psH3[ph]])
                    k.op("dve", lambda q: q.tensor_tensor(out=hd[hs][:, m, :], in0=sil[ph][:], in1=psH3[ph][:, 0:CAP],
                                                          op=ALU.mult),
                         reads=[b_sil[ph], b_psH3[ph]], writes=[b_hd[hs][m]])
                return f

            def grp_Y(e, blk, half):
                def f():
                    hs = e % 2
                    ws = e % NWB
                    py = (blk * 2 + half) % 2
                    for c in range(4):
                        k.op("pe", lambda q: q.matmul(psY[py][:], lhsT=hd[hs][:, c, blk * 128:(blk + 1) * 128],
                                                      rhs=w2t[ws][:, c, half * 512:(half + 1) * 512],
                                                      start=(c == 0), stop=(c == 3)),
                             reads=[b_hd[hs][c], b_w2[ws]], writes=[b_psY[py]])
                    if half == 0:
                        k.op("act", lambda q: q.activation(out=yt[hs][:, blk, 0:512], in_=psY[py][:], func=AF.Copy),
                             reads=[b_psY[py]], writes=[b_yt[hs][blk][0]])
                    else:
                        k.op("dve", lambda q: q.tensor_copy(out=yt[hs][:, blk, 512:1024], in_=psY[py][:]),
                             reads=[b_psY[py]], writes=[b_yt[hs][blk][1]])
                return f

            def store_Y(e):
                hs = e % 2
                k.dma("sp", lambda q: q.dma_start(
                    out=ybuf[e * CAP:(e + 1) * CAP, :].rearrange("(b p) d -> p b d", p=128), in_=yt[hs][:]),
                    reads=[bb for blk in b_yt[hs] for bb in blk], writes=[b_ybuf])

            for e0 in range(min(NXS, NE)):
                stage_load(e0)
            for blk in range(NB):
                grp_T(0, blk)()
            for blk in range(NB):
                grp_T(1, blk)()
            for m in range(4):
                grp_H(0, m)()
            for e in range(NE):
                Tg = [grp_T(e + 2, blk) for blk in range(NB)] if e + 2 < NE else []
                Hg = [grp_H(e + 1, m) for m in range(4)] if e + 1 < NE else []
                Yg = [grp_Y(e, blk, half) for blk in range(NB) for half in range(2)]
                order = []
                ti = hi = 0
                for yi, yg in enumerate(Yg):
                    order.append(yg)
                    if yi % 2 == 0 and hi < len(Hg):
                        order.append(Hg[hi]); hi += 1
                    if yi % 2 == 1 and ti < len(Tg):
                        order.append(Tg[ti]); ti += 1
                order += Hg[hi:] + Tg[ti:]
                for g_ in order:
                    g_()
                store_Y(e)
                if e + NWB < NE:
                    load_weights(e + NWB)
                if e + NXS < NE:
                    stage_load(e + NXS)
            barrier(k)
        k.es = es

        with ExitStack() as es2:
            k.es = es2
            NY, NA = 3, 6
            y1 = [k.sb(f"{T}y1_{i}", [128, D], BF16) for i in range(NY)]
            y2 = [k.sb(f"{T}y2_{i}", [128, D], BF16) for i in range(NY)]
            b_y1 = [k.buf() for _ in range(NY)]
            b_y2 = [k.buf() for _ in range(NY)]
            xr = [k.sb(f"{T}xr_{i}", [128, D], F32) for i in range(NY)]
            b_xr = [k.buf() for _ in range(NY)]
            acc = [k.sb(f"{T}acc_{i}", [128, D], F32) for i in range(NA)]
            b_acc = [k.buf() for _ in range(NA)]
            ot = [k.sb(f"{T}ot_{i}", [128, D], F32) for i in range(3)]
            b_ot = [k.buf() for _ in range(3)]
            for i in range(NY):
                k.op("pool", lambda q: q.memset(y1[i][:], 0.0), writes=[b_y1[i]])
                k.op("pool", lambda q: q.memset(y2[i][:], 0.0), writes=[b_y2[i]])
            lnp = {}

            def cA(i):
                s, sa = i % NY, i % NA
                k.dma("sp", lambda q: q.dma_start(out=xr[s][:], in_=xin[i * 128:(i + 1) * 128, :]),
                      reads=[b_xin], writes=[b_xr[s]])
                k.dma("pool", lambda g: g.indirect_dma_start(
                    out=y1[s][:], out_offset=None, in_=ybuf,
                    in_offset=bass.IndirectOffsetOnAxis(ap=idx_all[:, i, 0:1], axis=0),
                    bounds_check=cm.bound_reg, oob_is_err=False), reads=[b_ybuf, b_idx], writes=[b_y1[s]])
                k.dma("pool", lambda g: g.indirect_dma_start(
                    out=y2[s][:], out_offset=None, in_=ybuf,
                    in_offset=bass.IndirectOffsetOnAxis(ap=idx_all[:, i, 1:2], axis=0),
                    bounds_check=cm.bound_reg, oob_is_err=False), reads=[b_ybuf, b_idx], writes=[b_y2[s]])

            def cB(i):
                s, sa = i % NY, i % NA
                k.op("act", lambda q: q.activation(out=acc[sa][:], in_=xr[s][:], func=AF.Copy, scale=ALPHA),
                     reads=[b_xr[s]], writes=[b_acc[sa]])
                k.op("dve", lambda q: q.scalar_tensor_tensor(out=acc[sa][:], in0=y1[s][:], scalar=gate_all[:, i, 0:1],
                                                             in1=acc[sa][:], op0=ALU.mult, op1=ALU.add),
                     reads=[b_y1[s], b_gate, b_acc[sa]], writes=[b_acc[sa]])
                k.op("dve", lambda q: q.scalar_tensor_tensor(out=acc[sa][:], in0=y2[s][:], scalar=gate_all[:, i, 1:2],
                                                             in1=acc[sa][:], op0=ALU.mult, op1=ALU.add),
                     reads=[b_y2[s], b_gate, b_acc[sa]], writes=[b_acc[sa]])
                lnp[i] = layer_norm_parts(k, T + "ln", acc[sa], b_acc[sa], ot[i % 3], b_ot[i % 3], gam, bet, b_gb, sa)

            def cC(i):
                lnp[i][0]()

            def cD(i):
                lnp[i][1]()

            def cE(i):
                lnp[i][2]()
                k.dma("sp", lambda q: q.dma_start(out=xout[i * 128:(i + 1) * 128, :], in_=ot[i % 3][:]),
                      reads=[b_ot[i % 3]], writes=[b_xout])

            run_pipeline(NT, [cA, cB, cC, cD, cE])
            barrier(k)
        k.es = es
    k.es = es_outer


_ln_scratch = {}


def layer_norm_parts(k, tag, acc, b_acc, ot, b_ot, gam, bet, b_gb, s):
    key = (tag, s, id(k))
    if key not in _ln_scratch:
        _ln_scratch[key] = (k.sb(f"{tag}st{s}", [128, 2, 6], F32), k.sb(f"{tag}mv{s}", [128, 2], F32),
                            k.sb(f"{tag}rs{s}", [128, 1], F32), k.buf())
    stt, mv, rs, b_s = _ln_scratch[key]
    mhalf = k.mhalf

    def part_a():
        for h in range(2):
            k.op("dve", lambda q: q.bn_stats(out=stt[:, h, :], in_=acc[:, h * 512:(h + 1) * 512]),
                 reads=[b_acc], writes=[b_s])
        k.op("dve", lambda q: q.bn_aggr(out=mv[:], in_=stt[:].rearrange("p a b -> p (a b)")), reads=[b_s], writes=[b_s])
        k.op("dve", lambda q: q.tensor_scalar(out=rs[:], in0=mv[:, 1:2], scalar1=LN_EPS, scalar2=None, op0=ALU.add),
             reads=[b_s], writes=[b_s])
        k.op("pool", lambda q: q.tensor_tensor(out=rs[:], in0=rs[:], in1=mhalf[:], op=ALU.pow),
             reads=[b_s], writes=[b_s])

    def part_b():
        k.op("dve", lambda q: q.tensor_scalar(out=acc[:], in0=acc[:], scalar1=mv[:, 0:1], scalar2=rs[:, 0:1],
                                              op0=ALU.subtract, op1=ALU.mult),
             reads=[b_s, b_acc], writes=[b_acc])
        k.op("dve", lambda q: q.tensor_tensor(out=acc[:], in0=acc[:], in1=gam[:], op=ALU.mult),
             reads=[b_acc, b_gb], writes=[b_acc])

    def part_c():
        k.op("dve", lambda q: q.tensor_tensor(out=ot[:], in0=acc[:], in1=bet[:], op=ALU.add),
             reads=[b_acc, b_gb], writes=[b_ot])

    return part_a, part_b, part_c


def layer_norm_tile(k, tag, acc, b_acc, ot, b_ot, gam, bet, b_gb, s):
    for part in layer_norm_parts(k, tag, acc, b_acc, ot, b_ot, gam, bet, b_gb, s):
        part()


class Deferred:
    def __init__(self):
        self.q = []
        self.it = 0

    def add(self, delay, fn):
        self.q.append((self.it + delay, fn))

    def tick(self, it):
        self.it = it + 1
        keep = []
        for due, fn in self.q:
            if due <= it:
                fn()
            else:
                keep.append((due, fn))
        self.q = keep

    def flush(self):
        while self.q:
            q, self.q = self.q, []
            for _, fn in q:
                fn()


def run_pipeline(n, stages, deferred=None):
    S = len(stages)
    for it in range(n + S - 1):
        if deferred is not None:
            deferred.it = it
        for st in range(S):
            i = it - st
            if 0 <= i < n:
                stages[st](i)
        if deferred is not None:
            deferred.tick(it)
    if deferred is not None:
        deferred.flush()


def barrier(k):
    deps = {}
    for n in k.engs:
        if k.cnt[n] > 0:
            s = k.sem[n]
            deps[id(s)] = (s, k.cnt[n])
    for q, ring in k.ring.items():
        for s, v in ring:
            if v > 0:
                deps[id(s)] = (s, v)
    for n in k.engs:
        k._wait(n, dict(deps))


def build_program(phases=("rglru", "moe0", "attn", "moe1")):
    nc = bass.Bass("TRN2", target_bir_lowering=False)

    need_moe = any(p.startswith("moe") for p in phases)

    def inp(name, shape):
        if name.startswith("moe_") and not need_moe:
            return None
        if name.startswith("rec_") and "rglru" not in phases:
            return None
        if name.startswith("att_") and "attn" not in phases:
            return None
        return nc.dram_tensor(name, list(shape), F32, kind="ExternalInput").ap()

    x = inp("x", [TOK, D])
    consts = inp("consts", [128, NCONST])
    moe_w_group = inp("moe_w_group", [2, D, 4])
    moe_b_group = inp("moe_b_group", [2, 4])
    moe_w_expert = inp("moe_w_expert", [2, D, NE])
    moe_b_expert = inp("moe_b_expert", [2, NE])
    moe_w1 = inp("moe_w1", [2, NE, D, DE])
    moe_w3 = inp("moe_w3", [2, NE, D, DE])
    moe_w2 = inp("moe_w2", [2, NE, DE, D])
    ln_g = inp("ln_g", [2, 2, D])
    ln_b = inp("ln_b", [2, 2, D])
    rec_w_in = inp("rec_w_in", [D, 2 * D_RNN])
    rec_cdiag = inp("rec_cdiag", [RC, NRC, 4, RC])
    rec_prm = inp("rec_prm", [RC, 4, NRC])
    rec_w_r = inp("rec_w_r", [NRC, RC, RC])
    rec_w_i = inp("rec_w_i", [NRC, RC, RC])
    rec_w_out = inp("rec_w_out", [D_RNN, D])
    att_w_qkv = inp("att_w_qkv", [D, 1536])
    att_sinks = inp("att_sinks", [NH])
    att_w_o = inp("att_w_o", [D, D])
    att_bias = inp("att_bias", [128, NH, 256])
    out = nc.dram_tensor("out", [TOK, D], F32, kind="ExternalOutput").ap()
    with ExitStack() as es:
        k = K(nc, es)
        cm = Common(k, consts)
        xbuf = k.dram("xbuf", [NSLOT, D], BF16)
        ybuf = k.dram("ybuf", [NSLOT, D], BF16)
        cur, b_cur = x, k.buf("x")
        b_out = k.buf("out", nowaw=True)
        for pi, ph in enumerate(phases):
            last = pi == len(phases) - 1
            if last:
                nxt, b_nxt = out, b_out
            else:
                nxt, b_nxt = k.dram(f"act{pi}", [TOK, D], F32), k.buf(f"act{pi}", nowaw=True)
            if ph == "rglru":
                rglru_phase(k, cm, "r0", cur, b_cur, nxt, b_nxt, rec_w_in, rec_cdiag, rec_prm, rec_w_r, rec_w_i,
                            rec_w_out, ln_g[0, 0], ln_b[0, 0])
            elif ph == "attn":
                attn_phase(k, cm, "a1", cur, b_cur, nxt, b_nxt, att_w_qkv, att_sinks, att_w_o, att_bias,
                           ln_g[1, 0], ln_b[1, 0])
            elif ph in ("moe0", "moe1"):
                l = int(ph[3])
                moe_phase(k, cm, f"m{l}", cur, b_cur, nxt, b_nxt, moe_w_group[l], moe_b_group[l], moe_w_expert[l],
                          moe_b_expert[l], moe_w1[l], moe_w3[l], moe_w2[l], ln_g[l, 1], ln_b[l, 1], xbuf, ybuf)
            cur, b_cur = nxt, b_nxt
        if DBG["stop"]:
            k.dma("sp", lambda e: e.dma_start(out=out[0:128, :], in_=x[0:128, :]), writes=[b_out])
        k.finish([b_out])
        barrier(k)
    return nc


def host_inputs(inputs):
    f = lambda a: np.ascontiguousarray(np.asarray(a, dtype=np.float32))
    d = {}
    d["consts"] = make_consts()
    for n in ("moe_w_group", "moe_b_group", "moe_w_expert", "moe_b_expert", "moe_w1", "moe_w3", "moe_w2",
              "ln_g", "ln_b"):
        d[n] = f(inputs[n])
    d["rec_w_in"] = f(inputs["rec_w_in"][0])
    cw = np.asarray(inputs["rec_conv_w"][0], np.float32)
    cdiag = np.zeros((RC, NRC, 4, RC), np.float32)
    pp = np.arange(RC)
    for c in range(NRC):
        for j in range(4):
            cdiag[pp, c, j, pp] = cw[j, c * RC:(c + 1) * RC]
    d["rec_cdiag"] = cdiag
    prm = np.stack([np.asarray(inputs[n][0], np.float32).reshape(NRC, RC).T
                    for n in ("rec_conv_b", "rec_b_r", "rec_b_i", "rec_lambda")], axis=1)
    d["rec_prm"] = f(prm)
    d["rec_w_r"] = f(inputs["rec_w_r"][0])
    d["rec_w_i"] = f(inputs["rec_w_i"][0])
    d["rec_w_out"] = f(inputs["rec_w_out"][0])
    d["att_w_qkv"] = f(inputs["att_w_qkv"][0])
    d["att_sinks"] = f(inputs["att_sinks"][0])
    d["att_w_o"] = f(inputs["att_w_o"][0])
    d["att_bias"] = make_att_bias()
    return d


def make_att_bias():
    slopes = 2.0 ** (-8.0 * np.arange(1, NH + 1, dtype=np.float32) / NH)
    qi = np.arange(128)[:, None]
    sj = np.arange(256)[None, :]
    dist = qi - sj + 128
    valid = (dist >= 0) & (dist < 128)
    b = np.where(valid[:, None, :], -slopes[None, :, None] * dist[:, None, :].astype(np.float32), -30000.0)
    return np.ascontiguousarray(b.astype(np.float32))


_PROG = {}


def kernel(**inputs):
    key = "full"
    if key not in _PROG:
        _PROG[key] = build_program()
    nc = _PROG[key]
    shared = host_inputs(inputs)
    x = np.ascontiguousarray(np.asarray(inputs["x"], np.float32)).reshape(NCORES, TOK, D)
    in_maps = [dict(shared, x=x[c]) for c in range(NCORES)]
    res = run_bass_kernel_spmd(nc, in_maps, core_ids=list(range(NCORES)))
    out = np.stack([np.asarray(r["out"]) for r in res.results], axis=0)
    return out.reshape(16, SEQ, D).astype(np.float32)


GELU_K = 0.7978845608028654
DBG = {"stop": 0}


class _Stop(Exception):
    pass

TCH = 1024


def rglru_phase(k, cm, T, xin, b_xin, xout, b_xout, w_in, cdiag, prm_d, w_r, w_i, w_out, ln_g, ln_b):
    nc = k.nc
    es_outer = k.es
    with ExitStack() as es:
        k.es = es
        P = RC
        gam = k.sb(T + "gam", [128, D], F32)
        bet = k.sb(T + "bet", [128, D], F32)
        b_gb = k.buf()
        k.dma("sp", lambda e: e.dma_start(out=gam[:], in_=ln_g.partition_broadcast(128)), writes=[b_gb])
        k.dma("sp", lambda e: e.dma_start(out=bet[:], in_=ln_b.partition_broadcast(128)), writes=[b_gb])
        wo_full = k.sb(T + "wo", [128, NRC, D], BF16)
        b_wo = k.buf()
        k.op("pool", lambda e: e.memset(wo_full[:], 0.0), writes=[b_wo])
        wo = wo_full[0:P]
        k.dma("pool", lambda g: g.dma_start(out=wo[:], in_=w_out.rearrange("(c p) n -> p c n", p=P)), writes=[b_wo])
        cd_full = k.sb(T + "cd", [128, NRC, 4, P], BF16)
        b_cd = k.buf()
        k.op("pool", lambda e: e.memset(cd_full[:], 0.0), writes=[b_cd])
        cd = cd_full[0:P]
        for c in range(NRC):
            k.dma("pool", lambda g: g.dma_start(out=cd[:, c, :, :], in_=cdiag[:, c, :, :]), writes=[b_cd])
        wrt_full = k.sb(T + "wrt", [128, NRC, P], BF16)
        b_wri = k.buf()
        k.op("pool", lambda e: e.memset(wrt_full[:], 0.0), writes=[b_wri])
        wrt = wrt_full[0:P]
        wit_full = k.sb(T + "wit", [128, NRC, P], BF16)
        k.op("pool", lambda e: e.memset(wit_full[:], 0.0), writes=[b_wri])
        wit = wit_full[0:P]
        k.dma("pool", lambda g: g.dma_start(out=wrt[:], in_=w_r.rearrange("n k j -> k n j")), writes=[b_wri])
        k.dma("pool", lambda g: g.dma_start(out=wit[:], in_=w_i.rearrange("n k j -> k n j")), writes=[b_wri])
        prm = k.sb(T + "prm", [P, 4, NRC], F32)
        b_prm = k.buf()
        k.dma("sp", lambda e: e.dma_start(out=prm[:], in_=prm_d), writes=[b_prm])
        der = k.sb(T + "der", [P, 5, NRC], F32)
        b_der = k.buf()
        k.op("dve", lambda e: e.tensor_scalar(out=der[:, 0:2, :], in0=prm[:, 1:3, :], scalar1=0.5, scalar2=None,
                                              op0=ALU.mult), reads=[b_prm], writes=[b_der])
        k.op("act", lambda e: e.activation(out=der[:, 4, :], in_=prm[:, 3, :], func=AF.Exp, scale=-1.0),
             reads=[b_prm, b_der], writes=[b_der])
        k.op("act", lambda e: e.activation(out=der[:, 4, :], in_=der[:, 4, :], func=AF.Ln, bias=1.0),
             reads=[b_der], writes=[b_der])
        k.op("dve", lambda e: e.tensor_scalar(out=der[:, 2, :], in0=der[:, 4, :], scalar1=-4.0, scalar2=None,
                                              op0=ALU.mult), reads=[b_der], writes=[b_der])
        k.op("dve", lambda e: e.tensor_scalar(out=der[:, 3, :], in0=der[:, 4, :], scalar1=-8.0, scalar2=None,
                                              op0=ALU.mult), reads=[b_der], writes=[b_der])

        if DBG["stop"] == 1:
            barrier(k); k.es = es_outer; return
        cvb = k.sb(T + "cvb", [P, NRC], F32)
        k.op("dve", lambda e: e.tensor_copy(out=cvb[:], in_=prm[:, 0, :]), reads=[b_prm], writes=[b_prm])
        xT = k.sb(T + "xT", [128, 8, TCH], BF16)
        b_xT = k.buf()
        YT_full = k.sb(T + "YT", [128, NRC, TCH], BF16)
        k.op("pool", lambda e: e.memset(YT_full[:], 0.0))
        YT = YT_full[0:P]
        b_YT = [k.buf() for _ in range(NRC)]
        xrc = k.sb(T + "xrc", [P, NRC, 4], F32)
        hc = k.sb(T + "hc", [P, NRC], F32)
        b_xrc = [k.buf() for _ in range(NRC)]
        b_hc = [k.buf() for _ in range(NRC)]
        xt = [k.sb(f"{T}xt{i}", [128, D], F32) for i in range(2)]
        b_xt = [k.buf() for _ in range(2)]
        xtb = [k.sb(f"{T}xtb{i}", [128, D], BF16) for i in range(2)]
        b_xtb = [k.buf() for _ in range(2)]
        wg = [[k.sb(f"{T}wg{i}_{j}", [128, 8, P], BF16) for j in range(2)] for i in range(2)]
        b_wg = [k.buf(nowaw=True) for _ in range(2)]
        G = [k.sb(f"{T}G{i}", [P, TCH], F32) for i in range(2)]
        XC = [k.sb(f"{T}XC{i}", [P, TCH], F32) for i in range(2)]
        TR = [k.sb(f"{T}TR{i}", [P, TCH], F32) for i in range(2)]
        TI = [k.sb(f"{T}TI{i}", [P, TCH], F32) for i in range(2)]
        NQ = TCH // 512
        b_G = [[k.buf() for _ in range(NQ)] for _ in range(2)]
        b_XC = [[k.buf() for _ in range(NQ)] for _ in range(2)]
        b_TR = [[k.buf() for _ in range(NQ)] for _ in range(2)]
        b_TI = [[k.buf() for _ in range(NQ)] for _ in range(2)]
        AA = k.sb(T + "AA", [P, TCH], F32)
        SQ = k.sb(T + "SQ", [P, TCH], F32)
        S2 = k.sb(T + "S2", [P, TCH], F32)
        HH = SQ
        XR_full = k.sb(T + "XR", [128, TCH + 4], BF16)
        k.op("pool", lambda e: e.memset(XR_full[:], 0.0))
        XR = XR_full[0:P]
        XCb_full = k.sb(T + "XCb", [128, TCH], BF16)
        k.op("pool", lambda e: e.memset(XCb_full[:], 0.0))
        XCb = XCb_full[0:P]
        XRf = k.sb(T + "XRf", [P, TCH + 4], F32)
        b_XRf = k.buf()
        XRb_full = k.sb(T + "XRb", [128, TCH + 4], BF16)
        k.op("pool", lambda e: e.memset(XRb_full[:], 0.0))
        XRb = XRb_full[0:P]
        b_XRb = k.buf()
        b_AA, b_SQ, b_S2, b_XR = [k.buf() for _ in range(4)]
        b_XCb = [k.buf() for _ in range(NQ)]
        NACC = 4
        acc = [k.sb(f"{T}acc{i}", [128, D], F32) for i in range(NACC)]
        b_acc = [k.buf() for _ in range(NACC)]
        psTb = k.ps(T + "psTb", [128, D], BF16)
        b_psTb = k.buf()
        pA = [k.ps(f"{T}pA{i}", [128, 512], F32) for i in range(5)]
        b_pA = [k.buf() for _ in range(5)]
        pO = [k.ps(f"{T}pO{i}", [128, 512], F32) for i in range(2)]
        b_pO = [k.buf() for _ in range(2)]
        barrier(k)
        npa = [0]

        def nxt():
            npa[0] += 1
            return npa[0] % 5

        def qs(q):
            return slice(q * 512, (q + 1) * 512)

        def load_wchunk(c):
            s = c % 2
            k.dma("pool", lambda g: g.dma_start(out=wg[s][0][:],
                                                in_=w_in[:, c * P:(c + 1) * P].rearrange("(c p) n -> p c n", p=128)),
                  writes=[b_wg[s]])
            k.dma("pool", lambda g: g.dma_start(out=wg[s][1][:],
                                                in_=w_in[:, D_RNN + c * P:D_RNN + (c + 1) * P].rearrange(
                                                    "(c p) n -> p c n", p=128)),
                  writes=[b_wg[s]])

        for sq in range(2):
            for th in range(SEQ // TCH):
                tok0 = sq * SEQ + th * TCH
                load_wchunk(0)
                for tl in range(TCH // 128):
                    s = tl % 2
                    r0 = tok0 + tl * 128
                    k.dma("sp", lambda e: e.dma_start(out=xt[s][:], in_=xin[r0:r0 + 128, :]),
                          reads=[b_xin], writes=[b_xt[s]])
                    k.op("act", lambda e: e.activation(out=xtb[s][:], in_=xt[s][:], func=AF.Copy),
                         reads=[b_xt[s]], writes=[b_xtb[s]])
                    for c in range(8):
                        k.op("pe", lambda e: e.transpose(out=psTb[:, c * 128:(c + 1) * 128],
                                                         in_=xtb[s][:, c * 128:(c + 1) * 128], identity=cm.ident_b),
                             reads=[b_xtb[s], cm.b_cb], writes=[b_psTb])
                    k.op("dve", lambda e: e.tensor_copy(out=xT[:, :, tl * 128:(tl + 1) * 128],
                                                        in_=psTb[:].rearrange("p (c t) -> p c t", c=8)),
                         reads=[b_psTb], writes=[b_xT])

                def stA(c):
                    u = c % 2
                    ws = c % 2
                    if c + 1 < NRC:
                        load_wchunk(c + 1)
                    for q in range(NQ):
                        pa = nxt()
                        for kk in range(8):
                            k.op("pe", lambda e: e.matmul(pA[pa][0:P, :], lhsT=wg[ws][0][:, kk, :], rhs=xT[:, kk, qs(q)],
                                                          start=(kk == 0), stop=(kk == 7)),
                                 reads=[b_wg[ws], b_xT], writes=[b_pA[pa]])
                        k.op("act", lambda e: e.activation(out=G[u][:, qs(q)], in_=pA[pa][0:P, :], func=AF.Copy),
                             reads=[b_pA[pa]], writes=[b_G[u][q]])
                    for q in range(NQ):
                        pa = nxt()
                        for kk in range(8):
                            k.op("pe", lambda e: e.matmul(pA[pa][0:P, :], lhsT=wg[ws][1][:, kk, :],
                                                          rhs=xT[:, kk, qs(q)], start=(kk == 0), stop=(kk == 7)),
                                 reads=[b_wg[ws], b_xT], writes=[b_pA[pa]])
                        k.op("dve", lambda e: e.tensor_copy(out=XRf[:, 4 + q * 512:4 + (q + 1) * 512], in_=pA[pa][0:P, :]),
                             reads=[b_pA[pa]], writes=[b_XRf])
                    yield
                    if th == 0:
                        k.op("dve", lambda e: e.memset(XRf[:, 0:4], 0.0), writes=[b_XRf])
                        yield
                    else:
                        k.op("dve", lambda e: e.tensor_copy(out=XRf[:, 0:4], in_=xrc[:, c, :]),
                             reads=[b_xrc[c]], writes=[b_XRf])
                        yield
                    k.op("dve", lambda e: e.tensor_copy(out=xrc[:, c, :], in_=XRf[:, TCH:TCH + 4]),
                         reads=[b_XRf], writes=[b_xrc[c]])
                    yield
                    k.op("dve", lambda e: e.tensor_copy(out=XR[:, 0:TCH + 4], in_=XRf[:, 0:TCH + 4]),
                         reads=[b_XRf], writes=[b_XR])
                    yield
                    k.op("act", lambda e: e.activation(out=XRb[:, 0:TCH + 2], in_=XRf[:, 1:TCH + 3], func=AF.Copy),
                         reads=[b_XRf], writes=[b_XRb])
                    yield
                    for q in range(NQ):
                        pa = nxt()
                        for j in range(4):
                            o = 1 + q * 512 + j
                            src = XR_full[:, o:o + 512] if o % 2 == 0 else XRb_full[:, o - 1:o - 1 + 512]
                            k.op("pe", lambda e: e.matmul(pA[pa][0:P, :], lhsT=cd_full[:, c, j, :], rhs=src,
                                                          start=(j == 0), stop=(j == 3)),
                                 reads=[b_cd, b_XR, b_XRb], writes=[b_pA[pa]])
                            yield
                        k.op("dve", lambda e: e.tensor_scalar(out=XC[u][:, qs(q)], in0=pA[pa][0:P, :],
                                                              scalar1=cvb[:, c:c + 1], scalar2=None, op0=ALU.add),
                             reads=[b_pA[pa], b_prm], writes=[b_XC[u][q]])
                        yield
                        k.op("act", lambda e: e.activation(out=XCb[:, qs(q)], in_=XC[u][:, qs(q)], func=AF.Copy),
                             reads=[b_XC[u][q]], writes=[b_XCb[q]])
                        yield
                    for q in range(NQ):
                        pa = nxt()
                        k.op("pe", lambda e: e.matmul(pA[pa][0:P, :], lhsT=wrt_full[:, c, :], rhs=XCb_full[:, qs(q)],
                                                      start=True, stop=True),
                             reads=[b_wri, b_XCb[q]], writes=[b_pA[pa]])
                        yield
                        k.op("act", lambda e: e.activation(out=TR[u][:, qs(q)], in_=pA[pa][0:P, :], func=AF.Tanh,
                                                           scale=0.5, bias=der[:, 0, c:c + 1]),
                             reads=[b_pA[pa], b_der], writes=[b_TR[u][q]])
                        yield
                        pa = nxt()
                        k.op("pe", lambda e: e.matmul(pA[pa][0:P, :], lhsT=wit_full[:, c, :], rhs=XCb_full[:, qs(q)],
                                                      start=True, stop=True),
                             reads=[b_wri, b_XCb[q]], writes=[b_pA[pa]])
                        yield
                        k.op("act", lambda e: e.activation(out=TI[u][:, qs(q)], in_=pA[pa][0:P, :], func=AF.Tanh,
                                                           scale=0.5, bias=der[:, 1, c:c + 1]),
                             reads=[b_pA[pa], b_der], writes=[b_TI[u][q]])
                        yield

                def stB(c):
                    u = c % 2
                    k.op("act", lambda e: e.activation(out=S2[:], in_=G[u][:], func=AF.Square),
                         reads=b_G[u], writes=[b_S2])
                    yield
                    k.op("dve", lambda e: e.tensor_scalar(out=S2[:], in0=S2[:], scalar1=0.044715, scalar2=1.0,
                                                          op0=ALU.mult, op1=ALU.add), reads=[b_S2], writes=[b_S2])
                    yield
                    k.op("dve", lambda e: e.tensor_tensor(out=S2[:], in0=S2[:], in1=G[u][:], op=ALU.mult),
                         reads=[b_S2, *b_G[u]], writes=[b_S2])
                    yield
                    k.op("act", lambda e: e.activation(out=AA[:], in_=TR[u][:], func=AF.Exp, scale=der[:, 2, c:c + 1],
                                                       bias=der[:, 2, c:c + 1]),
                         reads=[*b_TR[u], b_der], writes=[b_AA])
                    yield
                    k.op("act", lambda e: e.activation(out=SQ[:], in_=TR[u][:], func=AF.Exp, scale=der[:, 3, c:c + 1],
                                                       bias=der[:, 3, c:c + 1]),
                         reads=[*b_TR[u], b_der], writes=[b_SQ])
                    yield
                    k.op("act", lambda e: e.activation(out=S2[:], in_=S2[:], func=AF.Tanh, scale=GELU_K),
                         reads=[b_S2], writes=[b_S2])
                    yield
                    k.op("act", lambda e: e.activation(out=SQ[:], in_=SQ[:], func=AF.Sqrt, scale=-1.0, bias=1.0),
                         reads=[b_SQ], writes=[b_SQ])
                    yield
                    k.op("dve", lambda e: e.scalar_tensor_tensor(out=S2[:], in0=S2[:], scalar=1.0, in1=G[u][:],
                                                                 op0=ALU.add, op1=ALU.mult),
                         reads=[b_S2, *b_G[u]], writes=[b_S2])
                    yield
                    k.op("dve", lambda e: e.scalar_tensor_tensor(out=TI[u][:], in0=TI[u][:], scalar=1.0, in1=XC[u][:],
                                                                 op0=ALU.add, op1=ALU.mult),
                         reads=[*b_TI[u], *b_XC[u]], writes=b_TI[u])
                    yield
                    k.op("dve", lambda e: e.tensor_tensor(out=TI[u][:], in0=TI[u][:], in1=SQ[:], op=ALU.mult),
                         reads=[*b_TI[u], b_SQ], writes=b_TI[u])
                    yield
                    init = 0.0 if th == 0 else hc[:, c:c + 1]
                    k.op("dve", lambda e: e.tensor_tensor_scan(out=HH[:], data0=AA[:], data1=TI[u][:], initial=init,
                                                               op0=ALU.mult, op1=ALU.add),
                         reads=[b_AA, *b_TI[u], b_hc[c], b_SQ], writes=[b_SQ])
                    yield
                    k.op("dve", lambda e: e.tensor_copy(out=hc[:, c:c + 1], in_=HH[:, TCH - 1:TCH]),
                         reads=[b_SQ], writes=[b_hc[c]])
                    yield
                    k.op("dve", lambda e: e.scalar_tensor_tensor(out=YT[:, c, :], in0=S2[:], scalar=0.25, in1=HH[:],
                                                                 op0=ALU.mult, op1=ALU.mult),
                         reads=[b_S2, b_SQ], writes=[b_YT[c]])
                    yield

                def drive(gens):
                    live = list(gens)
                    while live:
                        for g_ in list(live):
                            try:
                                next(g_)
                            except StopIteration:
                                live.remove(g_)

                if DBG.get("var") == "seq":
                    for c in range(NRC):
                        drive([stA(c)])
                        drive([stB(c)])
                else:
                    for t in range(NRC + 1):
                        gens = []
                        if t < NRC:
                            ga = stA(t)
                            next(ga)
                            gens.append(ga)
                        if t >= 1:
                            gens.append(stB(t - 1))
                        drive(gens)

                lnp = {}

                def p0(tl):
                    s, sa = tl % 2, tl % NACC
                    r0 = tok0 + tl * 128
                    k.dma("sp", lambda e: e.dma_start(out=xt[s][:], in_=xin[r0:r0 + 128, :]),
                          reads=[b_xin], writes=[b_xt[s]])
                    k.op("act", lambda e: e.activation(out=acc[sa][:], in_=xt[s][:], func=AF.Copy, scale=ALPHA),
                         reads=[b_xt[s]], writes=[b_acc[sa]])

                def p1(tl):
                    sa = tl % NACC
                    for h in range(2):
                        for c in range(NRC):
                            k.op("pe", lambda e: e.matmul(pO[h][:], lhsT=YT_full[:, c, tl * 128:(tl + 1) * 128],
                                                          rhs=wo_full[:, c, h * 512:(h + 1) * 512],
                                                          start=(c == 0), stop=(c == NRC - 1)),
                                 reads=[b_YT[c], b_wo], writes=[b_pO[h]])
                        k.op("dve", lambda e: e.tensor_tensor(out=acc[sa][:, h * 512:(h + 1) * 512],
                                                              in0=acc[sa][:, h * 512:(h + 1) * 512], in1=pO[h][:],
                                                              op=ALU.add),
                             reads=[b_pO[h], b_acc[sa]], writes=[b_acc[sa]])
                    lnp[tl] = layer_norm_parts(k, T + "ln", acc[sa], b_acc[sa], acc[sa], b_acc[sa], gam, bet, b_gb, sa)

                def p2(tl):
                    lnp[tl][0]()

                def p3(tl):
                    sa = tl % NACC
                    r0 = tok0 + tl * 128
                    lnp[tl][1]()
                    lnp[tl][2]()
                    k.dma("sp", lambda e: e.dma_start(out=xout[r0:r0 + 128, :], in_=acc[sa][:]),
                          reads=[b_acc[sa]], writes=[b_xout])

                run_pipeline(TCH // 128, [p0, p1, p2, p3])
        barrier(k)
    k.es = es_outer


def attn_phase(k, cm, T, xin, b_xin, xout, b_xout, w_qkv, sinks, w_o, att_bias, ln_g, ln_b):
    es_outer = k.es
    HALF = 1024
    with ExitStack() as es:
        k.es = es
        gam = k.sb(T + "gam", [128, D], F32)
        bet = k.sb(T + "bet", [128, D], F32)
        b_gb = k.buf()
        k.dma("sp", lambda e: e.dma_start(out=gam[:], in_=ln_g.partition_broadcast(128)), writes=[b_gb])
        k.dma("sp", lambda e: e.dma_start(out=bet[:], in_=ln_b.partition_broadcast(128)), writes=[b_gb])
        wq = k.sb(T + "wq", [128, 8, 1024], BF16)
        wk = k.sb(T + "wk", [128, 8, 256], BF16)
        wv = k.sb(T + "wv", [128, 8, 256], BF16)
        wo = k.sb(T + "wo", [128, 8, 1024], BF16)
        b_w = k.buf(nowaw=True)
        k.dma("pool", lambda g: g.dma_start(out=wq[:], in_=w_qkv[:, 0:1024].rearrange("(c p) n -> p c n", p=128)),
              writes=[b_w])
        k.dma("pool", lambda g: g.dma_start(out=wk[:], in_=w_qkv[:, 1024:1280].rearrange("(c p) n -> p c n", p=128)),
              writes=[b_w])
        k.dma("pool", lambda g: g.dma_start(out=wv[:], in_=w_qkv[:, 1280:1536].rearrange("(c p) n -> p c n", p=128)),
              writes=[b_w])
        k.dma("pool", lambda g: g.dma_start(out=wo[:], in_=w_o.rearrange("(c p) n -> p c n", p=128)), writes=[b_w])
        sk = k.sb(T + "sk", [128, NH], F32)
        bt = k.sb(T + "bt", [128, NH, 256], F32)
        b_c = k.buf()
        k.dma("sp", lambda e: e.dma_start(out=sk[:], in_=sinks.partition_broadcast(128)), writes=[b_c])
        k.dma("sp", lambda e: e.dma_start(out=bt[:], in_=att_bias), writes=[b_c])
        nsk = k.sb(T + "nsk", [128, NH], F32)
        k.op("dve", lambda e: e.tensor_scalar(out=nsk[:], in0=sk[:], scalar1=-1.0, scalar2=None, op0=ALU.mult),
             reads=[b_c], writes=[b_c])

        xT = k.sb(T + "xT", [128, 8, HALF], BF16)
        b_xT = k.buf()
        QT = k.sb(T + "QT", [HD, NH, HALF], BF16)
        b_QT = k.buf()
        KT = k.sb(T + "KT", [HD, NKV, SEQ], BF16)
        b_KT = k.buf()
        V = k.sb(T + "V", [128, SEQ // 128, NKV * HD], BF16)
        b_V = k.buf()
        xt = [k.sb(f"{T}xt{i}", [128, D], F32) for i in range(2)]
        b_xt = [k.buf() for _ in range(2)]
        xtb = [k.sb(f"{T}xtb{i}", [128, D], BF16) for i in range(2)]
        b_xtb = [k.buf() for _ in range(2)]
        Sb = [k.sb(f"{T}Sb{i}", [128, 2, 256], F32) for i in range(3)]
        Pm = [k.sb(f"{T}Pm{i}", [128, 2, 256], BF16) for i in range(3)]
        PT = [k.sb(f"{T}PT{i}", [128, 2, 256], BF16) for i in range(3)]
        sm = [k.sb(f"{T}sm{i}", [128, 12], F32) for i in range(4)]
        b_Sb = [k.buf() for _ in range(3)]
        b_Pm = [k.buf() for _ in range(3)]
        b_PT = [k.buf() for _ in range(3)]
        b_sm = [[k.buf() for _ in range(6)] for _ in range(4)]
        Ot = [k.sb(f"{T}Ot{i}", [128, D], BF16) for i in range(2)]
        b_Ot = [k.buf() for _ in range(2)]
        OT = [k.sb(f"{T}OT{i}", [128, 8, 128], BF16) for i in range(2)]
        b_OT = [k.buf() for _ in range(2)]
        acc = [k.sb(f"{T}acc{i}", [128, D], F32) for i in range(2)]
        b_acc = [k.buf() for _ in range(2)]
        ot = [k.sb(f"{T}ot{i}", [128, D], F32) for i in range(2)]
        b_ot = [k.buf() for _ in range(2)]
        psTb = k.ps(T + "psTb", [128, D], BF16)
        b_psTb = k.buf()
        psS_bk = [k.ps(f"{T}psS{i}", [128, 512], F32) for i in range(2)]
        _bS = [k.buf() for _ in range(2)]
        psS = [psS_bk[i % 2][:, 0:256] for i in range(4)]
        b_psS = [_bS[i % 2] for i in range(4)]
        psPT_bk = [k.ps(f"{T}psPT{i}", [128, 1024], BF16) for i in range(2)]
        _bP = [k.buf() for _ in range(2)]
        psPT = [psPT_bk[i % 2][:, 0:256] for i in range(4)]
        b_psPT = [_bP[i % 2] for i in range(4)]
        psO_bk = [k.ps(f"{T}psO{i}", [128, 512], F32) for i in range(2)]
        _bO = [k.buf() for _ in range(2)]
        psO = [psO_bk[i % 2][:, 0:HD] for i in range(8)]
        b_psO = [_bO[i % 2] for i in range(8)]
        pE = k.ps(T + "pE", [128, 512], F32)
        b_pE = k.buf()
        pA = psS_bk
        b_pA = _bS
        npa = [0]

        def next_pa():
            npa[0] += 1
            return npa[0] % 2

        def qs(q):
            return slice(q * 512, (q + 1) * 512)

        hcnt = 0
        for sq in range(2):
            for hf in range(SEQ // HALF):
                tok0 = sq * SEQ + hf * HALF
                for tl in range(HALF // 128):
                    s = tl % 2
                    r0 = tok0 + tl * 128
                    k.dma("sp", lambda e: e.dma_start(out=xt[s][:], in_=xin[r0:r0 + 128, :]),
                          reads=[b_xin], writes=[b_xt[s]])
                    k.op("act", lambda e: e.activation(out=xtb[s][:], in_=xt[s][:], func=AF.Copy),
                         reads=[b_xt[s]], writes=[b_xtb[s]])
                    for c in range(8):
                        k.op("pe", lambda e: e.transpose(out=psTb[:, c * 128:(c + 1) * 128],
                                                         in_=xtb[s][:, c * 128:(c + 1) * 128], identity=cm.ident_b),
                             reads=[b_xtb[s], cm.b_cb], writes=[b_psTb])
                    k.op("dve", lambda e: e.tensor_copy(out=xT[:, :, tl * 128:(tl + 1) * 128],
                                                        in_=psTb[:].rearrange("p (c t) -> p c t", c=8)),
                         reads=[b_psTb], writes=[b_xT])
                for h in range(NH):
                    for q in range(HALF // 512):
                        pa = next_pa()
                        for kk in range(8):
                            k.op("pe", lambda e: e.matmul(pA[pa][0:HD, :], lhsT=wq[:, kk, h * HD:(h + 1) * HD],
                                                          rhs=xT[:, kk, qs(q)], start=(kk == 0), stop=(kk == 7)),
                                 reads=[b_w, b_xT], writes=[b_pA[pa]])
                        if (h + q) % 2 == 0:
                            k.op("act", lambda e: e.activation(out=QT[:, h, qs(q)], in_=pA[pa][0:HD, :], func=AF.Copy,
                                                               scale=HD ** -0.5),
                                 reads=[b_pA[pa]], writes=[b_QT])
                        else:
                            k.op("dve", lambda e: e.tensor_scalar(out=QT[:, h, qs(q)], in0=pA[pa][0:HD, :],
                                                                  scalar1=HD ** -0.5, scalar2=None, op0=ALU.mult),
                                 reads=[b_pA[pa]], writes=[b_QT])
                for kv in range(NKV):
                    for q in range(HALF // 512):
                        pa = next_pa()
                        for kk in range(8):
                            k.op("pe", lambda e: e.matmul(pA[pa][0:HD, :], lhsT=wk[:, kk, kv * HD:(kv + 1) * HD],
                                                          rhs=xT[:, kk, qs(q)], start=(kk == 0), stop=(kk == 7)),
                                 reads=[b_w, b_xT], writes=[b_pA[pa]])
                        c0 = hf * HALF + q * 512
                        k.op("act", lambda e: e.activation(out=KT[:, kv, c0:c0 + 512], in_=pA[pa][0:HD, :], func=AF.Copy),
                             reads=[b_pA[pa]], writes=[b_KT])
                for tl in range(HALF // 128):
                    pa = next_pa()
                    for kk in range(8):
                        k.op("pe", lambda e: e.matmul(pA[pa][:, 0:256], lhsT=xT[:, kk, tl * 128:(tl + 1) * 128],
                                                      rhs=wv[:, kk, :], start=(kk == 0), stop=(kk == 7)),
                             reads=[b_w, b_xT], writes=[b_pA[pa]])
                    k.op("dve", lambda e: e.tensor_copy(out=V[:, hf * 8 + tl, :], in_=pA[pa][:, 0:256]),
                         reads=[b_pA[pa]], writes=[b_V])
                NB_ = HALF // 128
                items = [(b, hp) for b in range(NB_) for hp in range(NH // 2)]

                def geom(b):
                    g = hf * NB_ + b
                    has_prev = g > 0
                    cs = slice(0, 256) if has_prev else slice(128, 256)
                    k0 = (g - 1) * 128 if has_prev else 0
                    return g, has_prev, cs, k0, (g + 1) * 128

                def st1(n):
                    b, hp = items[n]
                    g, has_prev, cs, k0, k1 = geom(b)
                    h0 = 2 * hp
                    kv = h0 // 4
                    os_ = b % 2
                    r0 = tok0 + b * 128
                    if hp == 0:
                        k.dma("sp", lambda e: e.dma_start(out=xt[os_][:], in_=xin[r0:r0 + 128, :]),
                              reads=[b_xin], writes=[b_xt[os_]])
                        k.op("act", lambda e: e.activation(out=acc[os_][:], in_=xt[os_][:], func=AF.Copy, scale=ALPHA),
                             reads=[b_xt[os_]], writes=[b_acc[os_]])
                    s2, s3, s4 = n % 2, n % 3, n % 4
                    pS = psS_bk[s2][:].rearrange("p (j c) -> p j c", j=2)
                    for j in range(2):
                        k.op("pe", lambda e: e.matmul(pS[:, j, cs], lhsT=QT[:, h0 + j, b * 128:(b + 1) * 128],
                                                      rhs=KT[:, kv, k0:k1], start=True, stop=True),
                             reads=[b_QT, b_KT], writes=[_bS[s2]])
                    k.op("dve", lambda e: e.tensor_tensor(out=Sb[s3][:, :, cs], in0=pS[:, :, cs], in1=bt[:, h0:h0 + 2, cs],
                                                          op=ALU.add),
                         reads=[_bS[s2], b_c], writes=[b_Sb[s3]])
                    k.op("dve", lambda e: e.tensor_reduce(out=sm[s4][:, 0:2], in_=Sb[s3][:, :, cs], axis=AX.X, op=ALU.max),
                         reads=[b_Sb[s3]], writes=[b_sm[s4][0]])
                    k.op("dve", lambda e: e.scalar_tensor_tensor(out=sm[s4][:, 2:4], in0=sm[s4][:, 0:2], scalar=-1.0,
                                                                 in1=nsk[:, h0:h0 + 2], op0=ALU.mult, op1=ALU.min),
                         reads=[b_sm[s4][0], b_c], writes=[b_sm[s4][1]])
                    for j in range(2):
                        k.op("act", lambda e: e.activation(out=Pm[s3][:, j, cs], in_=Sb[s3][:, j, cs], func=AF.Exp,
                                                           bias=sm[s4][:, 2 + j:3 + j], accum_out=sm[s4][:, 4 + j:5 + j]),
                             reads=[b_Sb[s3], b_sm[s4][1]], writes=[b_Pm[s3], b_sm[s4][2]])
                    k.op("dve", lambda e: e.tensor_tensor(out=sm[s4][:, 6:8], in0=sk[:, h0:h0 + 2], in1=sm[s4][:, 2:4],
                                                          op=ALU.add),
                         reads=[b_sm[s4][1], b_c], writes=[b_sm[s4][3]])
                    k.op("act", lambda e: e.activation(out=sm[s4][:, 6:8], in_=sm[s4][:, 6:8], func=AF.Exp),
                         reads=[b_sm[s4][3]], writes=[b_sm[s4][3]])

                def st2(n):
                    b, hp = items[n]
                    g, has_prev, cs, k0, k1 = geom(b)
                    s2, s3, s4 = n % 2, n % 3, n % 4
                    k.op("dve", lambda e: e.tensor_tensor(out=sm[s4][:, 8:10], in0=sm[s4][:, 4:6], in1=sm[s4][:, 6:8],
                                                          op=ALU.add),
                         reads=[b_sm[s4][2], b_sm[s4][3]], writes=[b_sm[s4][4]])
                    k.op("dve", lambda e: e.reciprocal(out=sm[s4][:, 10:12], in_=sm[s4][:, 8:10]),
                         reads=[b_sm[s4][4]], writes=[b_sm[s4][5]])
                    pP = psPT_bk[s2][:, 0:512].rearrange("p (j c) -> p j c", j=2)
                    for j in range(2):
                        if has_prev:
                            k.op("pe", lambda e: e.transpose(out=pP[:, j, 0:128], in_=Pm[s3][:, j, 0:128],
                                                             identity=cm.ident_b),
                                 reads=[b_Pm[s3], cm.b_cb], writes=[_bP[s2]])
                        k.op("pe", lambda e: e.transpose(out=pP[:, j, 128:256], in_=Pm[s3][:, j, 128:256],
                                                         identity=cm.ident_b),
                             reads=[b_Pm[s3], cm.b_cb], writes=[_bP[s2]])
                    if n % 2 == 0:
                        k.op("act", lambda e: e.activation(out=PT[s3][:, :, cs], in_=pP[:, :, cs], func=AF.Copy),
                             reads=[_bP[s2]], writes=[b_PT[s3]])
                    else:
                        k.op("dve", lambda e: e.tensor_copy(out=PT[s3][:, :, cs], in_=pP[:, :, cs]),
                             reads=[_bP[s2]], writes=[b_PT[s3]])

                def st3(n):
                    b, hp = items[n]
                    g, has_prev, cs, k0, k1 = geom(b)
                    h0 = 2 * hp
                    kv = h0 // 4
                    os_ = b % 2
                    s2, s3, s4 = n % 2, n % 3, n % 4
                    pO_ = psO_bk[s2][:, 0:2 * HD].rearrange("p (j d) -> p j d", j=2)
                    for j in range(2):
                        if has_prev:
                            k.op("pe", lambda e: e.matmul(pO_[:, j, :], lhsT=PT[s3][:, j, 0:128],
                                                          rhs=V[:, g - 1, kv * HD:(kv + 1) * HD], start=True, stop=False),
                                 reads=[b_PT[s3], b_V], writes=[_bO[s2]])
                        k.op("pe", lambda e: e.matmul(pO_[:, j, :], lhsT=PT[s3][:, j, 128:256],
                                                      rhs=V[:, g, kv * HD:(kv + 1) * HD], start=(not has_prev), stop=True),
                             reads=[b_PT[s3], b_V], writes=[_bO[s2]])
                    k.op("dve", lambda e: e.tensor_tensor(
                        out=Ot[os_][:, h0 * HD:(h0 + 2) * HD].rearrange("p (j d) -> p j d", j=2), in0=pO_,
                        in1=sm[s4][:, 10:12].unsqueeze(2).to_broadcast([128, 2, HD]), op=ALU.mult),
                        reads=[_bO[s2], b_sm[s4][5]], writes=[b_Ot[os_]])
                    if hp == NH // 2 - 1:
                        epilogue(b)

                dfr = Deferred()

                def epilogue(b):
                    os_ = b % 2
                    r0 = tok0 + b * 128

                    def e1():
                        for c in range(8):
                            k.op("pe", lambda e: e.transpose(out=psTb[:, c * 128:(c + 1) * 128],
                                                             in_=Ot[os_][:, c * 128:(c + 1) * 128], identity=cm.ident_b),
                                 reads=[b_Ot[os_], cm.b_cb], writes=[b_psTb])
                        k.op("act", lambda e: e.activation(out=OT[os_][:], in_=psTb[:].rearrange("p (c t) -> p c t", c=8),
                                                           func=AF.Copy),
                             reads=[b_psTb], writes=[b_OT[os_]])

                    def e2(hh):
                        def f():
                            for c in range(8):
                                k.op("pe", lambda e: e.matmul(pE[:], lhsT=OT[os_][:, c, :],
                                                              rhs=wo[:, c, hh * 512:(hh + 1) * 512],
                                                              start=(c == 0), stop=(c == 7)),
                                     reads=[b_OT[os_], b_w], writes=[b_pE])
                        return f

                    def e3(hh):
                        def f():
                            k.op("dve", lambda e: e.tensor_tensor(out=acc[os_][:, hh * 512:(hh + 1) * 512],
                                                                  in0=acc[os_][:, hh * 512:(hh + 1) * 512], in1=pE[:],
                                                                  op=ALU.add),
                                 reads=[b_pE, b_acc[os_]], writes=[b_acc[os_]])
                        return f

                    pa_, pb_, pc_ = layer_norm_parts(k, T + "ln", acc[os_], b_acc[os_], ot[os_], b_ot[os_], gam, bet,
                                                     b_gb, os_)

                    def store():
                        k.dma("sp", lambda e: e.dma_start(out=xout[r0:r0 + 128, :], in_=ot[os_][:]),
                              reads=[b_ot[os_]], writes=[b_xout])

                    e1()
                    dfr.add(1, e2(0))
                    dfr.add(2, e3(0))
                    dfr.add(2, e2(1))
                    dfr.add(3, e3(1))
                    dfr.add(3, pa_)
                    dfr.add(4, pb_)
                    dfr.add(5, pc_)
                    dfr.add(6, store)

                run_pipeline(len(items), [st1, st2, st3], dfr)
        barrier(k)
    k.es = es_outer
```

```python
from contextlib import ExitStack

import numpy as np
import concourse.bass as bass
import concourse.mybir as mybir
from concourse.bass_utils import run_bass_kernel_spmd

F32 = mybir.dt.float32
BF16 = mybir.dt.bfloat16
I32 = mybir.dt.int32
AF = mybir.ActivationFunctionType
ALU = mybir.AluOpType
AX = mybir.AxisListType

NCORES = 8
D = 1024
SEQ = 2048
TOK = 4096
NT = TOK // 128
NE = 32
DE = 512
CAP = 384
NSLOT = NE * CAP
ALPHA = float((2 * 2) ** 0.25)
LN_EPS = 1e-5
D_RNN = 1280
RC = 80
NRC = D_RNN // RC
NH = 16
NKV = 4
HD = 64


class Buf:
    __slots__ = ("name", "writers", "readers", "nowaw")

    def __init__(self, name, nowaw=False):
        self.name = name
        self.writers = {}
        self.readers = {}
        self.nowaw = nowaw


def _merge(dst, src):
    for k, (s, v) in src.items():
        if k not in dst or dst[k][1] < v:
            dst[k] = (s, v)


class K:
    def __init__(self, nc, es):
        self.nc = nc
        self.es = es
        self.es_root = es
        self.engs = {"pe": nc.tensor, "dve": nc.vector, "act": nc.scalar, "pool": nc.gpsimd, "sp": nc.sync}
        self.sem = {}
        self.cnt = {}
        self.waited = {n: {} for n in self.engs}
        for n in self.engs:
            self.sem[n] = es.enter_context(nc.semaphore("s_" + n))
            self.cnt[n] = 0
        self.ring = {}
        for q, n in (("sp", 10), ("pool", 10), ("act", 4)):
            self.ring[q] = [[es.enter_context(nc.semaphore(f"d_{q}{i}")), 0] for i in range(n)]
        self.ring_pos = {q: 0 for q in self.ring}
        self.nbuf = 0

    def sb(self, name, shape, dt):
        return self.es.enter_context(self.nc.sbuf_tensor(name, list(shape), dt))

    def ps(self, name, shape, dt):
        return self.es.enter_context(self.nc.psum_tensor(name, list(shape), dt))

    def dram(self, name, shape, dt):
        return self.nc.dram_tensor(name, list(shape), dt, kind="Internal").ap()

    def buf(self, name=None, nowaw=False):
        self.nbuf += 1
        return Buf(name or f"b{self.nbuf}", nowaw)

    def _wait(self, engname, deps):
        e = self.engs[engname]
        w = self.waited[engname]
        for k, (s, v) in deps.items():
            if engname == "pe" and k == id(self.sem["pe"]):
                continue
            if w.get(k, 0) >= v:
                continue
            e.wait_ge(s, v)
            w[k] = v

    def _deps(self, reads, writes):
        deps = {}
        for b in reads:
            _merge(deps, b.writers)
        for b in writes:
            if not b.nowaw:
                _merge(deps, b.writers)
            _merge(deps, b.readers)
        return deps

    def _record(self, tok, reads, writes):
        k, s, v = tok
        for b in reads:
            if k not in b.readers or b.readers[k][1] < v:
                b.readers[k] = (s, v)
        for b in writes:
            if b.readers:
                b.writers = {}
                b.readers = {}
            b.writers[k] = (s, v)

    def op(self, engname, fn, reads=(), writes=()):
        deps = self._deps(reads, writes)
        self._wait(engname, deps)
        inst = fn(self.engs[engname])
        s = self.sem[engname]
        self.cnt[engname] += 1
        inst.then_inc(s, 1)
        tok = (id(s), s, self.cnt[engname])
        self._record(tok, reads, writes)
        return tok

    def dma(self, q, fn, reads=(), writes=()):
        deps = self._deps(reads, writes)
        ring = self.ring[q]
        pos = self.ring_pos[q]
        self.ring_pos[q] = (pos + 1) % len(ring)
        ent = ring[pos]
        s = ent[0]
        if ent[1] > 0:
            deps[id(s)] = (s, ent[1])
        self._wait(q, deps)
        inst = fn(self.engs[q])
        ent[1] += 16
        inst.then_inc(s, 16)
        tok = (id(s), s, ent[1])
        self._record(tok, reads, writes)
        return tok

    def finish(self, bufs):
        deps = {}
        for b in bufs:
            _merge(deps, b.writers)
        self._wait("sp", deps)


C_IDENT = 0
C_UPPER = 128
C_ONES = 256
C_EBASE = 384
NCONST = 416


def make_consts():
    c = np.zeros((128, NCONST), np.float32)
    c[:, C_IDENT:C_IDENT + 128] = np.eye(128, dtype=np.float32)
    c[:, C_UPPER:C_UPPER + 128] = np.triu(np.ones((128, 128), np.float32), 1)
    c[:, C_ONES:C_ONES + 128] = 1.0
    c[:, C_EBASE:C_EBASE + NE] = (np.arange(NE, dtype=np.float32) * CAP)[None, :]
    return c


class Common:
    def __init__(self, k, consts_ap):
        self.k = k
        nc = k.nc
        self.cf = k.sb("cf", [128, NCONST], F32)
        self.b_cf = k.buf("cf")
        k.dma("sp", lambda e: e.dma_start(out=self.cf[:], in_=consts_ap), writes=[self.b_cf])
        self.cb = k.sb("cb", [128, 384], BF16)
        self.b_cb = k.buf("cb")
        k.op("dve", lambda e: e.tensor_copy(out=self.cb[:], in_=self.cf[:, 0:384]),
             reads=[self.b_cf], writes=[self.b_cb])
        self.ident_f = self.cf[:, C_IDENT:C_IDENT + 128]
        self.ident_b = self.cb[:, C_IDENT:C_IDENT + 128]
        self.upper_b = self.cb[:, C_UPPER:C_UPPER + 128]
        self.ones_b = self.cb[:, C_ONES:C_ONES + 128]
        self.ebase = self.cf[:, C_EBASE:C_EBASE + NE]
        self.bound_reg = nc.gpsimd.to_reg(NSLOT - 1)
        k.mhalf = k.sb("ln_mhalf", [128, 1], F32)
        k.op("pool", lambda q: q.memset(k.mhalf[:], -0.5))
        barrier(k)


def moe_phase(k, cm, tag, xin, b_xin, xout, b_xout, w_rg, b_rg, w_re, b_re, w1, w3, w2, ln_g, ln_b,
              xbuf, ybuf):
    nc = k.nc
    es_outer = k.es
    with ExitStack() as es:
        k.es = es
        T = tag
        wr = k.sb(T + "wr", [128, 8, 36], F32)
        b_wr = k.buf()
        k.dma("sp", lambda e: e.dma_start(out=wr[:, :, 0:4], in_=w_rg.rearrange("(c p) n -> p c n", p=128)),
              writes=[b_wr])
        k.dma("sp", lambda e: e.dma_start(out=wr[:, :, 4:36], in_=w_re.rearrange("(c p) n -> p c n", p=128)),
              writes=[b_wr])
        rbias = k.sb(T + "rbias", [128, 36], F32)
        b_rbias = k.buf()
        k.dma("sp", lambda e: e.dma_start(out=rbias[:, 0:4], in_=b_rg.partition_broadcast(128)), writes=[b_rbias])
        k.dma("sp", lambda e: e.dma_start(out=rbias[:, 4:36], in_=b_re.partition_broadcast(128)), writes=[b_rbias])
        gam = k.sb(T + "gam", [128, D], F32)
        bet = k.sb(T + "bet", [128, D], F32)
        b_gb = k.buf()
        k.dma("sp", lambda e: e.dma_start(out=gam[:], in_=ln_g.partition_broadcast(128)), writes=[b_gb])
        k.dma("sp", lambda e: e.dma_start(out=bet[:], in_=ln_b.partition_broadcast(128)), writes=[b_gb])
        idx_all = k.sb(T + "idx", [128, NT, 2], I32)
        gate_all = k.sb(T + "gate", [128, NT, 2], F32)
        b_idx = k.buf()
        b_gate = k.buf()

        NWB = 5
        w1t = [k.sb(f"{T}w1_{i}", [128, 8, DE], BF16) for i in range(NWB)]
        w3t = [k.sb(f"{T}w3_{i}", [128, 8, DE], BF16) for i in range(NWB)]
        w2t = [k.sb(f"{T}w2_{i}", [128, 4, D], BF16) for i in range(NWB)]
        b_w13 = [k.buf(nowaw=True) for _ in range(NWB)]
        b_w2 = [k.buf() for _ in range(NWB)]

        def load_weights(e):
            s = e % NWB
            k.dma("pool", lambda g: g.dma_start(out=w1t[s][:], in_=w1[e].rearrange("(c p) n -> p c n", p=128)),
                  writes=[b_w13[s]])
            k.dma("pool", lambda g: g.dma_start(out=w3t[s][:], in_=w3[e].rearrange("(c p) n -> p c n", p=128)),
                  writes=[b_w13[s]])
            k.dma("pool", lambda g: g.dma_start(out=w2t[s][:], in_=w2[e].rearrange("(c p) n -> p c n", p=128)),
                  writes=[b_w2[s]])

        for e in range(NWB):
            load_weights(e)

        with ExitStack() as es2:
            k.es = es2
            lg_all = k.sb(T + "lg_all", [128, NT, 36], F32)
            b_lg = [k.buf() for _ in range(NT)]
            xt = [k.sb(f"{T}xt{i}", [128, D], F32) for i in range(3)]
            b_xt = [k.buf() for _ in range(3)]
            xT = [k.sb(f"{T}xT{i}", [128, 8, 128], F32) for i in range(3)]
            b_xTa = [k.buf() for _ in range(3)]
            b_xTb = [k.buf() for _ in range(3)]
            psT = [k.ps(f"{T}psT{i}", [128, 8, 128], F32) for i in range(2)]
            b_psT = [k.buf() for _ in range(2)]
            psL = [k.ps(f"{T}psL{i}", [128, 512], F32) for i in range(2)]
            b_psL = [k.buf() for _ in range(2)]

            zt = k.sb(T + "zt", [128, 4096], BF16)
            b_zt = k.buf()
            k.op("dve", lambda e: e.memset(zt[:], 0.0), writes=[b_zt])
            b_xz = k.buf(nowaw=True)
            NZ = NSLOT // 512

            def lg0(i):
                s = i % 3
                k.dma("sp", lambda e: e.dma_start(out=xt[s][:], in_=xin[i * 128:(i + 1) * 128, :]),
                      reads=[b_xin], writes=[b_xt[s]])
                if i < NZ:
                    k.dma("sp", lambda e: e.dma_start(
                        out=xbuf[i * 512:(i + 1) * 512, :].rearrange("(p j) d -> p (j d)", j=4), in_=zt[:]),
                        reads=[b_zt], writes=[b_xz])

            def lg1(i):
                s, p = i % 3, i % 2
                for c in range(8):
                    k.op("pe", lambda e: e.transpose(out=psT[p][:, c, :], in_=xt[s][:, c * 128:(c + 1) * 128],
                                                     identity=cm.ident_f),
                         reads=[b_xt[s], cm.b_cf], writes=[b_psT[p]])

            def lg2(i):
                s, p = i % 3, i % 2
                k.op("act", lambda e: e.activation(out=xT[s][:, 0:4, :], in_=psT[p][:, 0:4, :], func=AF.Copy),
                     reads=[b_psT[p]], writes=[b_xTa[s]])
                k.op("dve", lambda e: e.tensor_copy(out=xT[s][:, 4:8, :], in_=psT[p][:, 4:8, :]),
                     reads=[b_psT[p]], writes=[b_xTb[s]])

            def lg3(i):
                s, p = i % 3, i % 2
                for c in range(8):
                    k.op("pe", lambda e: e.matmul(psL[p][:, 0:36], lhsT=xT[s][:, c, :], rhs=wr[:, c, :],
                                                  start=(c == 0), stop=(c == 7)),
                         reads=[b_xTa[s], b_xTb[s], b_wr], writes=[b_psL[p]])

            def lg4(i):
                p = i % 2
                k.op("dve", lambda e: e.tensor_tensor(out=lg_all[:, i, :], in0=psL[p][:, 0:36], in1=rbias[:],
                                                      op=ALU.add),
                     reads=[b_psL[p], b_rbias], writes=[b_lg[i]])

            run_pipeline(NT, [lg0, lg1, lg2, lg3, lg4])

            def st(name, shape, dt=F32):
                return k.sb(T + name, shape, dt)

            lgg = lg_all[:, :, 0:4]
            lge = lg_all[:, :, 4:36]
            gmax = st("gmax", [128, NT])
            ohg = st("ohg", [128, NT, 4])
            egs = st("egs", [128, NT, 4])
            gsum = st("gsum", [128, NT])
            ggate = st("ggate", [128, NT])
            pen = st("pen", [128, NT, 4])
            me = st("me", [128, NT, 32])
            me2 = me
            v1 = st("v1", [128, NT])
            v2 = st("v2", [128, NT])
            sel1 = st("sel1", [128, NT, 32])
            sel2 = st("sel2", [128, NT, 32])
            dd = st("dd", [128, NT])
            p1 = st("p1", [128, NT])
            g1 = st("g1", [128, NT])
            g2 = st("g2", [128, NT])
            A = st("A", [128, NT, 32], BF16)
            dest = st("dest", [128, NT, 32])
            ov = st("ov", [128, NT, 32])
            tmp = st("tmp", [128, NT, 32])
            idxf = st("idxf", [128, NT, 2])
            ovs = st("ovs", [128, NT, 2])
            b_r = k.buf()

            def dv(fn, extra_r=()):
                k.op("dve", fn, reads=[b_r, *extra_r], writes=[b_r])

            def bc(ap, n):
                return ap.unsqueeze(2).to_broadcast([128, NT, n])

            dv(lambda e: e.tensor_reduce(out=gmax[:], in_=lgg, axis=AX.X, op=ALU.max), extra_r=b_lg)
            dv(lambda e: e.tensor_tensor(out=ohg[:], in0=lgg, in1=bc(gmax[:], 4), op=ALU.is_ge))
            dv(lambda e: e.tensor_tensor(out=egs[:], in0=lgg, in1=bc(gmax[:], 4), op=ALU.subtract))
            k.op("act", lambda e: e.activation(out=egs[:], in_=egs[:], func=AF.Exp), reads=[b_r], writes=[b_r])
            dv(lambda e: e.tensor_reduce(out=gsum[:], in_=egs[:], axis=AX.X, op=ALU.add))
            dv(lambda e: e.reciprocal(out=ggate[:], in_=gsum[:]))
            dv(lambda e: e.tensor_scalar(out=pen[:], in0=ohg[:], scalar1=1e30, scalar2=-1e30,
                                         op0=ALU.mult, op1=ALU.add))
            me4 = me[:].rearrange("p t (g j) -> p t g j", g=4)
            lge4 = lge.rearrange("p t (g j) -> p t g j", g=4)
            dv(lambda e: e.tensor_tensor(out=me4, in0=lge4, in1=ohg[:].unsqueeze(3).to_broadcast([128, NT, 4, 8]),
                                         op=ALU.mult))
            dv(lambda e: e.tensor_tensor(out=me4, in0=me4, in1=pen[:].unsqueeze(3).to_broadcast([128, NT, 4, 8]),
                                         op=ALU.add))
            dv(lambda e: e.tensor_reduce(out=v1[:], in_=me[:], axis=AX.X, op=ALU.max))
            dv(lambda e: e.tensor_tensor(out=sel1[:], in0=me[:], in1=bc(v1[:], 32), op=ALU.is_equal))
            dv(lambda e: e.scalar_tensor_tensor(out=me2[:], in0=sel1[:], scalar=-2e30, in1=me[:],
                                                op0=ALU.mult, op1=ALU.add))
            dv(lambda e: e.tensor_reduce(out=v2[:], in_=me2[:], axis=AX.X, op=ALU.max))
            dv(lambda e: e.tensor_tensor(out=sel2[:], in0=me2[:], in1=bc(v2[:], 32), op=ALU.is_equal))
            dv(lambda e: e.tensor_tensor(out=dd[:], in0=v2[:], in1=v1[:], op=ALU.subtract))
            k.op("act", lambda e: e.activation(out=dd[:], in_=dd[:], func=AF.Exp), reads=[b_r], writes=[b_r])
            dv(lambda e: e.tensor_scalar(out=dd[:], in0=dd[:], scalar1=1.0, scalar2=None, op0=ALU.add))
            dv(lambda e: e.reciprocal(out=p1[:], in_=dd[:]))
            dv(lambda e: e.tensor_tensor(out=g1[:], in0=p1[:], in1=ggate[:], op=ALU.mult))
            dv(lambda e: e.tensor_tensor(out=g2[:], in0=ggate[:], in1=g1[:], op=ALU.subtract))
            dv(lambda e: e.tensor_tensor(out=A[:], in0=sel1[:], in1=sel2[:], op=ALU.add))

            psP = k.ps(T + "psP", [128, NT, 32], F32)
            b_psP = k.buf()
            for i in range(NT):
                k.op("pe", lambda e: e.matmul(psP[:, i, :], lhsT=cm.upper_b, rhs=A[:, i, :], start=True,
                                              stop=(i == 0)),
                     reads=[b_r, cm.b_cb], writes=[b_psP])
                for j in range(i):
                    k.op("pe", lambda e: e.matmul(psP[:, i, :], lhsT=cm.ones_b, rhs=A[:, j, :], start=False,
                                                  stop=(j == i - 1)),
                         reads=[b_r, cm.b_cb], writes=[b_psP])

            dv(lambda e: e.tensor_tensor(out=dest[:], in0=psP[:], in1=cm.ebase.unsqueeze(1).to_broadcast([128, NT, 32]),
                                         op=ALU.add), extra_r=[b_psP, cm.b_cf])
            dv(lambda e: e.tensor_scalar(out=ov[:], in0=psP[:], scalar1=float(CAP), scalar2=None, op0=ALU.is_ge),
               extra_r=[b_psP])
            dv(lambda e: e.scalar_tensor_tensor(out=dest[:], in0=ov[:], scalar=1.0e6, in1=dest[:],
                                                op0=ALU.mult, op1=ALU.add))
            for j, sel in enumerate((sel1, sel2)):
                dv(lambda e: e.tensor_tensor(out=tmp[:], in0=sel[:], in1=dest[:], op=ALU.mult))
                dv(lambda e: e.tensor_reduce(out=idxf[:, :, j], in_=tmp[:], axis=AX.X, op=ALU.add))
                dv(lambda e: e.tensor_tensor(out=tmp[:], in0=sel[:], in1=ov[:], op=ALU.mult))
                dv(lambda e: e.tensor_reduce(out=ovs[:, :, j], in_=tmp[:], axis=AX.X, op=ALU.add))
            k.op("dve", lambda e: e.tensor_copy(out=idx_all[:], in_=idxf[:]), reads=[b_r], writes=[b_idx])
            dv(lambda e: e.tensor_scalar(out=ovs[:], in0=ovs[:], scalar1=-1.0, scalar2=1.0, op0=ALU.mult, op1=ALU.add))
            k.op("dve", lambda e: e.tensor_tensor(out=gate_all[:, :, 0], in0=g1[:], in1=ovs[:, :, 0], op=ALU.mult),
                 reads=[b_r], writes=[b_gate])
            k.op("dve", lambda e: e.tensor_tensor(out=gate_all[:, :, 1], in0=g2[:], in1=ovs[:, :, 1], op=ALU.mult),
                 reads=[b_r], writes=[b_gate])

            b_xbuf = k.buf(nowaw=True)
            xs = [xt[0][:], xt[1][:], xt[2][:], xT[0][:].rearrange("p c t -> p (c t)")]
            b_xs = [b_xt[0], b_xt[1], b_xt[2], k.buf()]
            barrier(k)
            for i in range(NT):
                s = i % 4
                k.dma("sp", lambda e: e.dma_start(out=xs[s], in_=xin[i * 128:(i + 1) * 128, :]),
                      reads=[b_xin], writes=[b_xs[s]])
                for j in range(2):
                    k.dma("pool", lambda g: g.indirect_dma_start(
                        out=xbuf, out_offset=bass.IndirectOffsetOnAxis(ap=idx_all[:, i, j:j + 1], axis=0),
                        in_=xs[s], in_offset=None, bounds_check=cm.bound_reg, oob_is_err=False),
                        reads=[b_xs[s], b_idx, b_xz], writes=[b_xbuf])
            barrier(k)
        k.es = es

        b_ybuf = k.buf(nowaw=True)
        with ExitStack() as es2:
            k.es = es2
            NB = CAP // 128
            NXS = 3
            xe = [k.sb(f"{T}xe{i}", [128, NB, D], BF16) for i in range(NXS)]
            b_xe = [k.buf() for _ in range(NXS)]
            xeT = [k.sb(f"{T}xeT{i}", [128, 8, CAP], BF16) for i in range(NXS)]
            b_xeT = [[k.buf() for _ in range(NB)] for _ in range(NXS)]
            hd = [k.sb(f"{T}hd{i}", [128, 4, CAP], BF16) for i in range(2)]
            b_hd = [[k.buf() for _ in range(4)] for _ in range(2)]
            sil = [k.sb(f"{T}sil{i}", [128, CAP], F32) for i in range(2)]
            b_sil = [k.buf() for _ in range(2)]
            yt = [k.sb(f"{T}yt{i}", [128, NB, D], BF16) for i in range(2)]
            b_yt = [[[k.buf() for _ in range(2)] for _ in range(NB)] for _ in range(2)]
            psX = [k.ps(f"{T}psX{i}", [128, D], BF16) for i in range(2)]
            b_psX = [k.buf() for _ in range(2)]
            psH1 = [k.ps(f"{T}psH1_{i}", [128, 512], F32) for i in range(2)]
            psH3 = [k.ps(f"{T}psH3_{i}", [128, 512], F32) for i in range(2)]
            b_psH1 = [k.buf() for _ in range(2)]
            b_psH3 = [k.buf() for _ in range(2)]
            psY = [k.ps(f"{T}psY{i}", [128, 512], F32) for i in range(2)]
            b_psY = [k.buf() for _ in range(2)]
            cntX = [0]

            def stage_load(e):
                s = e % NXS
                k.dma("sp", lambda q: q.dma_start(
                    out=xe[s][:], in_=xbuf[e * CAP:(e + 1) * CAP, :].rearrange("(b p) d -> p b d", p=128)),
                    reads=[b_xbuf], writes=[b_xe[s]])

            def grp_T(e, blk):
                def f():
                    s = e % NXS
                    px = cntX[0] % 2
                    cntX[0] += 1
                    for c in range(8):
                        k.op("pe", lambda q: q.transpose(out=psX[px][:, c * 128:(c + 1) * 128],
                                                         in_=xe[s][:, blk, c * 128:(c + 1) * 128],
                                                         identity=cm.ident_b),
                             reads=[b_xe[s], cm.b_cb], writes=[b_psX[px]])
                    src = psX[px][:].rearrange("p (c t) -> p c t", c=8)
                    if px == 0:
                        k.op("dve", lambda q: q.tensor_copy(out=xeT[s][:, :, blk * 128:(blk + 1) * 128], in_=src),
                             reads=[b_psX[px]], writes=[b_xeT[s][blk]])
                    else:
                        k.op("act", lambda q: q.activation(out=xeT[s][:, :, blk * 128:(blk + 1) * 128], in_=src,
                                                           func=AF.Copy),
                             reads=[b_psX[px]], writes=[b_xeT[s][blk]])
                return f

            def grp_H(e, m):
                def f():
                    s = e % NXS
                    hs = e % 2
                    ws = e % NWB
                    ph = m % 2
                    for c in range(8):
                        k.op("pe", lambda q: q.matmul(psH1[ph][:, 0:CAP], lhsT=w1t[ws][:, c, m * 128:(m + 1) * 128],
                                                      rhs=xeT[s][:, c, :], start=(c == 0), stop=(c == 7)),
                             reads=[*b_xeT[s], b_w13[ws]], writes=[b_psH1[ph]])
                    k.op("act", lambda q: q.activation(out=sil[ph][:], in_=psH1[ph][:, 0:CAP], func=AF.Silu),
                         reads=[b_psH1[ph]], writes=[b_sil[ph]])
                    for c in range(8):
                        k.op("pe", lambda q: q.matmul(psH3[ph][:, 0:CAP], lhsT=w3t[ws][:, c, m * 128:(m + 1) * 128],
                                                      rhs=xeT[s][:, c, :], start=(c == 0), stop=(c == 7)),
                             reads=[*b_xeT[s], b_w13[ws]], writes=[b_psH3[ph]])
                    k.op("dve", lambda q: q.tensor_tensor(out=hd[hs][:, m, :], in0=sil[ph][:], in1=psH3[ph][:, 0:CAP],
                                                          op=ALU.mult),
                         reads=[b_sil[ph], b_psH3[ph]], writes=[b_hd[hs][m]])
                return f

            def grp_Y(e, blk, half):
                def f():
                    hs = e % 2
                    ws = e % NWB
                    py = (blk * 2 + half) % 2
                    for c in range(4):
                        k.op("pe", lambda q: q.matmul(psY[py][:], lhsT=hd[hs][:, c, blk * 128:(blk + 1) * 128],
                                                      rhs=w2t[ws][:, c, half * 512:(half + 1) * 512],
                                                      start=(c == 0), stop=(c == 3)),
                             reads=[b_hd[hs][c], b_w2[ws]], writes=[b_psY[py]])
                    if half == 0:
                        k.op("act", lambda q: q.activation(out=yt[hs][:, blk, 0:512], in_=psY[py][:], func=AF.Copy),
                             reads=[b_psY[py]], writes=[b_yt[hs][blk][0]])
                    else:
                        k.op("dve", lambda q: q.tensor_copy(out=yt[hs][:, blk, 512:1024], in_=psY[py][:]),
                             reads=[b_psY[py]], writes=[b_yt[hs][blk][1]])
                return f

            def store_Y(e):
                hs = e % 2
                k.dma("sp", lambda q: q.dma_start(
                    out=ybuf[e * CAP:(e + 1) * CAP, :].rearrange("(b p) d -> p b d", p=128), in_=yt[hs][:]),
                    reads=[bb for blk in b_yt[hs] for bb in blk], writes=[b_ybuf])

            for e0 in range(min(NXS, NE)):
                stage_load(e0)
            for blk in range(NB):
                grp_T(0, blk)()
            for blk in range(NB):
                grp_T(1, blk)()
            for m in range(4):
                grp_H(0, m)()
            for e in range(NE):
                Tg = [grp_T(e + 2, blk) for blk in range(NB)] if e + 2 < NE else []
                Hg = [grp_H(e + 1, m) for m in range(4)] if e + 1 < NE else []
                Yg = [grp_Y(e, blk, half) for blk in range(NB) for half in range(2)]
                order = []
                ti = hi = 0
                for yi, yg in enumerate(Yg):
                    order.append(yg)
                    if yi % 2 == 0 and hi < len(Hg):
                        order.append(Hg[hi]); hi += 1
                    if yi % 2 == 1 and ti < len(Tg):
                        order.append(Tg[ti]); ti += 1
                order += Hg[hi:] + Tg[ti:]
                for g_ in order:
                    g_()
                store_Y(e)
                if e + NWB < NE:
                    load_weights(e + NWB)
                if e + NXS < NE:
                    stage_load(e + NXS)
            barrier(k)
        k.es = es

        with ExitStack() as es2:
            k.es = es2
            NY, NA = 3, 6
            y1 = [k.sb(f"{T}y1_{i}", [128, D], BF16) for i in range(NY)]
            y2 = [k.sb(f"{T}y2_{i}", [128, D], BF16) for i in range(NY)]
            b_y1 = [k.buf() for _ in range(NY)]
            b_y2 = [k.buf() for _ in range(NY)]
            xr = [k.sb(f"{T}xr_{i}", [128, D], F32) for i in range(NY)]
            b_xr = [k.buf() for _ in range(NY)]
            acc = [k.sb(f"{T}acc_{i}", [128, D], F32) for i in range(NA)]
            b_acc = [k.buf() for _ in range(NA)]
            ot = [k.sb(f"{T}ot_{i}", [128, D], F32) for i in range(3)]
            b_ot = [k.buf() for _ in range(3)]
            for i in range(NY):
                k.op("pool", lambda q: q.memset(y1[i][:], 0.0), writes=[b_y1[i]])
                k.op("pool", lambda q: q.memset(y2[i][:], 0.0), writes=[b_y2[i]])
            lnp = {}

            def cA(i):
                s, sa = i % NY, i % NA
                k.dma("sp", lambda q: q.dma_start(out=xr[s][:], in_=xin[i * 128:(i + 1) * 128, :]),
                      reads=[b_xin], writes=[b_xr[s]])
                k.dma("pool", lambda g: g.indirect_dma_start(
                    out=y1[s][:], out_offset=None, in_=ybuf,
                    in_offset=bass.IndirectOffsetOnAxis(ap=idx_all[:, i, 0:1], axis=0),
                    bounds_check=cm.bound_reg, oob_is_err=False), reads=[b_ybuf, b_idx], writes=[b_y1[s]])
                k.dma("pool", lambda g: g.indirect_dma_start(
                    out=y2[s][:], out_offset=None, in_=ybuf,
                    in_offset=bass.IndirectOffsetOnAxis(ap=idx_all[:, i, 1:2], axis=0),
                    bounds_check=cm.bound_reg, oob_is_err=False), reads=[b_ybuf, b_idx], writes=[b_y2[s]])

            def cB(i):
                s, sa = i % NY, i % NA
                k.op("act", lambda q: q.activation(out=acc[sa][:], in_=xr[s][:], func=AF.Copy, scale=ALPHA),
                     reads=[b_xr[s]], writes=[b_acc[sa]])
                k.op("dve", lambda q: q.scalar_tensor_tensor(out=acc[sa][:], in0=y1[s][:], scalar=gate_all[:, i, 0:1],
                                                             in1=acc[sa][:], op0=ALU.mult, op1=ALU.add),
                     reads=[b_y1[s], b_gate, b_acc[sa]], writes=[b_acc[sa]])
                k.op("dve", lambda q: q.scalar_tensor_tensor(out=acc[sa][:], in0=y2[s][:], scalar=gate_all[:, i, 1:2],
                                                             in1=acc[sa][:], op0=ALU.mult, op1=ALU.add),
                     reads=[b_y2[s], b_gate, b_acc[sa]], writes=[b_acc[sa]])
                lnp[i] = layer_norm_parts(k, T + "ln", acc[sa], b_acc[sa], ot[i % 3], b_ot[i % 3], gam, bet, b_gb, sa)

            def cC(i):
                lnp[i][0]()

            def cD(i):
                lnp[i][1]()

            def cE(i):
                lnp[i][2]()
                k.dma("sp", lambda q: q.dma_start(out=xout[i * 128:(i + 1) * 128, :], in_=ot[i % 3][:]),
                      reads=[b_ot[i % 3]], writes=[b_xout])

            run_pipeline(NT, [cA, cB, cC, cD, cE])
            barrier(k)
        k.es = es
    k.es = es_outer


_ln_scratch = {}


def layer_norm_parts(k, tag, acc, b_acc, ot, b_ot, gam, bet, b_gb, s):
    key = (tag, s, id(k))
    if key not in _ln_scratch:
        _ln_scratch[key] = (k.sb(f"{tag}st{s}", [128, 2, 6], F32), k.sb(f"{tag}mv{s}", [128, 2], F32),
                            k.sb(f"{tag}rs{s}", [128, 1], F32), k.buf())
    stt, mv, rs, b_s = _ln_scratch[key]
    mhalf = k.mhalf

    def part_a():
        for h in range(2):
            k.op("dve", lambda q: q.bn_stats(out=stt[:, h, :], in_=acc[:, h * 512:(h + 1) * 512]),
                 reads=[b_acc], writes=[b_s])
        k.op("dve", lambda q: q.bn_aggr(out=mv[:], in_=stt[:].rearrange("p a b -> p (a b)")), reads=[b_s], writes=[b_s])
        k.op("dve", lambda q: q.tensor_scalar(out=rs[:], in0=mv[:, 1:2], scalar1=LN_EPS, scalar2=None, op0=ALU.add),
             reads=[b_s], writes=[b_s])
        k.op("pool", lambda q: q.tensor_tensor(out=rs[:], in0=rs[:], in1=mhalf[:], op=ALU.pow),
             reads=[b_s], writes=[b_s])

    def part_b():
        k.op("dve", lambda q: q.tensor_scalar(out=acc[:], in0=acc[:], scalar1=mv[:, 0:1], scalar2=rs[:, 0:1],
                                              op0=ALU.subtract, op1=ALU.mult),
             reads=[b_s, b_acc], writes=[b_acc])
        k.op("dve", lambda q: q.tensor_tensor(out=acc[:], in0=acc[:], in1=gam[:], op=ALU.mult),
             reads=[b_acc, b_gb], writes=[b_acc])

    def part_c():
        k.op("dve", lambda q: q.tensor_tensor(out=ot[:], in0=acc[:], in1=bet[:], op=ALU.add),
             reads=[b_acc, b_gb], writes=[b_ot])

    return part_a, part_b, part_c


def layer_norm_tile(k, tag, acc, b_acc, ot, b_ot, gam, bet, b_gb, s):
    for part in layer_norm_parts(k, tag, acc, b_acc, ot, b_ot, gam, bet, b_gb, s):
        part()


class Deferred:
    def __init__(self):
        self.q = []
        self.it = 0

    def add(self, delay, fn):
        self.q.append((self.it + delay, fn))

    def tick(self, it):
        self.it = it + 1
        keep = []
        for due, fn in self.q:
            if due <= it:
                fn()
            else:
                keep.append((due, fn))
        self.q = keep

    def flush(self):
        while self.q:
            q, self.q = self.q, []
            for _, fn in q:
                fn()


def run_pipeline(n, stages, deferred=None):
    S = len(stages)
    for it in range(n + S - 1):
        if deferred is not None:
            deferred.it = it
        for st in range(S):
            i = it - st
            if 0 <= i < n:
                stages[st](i)
        if deferred is not None:
            deferred.tick(it)
    if deferred is not None:
        deferred.flush()


def barrier(k):
    deps = {}
    for n in k.engs:
        if k.cnt[n] > 0:
            s = k.sem[n]
            deps[id(s)] = (s, k.cnt[n])
    for q, ring in k.ring.items():
        for s, v in ring:
            if v > 0:
                deps[id(s)] = (s, v)
    for n in k.engs:
        k._wait(n, dict(deps))


def build_program(phases=("rglru", "moe0", "attn", "moe1")):
    nc = bass.Bass("TRN2", target_bir_lowering=False)

    need_moe = any(p.startswith("moe") for p in phases)

    def inp(name, shape):
        if name.startswith("moe_") and not need_moe:
            return None
        if name.startswith("rec_") and "rglru" not in phases:
            return None
        if name.startswith("att_") and "attn" not in phases:
            return None
        return nc.dram_tensor(name, list(shape), F32, kind="ExternalInput").ap()

    x = inp("x", [TOK, D])
    consts = inp("consts", [128, NCONST])
    moe_w_group = inp("moe_w_group", [2, D, 4])
    moe_b_group = inp("moe_b_group", [2, 4])
    moe_w_expert = inp("moe_w_expert", [2, D, NE])
    moe_b_expert = inp("moe_b_expert", [2, NE])
    moe_w1 = inp("moe_w1", [2, NE, D, DE])
    moe_w3 = inp("moe_w3", [2, NE, D, DE])
    moe_w2 = inp("moe_w2", [2, NE, DE, D])
    ln_g = inp("ln_g", [2, 2, D])
    ln_b = inp("ln_b", [2, 2, D])
    rec_w_in = inp("rec_w_in", [D, 2 * D_RNN])
    rec_cdiag = inp("rec_cdiag", [RC, NRC, 4, RC])
    rec_prm = inp("rec_prm", [RC, 4, NRC])
    rec_w_r = inp("rec_w_r", [NRC, RC, RC])
    rec_w_i = inp("rec_w_i", [NRC, RC, RC])
    rec_w_out = inp("rec_w_out", [D_RNN, D])
    att_w_qkv = inp("att_w_qkv", [D, 1536])
    att_sinks = inp("att_sinks", [NH])
    att_w_o = inp("att_w_o", [D, D])
    att_bias = inp("att_bias", [128, NH, 256])
    att_bias_lo = inp("att_bias_lo", [128, NH, 256])
    out = nc.dram_tensor("out", [TOK, D], F32, kind="ExternalOutput").ap()
    with ExitStack() as es:
        k = K(nc, es)
        cm = Common(k, consts)
        xbuf = k.dram("xbuf", [NSLOT, D], BF16)
        ybuf = k.dram("ybuf", [NSLOT, D], BF16)
        cur, b_cur = x, k.buf("x")
        b_out = k.buf("out", nowaw=True)
        for pi, ph in enumerate(phases):
            last = pi == len(phases) - 1
            if last:
                nxt, b_nxt = out, b_out
            else:
                nxt, b_nxt = k.dram(f"act{pi}", [TOK, D], F32), k.buf(f"act{pi}", nowaw=True)
            if ph == "rglru":
                rglru_phase(k, cm, "r0", cur, b_cur, nxt, b_nxt, rec_w_in, rec_cdiag, rec_prm, rec_w_r, rec_w_i,
                            rec_w_out, ln_g[0, 0], ln_b[0, 0])
            elif ph == "attn":
                attn_phase(k, cm, "a1", cur, b_cur, nxt, b_nxt, att_w_qkv, att_sinks, att_w_o, att_bias, att_bias_lo,
                           ln_g[1, 0], ln_b[1, 0])
            elif ph in ("moe0", "moe1"):
                l = int(ph[3])
                moe_phase(k, cm, f"m{l}", cur, b_cur, nxt, b_nxt, moe_w_group[l], moe_b_group[l], moe_w_expert[l],
                          moe_b_expert[l], moe_w1[l], moe_w3[l], moe_w2[l], ln_g[l, 1], ln_b[l, 1], xbuf, ybuf)
            cur, b_cur = nxt, b_nxt
        if DBG["stop"]:
            k.dma("sp", lambda e: e.dma_start(out=out[0:128, :], in_=x[0:128, :]), writes=[b_out])
        k.finish([b_out])
        barrier(k)
    return nc


def host_inputs(inputs):
    f = lambda a: np.ascontiguousarray(np.asarray(a, dtype=np.float32))
    d = {}
    d["consts"] = make_consts()
    for n in ("moe_w_group", "moe_b_group", "moe_w_expert", "moe_b_expert", "moe_w1", "moe_w3", "moe_w2",
              "ln_g", "ln_b"):
        d[n] = f(inputs[n])
    d["rec_w_in"] = f(inputs["rec_w_in"][0])
    cw = np.asarray(inputs["rec_conv_w"][0], np.float32)
    cdiag = np.zeros((RC, NRC, 4, RC), np.float32)
    pp = np.arange(RC)
    for c in range(NRC):
        for j in range(4):
            cdiag[pp, c, j, pp] = cw[j, c * RC:(c + 1) * RC]
    d["rec_cdiag"] = cdiag
    prm = np.stack([np.asarray(inputs[n][0], np.float32).reshape(NRC, RC).T
                    for n in ("rec_conv_b", "rec_b_r", "rec_b_i", "rec_lambda")], axis=1)
    d["rec_prm"] = f(prm)
    d["rec_w_r"] = f(inputs["rec_w_r"][0])
    d["rec_w_i"] = f(inputs["rec_w_i"][0])
    d["rec_w_out"] = f(inputs["rec_w_out"][0])
    d["att_w_qkv"] = f(inputs["att_w_qkv"][0])
    d["att_sinks"] = f(inputs["att_sinks"][0])
    d["att_w_o"] = f(inputs["att_w_o"][0])
    bias = make_att_bias()
    hi = _bf16_round(bias)
    d["att_bias"] = hi
    d["att_bias_lo"] = _bf16_round(bias - hi)
    return d


def _bf16_round(a):
    u = np.ascontiguousarray(a, dtype=np.float32).view(np.uint32)
    r = ((u >> 16) & 1) + 0x7FFF
    return ((u + r) & 0xFFFF0000).astype(np.uint32).view(np.float32)


def make_att_bias():
    slopes = 2.0 ** (-8.0 * np.arange(1, NH + 1, dtype=np.float32) / NH)
    qi = np.arange(128)[:, None]
    sj = np.arange(256)[None, :]
    dist = qi - sj + 128
    valid = (dist >= 0) & (dist < 128)
    b = np.where(valid[:, None, :], -slopes[None, :, None] * dist[:, None, :].astype(np.float32), -30000.0)
    return np.ascontiguousarray(b.astype(np.float32))


_PROG = {}


def kernel(**inputs):
    key = "full"
    if key not in _PROG:
        _PROG[key] = build_program()
    nc = _PROG[key]
    shared = host_inputs(inputs)
    x = np.ascontiguousarray(np.asarray(inputs["x"], np.float32)).reshape(NCORES, TOK, D)
    in_maps = [dict(shared, x=x[c]) for c in range(NCORES)]
    res = run_bass_kernel_spmd(nc, in_maps, core_ids=list(range(NCORES)))
    out = np.stack([np.asarray(r["out"]) for r in res.results], axis=0)
    return out.reshape(16, SEQ, D).astype(np.float32)


GELU_K = 0.7978845608028654
DBG = {"stop": 0}


class _Stop(Exception):
    pass

TCH = 1024


def rglru_phase(k, cm, T, xin, b_xin, xout, b_xout, w_in, cdiag, prm_d, w_r, w_i, w_out, ln_g, ln_b):
    nc = k.nc
    es_outer = k.es
    with ExitStack() as es:
        k.es = es
        P = RC
        gam = k.sb(T + "gam", [128, D], F32)
        bet = k.sb(T + "bet", [128, D], F32)
        b_gb = k.buf()
        k.dma("sp", lambda e: e.dma_start(out=gam[:], in_=ln_g.partition_broadcast(128)), writes=[b_gb])
        k.dma("sp", lambda e: e.dma_start(out=bet[:], in_=ln_b.partition_broadcast(128)), writes=[b_gb])
        wo_full = k.sb(T + "wo", [128, NRC, D], BF16)
        b_wo = k.buf()
        k.op("pool", lambda e: e.memset(wo_full[:], 0.0), writes=[b_wo])
        wo = wo_full[0:P]
        k.dma("pool", lambda g: g.dma_start(out=wo[:], in_=w_out.rearrange("(c p) n -> p c n", p=P)), writes=[b_wo])
        cd_full = k.sb(T + "cd", [128, NRC, 4, P], BF16)
        b_cd = k.buf()
        k.op("pool", lambda e: e.memset(cd_full[:], 0.0), writes=[b_cd])
        cd = cd_full[0:P]
        for c in range(NRC):
            k.dma("pool", lambda g: g.dma_start(out=cd[:, c, :, :], in_=cdiag[:, c, :, :]), writes=[b_cd])
        wrt_full = k.sb(T + "wrt", [128, NRC, P], BF16)
        b_wri = k.buf()
        k.op("pool", lambda e: e.memset(wrt_full[:], 0.0), writes=[b_wri])
        wrt = wrt_full[0:P]
        wit_full = k.sb(T + "wit", [128, NRC, P], BF16)
        k.op("pool", lambda e: e.memset(wit_full[:], 0.0), writes=[b_wri])
        wit = wit_full[0:P]
        k.dma("pool", lambda g: g.dma_start(out=wrt[:], in_=w_r.rearrange("n k j -> k n j")), writes=[b_wri])
        k.dma("pool", lambda g: g.dma_start(out=wit[:], in_=w_i.rearrange("n k j -> k n j")), writes=[b_wri])
        prm = k.sb(T + "prm", [P, 4, NRC], F32)
        b_prm = k.buf()
        k.dma("sp", lambda e: e.dma_start(out=prm[:], in_=prm_d), writes=[b_prm])
        der = k.sb(T + "der", [P, 5, NRC], F32)
        b_der = k.buf()
        k.op("dve", lambda e: e.tensor_scalar(out=der[:, 0:2, :], in0=prm[:, 1:3, :], scalar1=0.5, scalar2=None,
                                              op0=ALU.mult), reads=[b_prm], writes=[b_der])
        k.op("act", lambda e: e.activation(out=der[:, 4, :], in_=prm[:, 3, :], func=AF.Exp, scale=-1.0),
             reads=[b_prm, b_der], writes=[b_der])
        k.op("act", lambda e: e.activation(out=der[:, 4, :], in_=der[:, 4, :], func=AF.Ln, bias=1.0),
             reads=[b_der], writes=[b_der])
        k.op("dve", lambda e: e.tensor_scalar(out=der[:, 2, :], in0=der[:, 4, :], scalar1=-4.0, scalar2=None,
                                              op0=ALU.mult), reads=[b_der], writes=[b_der])
        k.op("dve", lambda e: e.tensor_scalar(out=der[:, 3, :], in0=der[:, 4, :], scalar1=-8.0, scalar2=None,
                                              op0=ALU.mult), reads=[b_der], writes=[b_der])

        if DBG["stop"] == 1:
            barrier(k); k.es = es_outer; return
        cvb = k.sb(T + "cvb", [P, NRC], F32)
        k.op("dve", lambda e: e.tensor_copy(out=cvb[:], in_=prm[:, 0, :]), reads=[b_prm], writes=[b_prm])
        xT = k.sb(T + "xT", [128, 8, TCH], BF16)
        b_xT = k.buf()
        YT_full = k.sb(T + "YT", [128, NRC, TCH], BF16)
        k.op("pool", lambda e: e.memset(YT_full[:], 0.0))
        YT = YT_full[0:P]
        b_YT = [k.buf() for _ in range(NRC)]
        xrc = k.sb(T + "xrc", [P, NRC, 4], F32)
        hc = k.sb(T + "hc", [P, NRC], F32)
        b_xrc = [k.buf() for _ in range(NRC)]
        b_hc = [k.buf() for _ in range(NRC)]
        xt = [k.sb(f"{T}xt{i}", [128, D], F32) for i in range(2)]
        b_xt = [k.buf() for _ in range(2)]
        xtb = [k.sb(f"{T}xtb{i}", [128, D], BF16) for i in range(2)]
        b_xtb = [k.buf() for _ in range(2)]
        wg = [[k.sb(f"{T}wg{i}_{j}", [128, 8, P], BF16) for j in range(2)] for i in range(2)]
        b_wg = [k.buf(nowaw=True) for _ in range(2)]
        G = [k.sb(f"{T}G{i}", [P, TCH], F32) for i in range(2)]
        XC = [k.sb(f"{T}XC{i}", [P, TCH], F32) for i in range(2)]
        TR = [k.sb(f"{T}TR{i}", [P, TCH], F32) for i in range(2)]
        TI = [k.sb(f"{T}TI{i}", [P, TCH], F32) for i in range(2)]
        NQ = TCH // 512
        b_G = [[k.buf() for _ in range(NQ)] for _ in range(2)]
        b_XC = [[k.buf() for _ in range(NQ)] for _ in range(2)]
        b_TR = [[k.buf() for _ in range(NQ)] for _ in range(2)]
        b_TI = [[k.buf() for _ in range(NQ)] for _ in range(2)]
        AA = k.sb(T + "AA", [P, TCH], F32)
        SQ = k.sb(T + "SQ", [P, TCH], F32)
        S2 = k.sb(T + "S2", [P, TCH], F32)
        HH = SQ
        XR_full = k.sb(T + "XR", [128, TCH + 4], BF16)
        k.op("pool", lambda e: e.memset(XR_full[:], 0.0))
        XR = XR_full[0:P]
        XCb_full = k.sb(T + "XCb", [128, TCH], BF16)
        k.op("pool", lambda e: e.memset(XCb_full[:], 0.0))
        XCb = XCb_full[0:P]
        XRf = k.sb(T + "XRf", [P, TCH + 4], F32)
        b_XRf = k.buf()
        XRb_full = k.sb(T + "XRb", [128, TCH + 4], BF16)
        k.op("pool", lambda e: e.memset(XRb_full[:], 0.0))
        XRb = XRb_full[0:P]
        b_XRb = k.buf()
        b_AA, b_SQ, b_S2, b_XR = [k.buf() for _ in range(4)]
        b_XCb = [k.buf() for _ in range(NQ)]
        NACC = 4
        acc = [k.sb(f"{T}acc{i}", [128, D], F32) for i in range(NACC)]
        b_acc = [k.buf() for _ in range(NACC)]
        psTb = k.ps(T + "psTb", [128, D], BF16)
        b_psTb = k.buf()
        pA = [k.ps(f"{T}pA{i}", [128, 512], F32) for i in range(5)]
        b_pA = [k.buf() for _ in range(5)]
        pO = [k.ps(f"{T}pO{i}", [128, 512], F32) for i in range(2)]
        b_pO = [k.buf() for _ in range(2)]
        barrier(k)
        npa = [0]

        def nxt():
            npa[0] += 1
            return npa[0] % 5

        def qs(q):
            return slice(q * 512, (q + 1) * 512)

        def load_wchunk(c):
            s = c % 2
            k.dma("pool", lambda g: g.dma_start(out=wg[s][0][:],
                                                in_=w_in[:, c * P:(c + 1) * P].rearrange("(c p) n -> p c n", p=128)),
                  writes=[b_wg[s]])
            k.dma("pool", lambda g: g.dma_start(out=wg[s][1][:],
                                                in_=w_in[:, D_RNN + c * P:D_RNN + (c + 1) * P].rearrange(
                                                    "(c p) n -> p c n", p=128)),
                  writes=[b_wg[s]])

        for sq in range(2):
            for th in range(SEQ // TCH):
                tok0 = sq * SEQ + th * TCH
                load_wchunk(0)
                for tl in range(TCH // 128):
                    s = tl % 2
                    r0 = tok0 + tl * 128
                    k.dma("sp", lambda e: e.dma_start(out=xt[s][:], in_=xin[r0:r0 + 128, :]),
                          reads=[b_xin], writes=[b_xt[s]])
                    k.op("act", lambda e: e.activation(out=xtb[s][:], in_=xt[s][:], func=AF.Copy),
                         reads=[b_xt[s]], writes=[b_xtb[s]])
                    for c in range(8):
                        k.op("pe", lambda e: e.transpose(out=psTb[:, c * 128:(c + 1) * 128],
                                                         in_=xtb[s][:, c * 128:(c + 1) * 128], identity=cm.ident_b),
                             reads=[b_xtb[s], cm.b_cb], writes=[b_psTb])
                    k.op("dve", lambda e: e.tensor_copy(out=xT[:, :, tl * 128:(tl + 1) * 128],
                                                        in_=psTb[:].rearrange("p (c t) -> p c t", c=8)),
                         reads=[b_psTb], writes=[b_xT])

                def stA(c):
                    u = c % 2
                    ws = c % 2
                    if c + 1 < NRC:
                        load_wchunk(c + 1)
                    for q in range(NQ):
                        pa = nxt()
                        for kk in range(8):
                            k.op("pe", lambda e: e.matmul(pA[pa][0:P, :], lhsT=wg[ws][0][:, kk, :], rhs=xT[:, kk, qs(q)],
                                                          start=(kk == 0), stop=(kk == 7)),
                                 reads=[b_wg[ws], b_xT], writes=[b_pA[pa]])
                        k.op("act", lambda e: e.activation(out=G[u][:, qs(q)], in_=pA[pa][0:P, :], func=AF.Copy),
                             reads=[b_pA[pa]], writes=[b_G[u][q]])
                    for q in range(NQ):
                        pa = nxt()
                        for kk in range(8):
                            k.op("pe", lambda e: e.matmul(pA[pa][0:P, :], lhsT=wg[ws][1][:, kk, :],
                                                          rhs=xT[:, kk, qs(q)], start=(kk == 0), stop=(kk == 7)),
                                 reads=[b_wg[ws], b_xT], writes=[b_pA[pa]])
                        k.op("dve", lambda e: e.tensor_copy(out=XRf[:, 4 + q * 512:4 + (q + 1) * 512], in_=pA[pa][0:P, :]),
                             reads=[b_pA[pa]], writes=[b_XRf])
                    yield
                    if th == 0:
                        k.op("dve", lambda e: e.memset(XRf[:, 0:4], 0.0), writes=[b_XRf])
                        yield
                    else:
                        k.op("dve", lambda e: e.tensor_copy(out=XRf[:, 0:4], in_=xrc[:, c, :]),
                             reads=[b_xrc[c]], writes=[b_XRf])
                        yield
                    k.op("dve", lambda e: e.tensor_copy(out=xrc[:, c, :], in_=XRf[:, TCH:TCH + 4]),
                         reads=[b_XRf], writes=[b_xrc[c]])
                    yield
                    k.op("dve", lambda e: e.tensor_copy(out=XR[:, 0:TCH + 4], in_=XRf[:, 0:TCH + 4]),
                         reads=[b_XRf], writes=[b_XR])
                    yield
                    k.op("act", lambda e: e.activation(out=XRb[:, 0:TCH + 2], in_=XRf[:, 1:TCH + 3], func=AF.Copy),
                         reads=[b_XRf], writes=[b_XRb])
                    yield
                    for q in range(NQ):
                        pa = nxt()
                        for j in range(4):
                            o = 1 + q * 512 + j
                            src = XR_full[:, o:o + 512] if o % 2 == 0 else XRb_full[:, o - 1:o - 1 + 512]
                            k.op("pe", lambda e: e.matmul(pA[pa][0:P, :], lhsT=cd_full[:, c, j, :], rhs=src,
                                                          start=(j == 0), stop=(j == 3)),
                                 reads=[b_cd, b_XR, b_XRb], writes=[b_pA[pa]])
                            yield
                        k.op("dve", lambda e: e.tensor_scalar(out=XC[u][:, qs(q)], in0=pA[pa][0:P, :],
                                                              scalar1=cvb[:, c:c + 1], scalar2=None, op0=ALU.add),
                             reads=[b_pA[pa], b_prm], writes=[b_XC[u][q]])
                        yield
                        k.op("act", lambda e: e.activation(out=XCb[:, qs(q)], in_=XC[u][:, qs(q)], func=AF.Copy),
                             reads=[b_XC[u][q]], writes=[b_XCb[q]])
                        yield
                    for q in range(NQ):
                        pa = nxt()
                        k.op("pe", lambda e: e.matmul(pA[pa][0:P, :], lhsT=wrt_full[:, c, :], rhs=XCb_full[:, qs(q)],
                                                      start=True, stop=True),
                             reads=[b_wri, b_XCb[q]], writes=[b_pA[pa]])
                        yield
                        k.op("act", lambda e: e.activation(out=TR[u][:, qs(q)], in_=pA[pa][0:P, :], func=AF.Tanh,
                                                           scale=0.5, bias=der[:, 0, c:c + 1]),
                             reads=[b_pA[pa], b_der], writes=[b_TR[u][q]])
                        yield
                        pa = nxt()
                        k.op("pe", lambda e: e.matmul(pA[pa][0:P, :], lhsT=wit_full[:, c, :], rhs=XCb_full[:, qs(q)],
                                                      start=True, stop=True),
                             reads=[b_wri, b_XCb[q]], writes=[b_pA[pa]])
                        yield
                        k.op("act", lambda e: e.activation(out=TI[u][:, qs(q)], in_=pA[pa][0:P, :], func=AF.Tanh,
                                                           scale=0.5, bias=der[:, 1, c:c + 1]),
                             reads=[b_pA[pa], b_der], writes=[b_TI[u][q]])
                        yield

                def stB(c):
                    u = c % 2
                    k.op("act", lambda e: e.activation(out=S2[:], in_=G[u][:], func=AF.Square),
                         reads=b_G[u], writes=[b_S2])
                    yield
                    k.op("dve", lambda e: e.tensor_scalar(out=S2[:], in0=S2[:], scalar1=0.044715, scalar2=1.0,
                                                          op0=ALU.mult, op1=ALU.add), reads=[b_S2], writes=[b_S2])
                    yield
                    k.op("dve", lambda e: e.tensor_tensor(out=S2[:], in0=S2[:], in1=G[u][:], op=ALU.mult),
                         reads=[b_S2, *b_G[u]], writes=[b_S2])
                    yield
                    k.op("act", lambda e: e.activation(out=AA[:], in_=TR[u][:], func=AF.Exp, scale=der[:, 2, c:c + 1],
                                                       bias=der[:, 2, c:c + 1]),
                         reads=[*b_TR[u], b_der], writes=[b_AA])
                    yield
                    k.op("act", lambda e: e.activation(out=SQ[:], in_=TR[u][:], func=AF.Exp, scale=der[:, 3, c:c + 1],
                                                       bias=der[:, 3, c:c + 1]),
                         reads=[*b_TR[u], b_der], writes=[b_SQ])
                    yield
                    k.op("act", lambda e: e.activation(out=S2[:], in_=S2[:], func=AF.Tanh, scale=GELU_K),
                         reads=[b_S2], writes=[b_S2])
                    yield
                    k.op("act", lambda e: e.activation(out=SQ[:], in_=SQ[:], func=AF.Sqrt, scale=-1.0, bias=1.0),
                         reads=[b_SQ], writes=[b_SQ])
                    yield
                    k.op("dve", lambda e: e.scalar_tensor_tensor(out=S2[:], in0=S2[:], scalar=1.0, in1=G[u][:],
                                                                 op0=ALU.add, op1=ALU.mult),
                         reads=[b_S2, *b_G[u]], writes=[b_S2])
                    yield
                    k.op("dve", lambda e: e.scalar_tensor_tensor(out=TI[u][:], in0=TI[u][:], scalar=1.0, in1=XC[u][:],
                                                                 op0=ALU.add, op1=ALU.mult),
                         reads=[*b_TI[u], *b_XC[u]], writes=b_TI[u])
                    yield
                    k.op("dve", lambda e: e.tensor_tensor(out=TI[u][:], in0=TI[u][:], in1=SQ[:], op=ALU.mult),
                         reads=[*b_TI[u], b_SQ], writes=b_TI[u])
                    yield
                    init = 0.0 if th == 0 else hc[:, c:c + 1]
                    k.op("dve", lambda e: e.tensor_tensor_scan(out=HH[:], data0=AA[:], data1=TI[u][:], initial=init,
                                                               op0=ALU.mult, op1=ALU.add),
                         reads=[b_AA, *b_TI[u], b_hc[c], b_SQ], writes=[b_SQ])
                    yield
                    k.op("dve", lambda e: e.tensor_copy(out=hc[:, c:c + 1], in_=HH[:, TCH - 1:TCH]),
                         reads=[b_SQ], writes=[b_hc[c]])
                    yield
                    k.op("dve", lambda e: e.scalar_tensor_tensor(out=YT[:, c, :], in0=S2[:], scalar=0.25, in1=HH[:],
                                                                 op0=ALU.mult, op1=ALU.mult),
                         reads=[b_S2, b_SQ], writes=[b_YT[c]])
                    yield

                def drive(gens):
                    live = list(gens)
                    while live:
                        for g_ in list(live):
                            try:
                                next(g_)
                            except StopIteration:
                                live.remove(g_)

                if DBG.get("var") == "seq":
                    for c in range(NRC):
                        drive([stA(c)])
                        drive([stB(c)])
                else:
                    for t in range(NRC + 1):
                        gens = []
                        if t < NRC:
                            ga = stA(t)
                            next(ga)
                            gens.append(ga)
                        if t >= 1:
                            gens.append(stB(t - 1))
                        drive(gens)

                lnp = {}

                def p0(tl):
                    s, sa = tl % 2, tl % NACC
                    r0 = tok0 + tl * 128
                    k.dma("sp", lambda e: e.dma_start(out=xt[s][:], in_=xin[r0:r0 + 128, :]),
                          reads=[b_xin], writes=[b_xt[s]])
                    k.op("act", lambda e: e.activation(out=acc[sa][:], in_=xt[s][:], func=AF.Copy, scale=ALPHA),
                         reads=[b_xt[s]], writes=[b_acc[sa]])

                def p1(tl):
                    sa = tl % NACC
                    for h in range(2):
                        for c in range(NRC):
                            k.op("pe", lambda e: e.matmul(pO[h][:], lhsT=YT_full[:, c, tl * 128:(tl + 1) * 128],
                                                          rhs=wo_full[:, c, h * 512:(h + 1) * 512],
                                                          start=(c == 0), stop=(c == NRC - 1)),
                                 reads=[b_YT[c], b_wo], writes=[b_pO[h]])
                        k.op("dve", lambda e: e.tensor_tensor(out=acc[sa][:, h * 512:(h + 1) * 512],
                                                              in0=acc[sa][:, h * 512:(h + 1) * 512], in1=pO[h][:],
                                                              op=ALU.add),
                             reads=[b_pO[h], b_acc[sa]], writes=[b_acc[sa]])
                    lnp[tl] = layer_norm_parts(k, T + "ln", acc[sa], b_acc[sa], acc[sa], b_acc[sa], gam, bet, b_gb, sa)

                def p2(tl):
                    lnp[tl][0]()

                def p3(tl):
                    sa = tl % NACC
                    r0 = tok0 + tl * 128
                    lnp[tl][1]()
                    lnp[tl][2]()
                    k.dma("sp", lambda e: e.dma_start(out=xout[r0:r0 + 128, :], in_=acc[sa][:]),
                          reads=[b_acc[sa]], writes=[b_xout])

                run_pipeline(TCH // 128, [p0, p1, p2, p3])
        barrier(k)
    k.es = es_outer


def attn_phase(k, cm, T, xin, b_xin, xout, b_xout, w_qkv, sinks, w_o, att_bias, att_bias_lo, ln_g, ln_b):
    es_outer = k.es
    HALF = 1024
    with ExitStack() as es:
        k.es = es
        gam = k.sb(T + "gam", [128, D], F32)
        bet = k.sb(T + "bet", [128, D], F32)
        b_gb = k.buf()
        k.dma("sp", lambda e: e.dma_start(out=gam[:], in_=ln_g.partition_broadcast(128)), writes=[b_gb])
        k.dma("sp", lambda e: e.dma_start(out=bet[:], in_=ln_b.partition_broadcast(128)), writes=[b_gb])
        wq = k.sb(T + "wq", [128, 8, 1024], BF16)
        wk = k.sb(T + "wk", [128, 8, 256], BF16)
        wv = k.sb(T + "wv", [128, 8, 256], BF16)
        wo = k.sb(T + "wo", [128, 8, 1024], BF16)
        b_w = k.buf(nowaw=True)
        k.dma("pool", lambda g: g.dma_start(out=wq[:], in_=w_qkv[:, 0:1024].rearrange("(c p) n -> p c n", p=128)),
              writes=[b_w])
        k.dma("pool", lambda g: g.dma_start(out=wk[:], in_=w_qkv[:, 1024:1280].rearrange("(c p) n -> p c n", p=128)),
              writes=[b_w])
        k.dma("pool", lambda g: g.dma_start(out=wv[:], in_=w_qkv[:, 1280:1536].rearrange("(c p) n -> p c n", p=128)),
              writes=[b_w])
        k.dma("pool", lambda g: g.dma_start(out=wo[:], in_=w_o.rearrange("(c p) n -> p c n", p=128)), writes=[b_w])
        sk = k.sb(T + "sk", [128, NH], F32)
        bth = k.sb(T + "bth", [128, NH, 256], BF16)
        btl = k.sb(T + "btl", [128, NH, 256], BF16)
        b_c = k.buf()
        b_bt = k.buf(nowaw=True)
        k.dma("sp", lambda e: e.dma_start(out=sk[:], in_=sinks.partition_broadcast(128)), writes=[b_c])
        k.dma("pool", lambda g: g.dma_start(out=bth[:], in_=att_bias), writes=[b_bt])
        k.dma("pool", lambda g: g.dma_start(out=btl[:], in_=att_bias_lo), writes=[b_bt])
        nsk = k.sb(T + "nsk", [128, NH], F32)
        k.op("dve", lambda e: e.tensor_scalar(out=nsk[:], in0=sk[:], scalar1=-1.0, scalar2=None, op0=ALU.mult),
             reads=[b_c], writes=[b_c])

        xT = k.sb(T + "xT", [128, 8, HALF], BF16)
        b_xT = k.buf()
        QT = k.sb(T + "QT", [128, NH // 2, HALF], BF16)
        b_QT = k.buf()
        KT = k.sb(T + "KT", [128, NKV, SEQ], BF16)
        b_KT = k.buf()
        wkd = k.sb(T + "wkd", [128, 8, NKV, 2, HD], BF16)
        b_wkd = k.buf()
        for r in range(2):
            k.op("dve", lambda e: e.tensor_copy(out=wkd[:, :, :, r, :],
                                                in_=wk[:].rearrange("p c (v d) -> p c v d", v=NKV)),
                 reads=[b_w], writes=[b_wkd])
        V = k.sb(T + "V", [128, SEQ // 128, NKV * HD], BF16)
        b_V = k.buf()
        xt = [k.sb(f"{T}xt{i}", [128, D], F32) for i in range(2)]
        b_xt = [k.buf() for _ in range(2)]
        xtb = [k.sb(f"{T}xtb{i}", [128, D], BF16) for i in range(2)]
        b_xtb = [k.buf() for _ in range(2)]
        Pm = [k.sb(f"{T}Pm{i}", [128, 2, 256], BF16) for i in range(3)]
        PT = [k.sb(f"{T}PT{i}", [128, 2, 256], BF16) for i in range(3)]
        sm = [k.sb(f"{T}sm{i}", [128, 12], F32) for i in range(4)]
        b_Sb = [k.buf() for _ in range(3)]
        b_Pm = [k.buf() for _ in range(3)]
        b_PT = [k.buf() for _ in range(3)]
        b_sm = [[k.buf() for _ in range(6)] for _ in range(4)]
        Ot = [k.sb(f"{T}Ot{i}", [128, D], BF16) for i in range(2)]
        b_Ot = [k.buf() for _ in range(2)]
        OT = [k.sb(f"{T}OT{i}", [128, 8, 128], BF16) for i in range(2)]
        b_OT = [k.buf() for _ in range(2)]
        acc = [k.sb(f"{T}acc{i}", [128, D], F32) for i in range(2)]
        b_acc = [k.buf() for _ in range(2)]
        ot = [k.sb(f"{T}ot{i}", [128, D], F32) for i in range(2)]
        b_ot = [k.buf() for _ in range(2)]
        psTb = k.ps(T + "psTb", [128, D], BF16)
        b_psTb = k.buf()
        psS_bk = [k.ps(f"{T}psS{i}", [128, 512], F32) for i in range(2)]
        _bS = [k.buf() for _ in range(2)]
        psS = [psS_bk[i % 2][:, 0:256] for i in range(4)]
        b_psS = [_bS[i % 2] for i in range(4)]
        psPT_bk = [k.ps(f"{T}psPT{i}", [128, 1024], BF16) for i in range(2)]
        _bP = [k.buf() for _ in range(2)]
        psPT = [psPT_bk[i % 2][:, 0:256] for i in range(4)]
        b_psPT = [_bP[i % 2] for i in range(4)]
        psO_bk = [k.ps(f"{T}psO{i}", [128, 512], F32) for i in range(2)]
        _bO = [k.buf() for _ in range(2)]
        psO = [psO_bk[i % 2][:, 0:HD] for i in range(8)]
        b_psO = [_bO[i % 2] for i in range(8)]
        pE = k.ps(T + "pE", [128, 512], F32)
        b_pE = k.buf()
        pA = psS_bk
        b_pA = _bS
        npa = [0]

        def next_pa():
            npa[0] += 1
            return npa[0] % 2

        def qs(q):
            return slice(q * 512, (q + 1) * 512)

        hcnt = 0
        for sq in range(2):
            for hf in range(SEQ // HALF):
                tok0 = sq * SEQ + hf * HALF
                for tl in range(HALF // 128):
                    s = tl % 2
                    r0 = tok0 + tl * 128
                    k.dma("sp", lambda e: e.dma_start(out=xt[s][:], in_=xin[r0:r0 + 128, :]),
                          reads=[b_xin], writes=[b_xt[s]])
                    k.op("act", lambda e: e.activation(out=xtb[s][:], in_=xt[s][:], func=AF.Copy),
                         reads=[b_xt[s]], writes=[b_xtb[s]])
                    for c in range(8):
                        k.op("pe", lambda e: e.transpose(out=psTb[:, c * 128:(c + 1) * 128],
                                                         in_=xtb[s][:, c * 128:(c + 1) * 128], identity=cm.ident_b),
                             reads=[b_xtb[s], cm.b_cb], writes=[b_psTb])
                    k.op("dve", lambda e: e.tensor_copy(out=xT[:, :, tl * 128:(tl + 1) * 128],
                                                        in_=psTb[:].rearrange("p (c t) -> p c t", c=8)),
                         reads=[b_psTb], writes=[b_xT])
                for m in range(NH // 2):
                    for q in range(HALF // 512):
                        pa = next_pa()
                        for kk in range(8):
                            k.op("pe", lambda e: e.matmul(pA[pa][:], lhsT=wq[:, kk, m * 128:(m + 1) * 128],
                                                          rhs=xT[:, kk, qs(q)], start=(kk == 0), stop=(kk == 7)),
                                 reads=[b_w, b_xT], writes=[b_pA[pa]])
                        if (m + q) % 2 == 0:
                            k.op("act", lambda e: e.activation(out=QT[:, m, qs(q)], in_=pA[pa][:], func=AF.Copy,
                                                               scale=HD ** -0.5),
                                 reads=[b_pA[pa]], writes=[b_QT])
                        else:
                            k.op("dve", lambda e: e.tensor_scalar(out=QT[:, m, qs(q)], in0=pA[pa][:],
                                                                  scalar1=HD ** -0.5, scalar2=None, op0=ALU.mult),
                                 reads=[b_pA[pa]], writes=[b_QT])
                for kv in range(NKV):
                    for q in range(HALF // 512):
                        pa = next_pa()
                        for kk in range(8):
                            k.op("pe", lambda e: e.matmul(pA[pa][:],
                                                          lhsT=wkd[:, kk, kv, :, :].rearrange("p r d -> p (r d)"),
                                                          rhs=xT[:, kk, qs(q)], start=(kk == 0), stop=(kk == 7)),
                                 reads=[b_wkd, b_xT], writes=[b_pA[pa]])
                        c0 = hf * HALF + q * 512
                        k.op("act", lambda e: e.activation(out=KT[:, kv, c0:c0 + 512], in_=pA[pa][:], func=AF.Copy),
                             reads=[b_pA[pa]], writes=[b_KT])
                for tl in range(HALF // 128):
                    pa = next_pa()
                    for kk in range(8):
                        k.op("pe", lambda e: e.matmul(pA[pa][:, 0:256], lhsT=xT[:, kk, tl * 128:(tl + 1) * 128],
                                                      rhs=wv[:, kk, :], start=(kk == 0), stop=(kk == 7)),
                             reads=[b_w, b_xT], writes=[b_pA[pa]])
                    k.op("dve", lambda e: e.tensor_copy(out=V[:, hf * 8 + tl, :], in_=pA[pa][:, 0:256]),
                         reads=[b_pA[pa]], writes=[b_V])
                NB_ = HALF // 128
                items = [(b, hp) for b in range(NB_) for hp in range(NH // 2)]

                def geom(b):
                    g = hf * NB_ + b
                    has_prev = g > 0
                    cs = slice(0, 256) if has_prev else slice(128, 256)
                    k0 = (g - 1) * 128 if has_prev else 0
                    return g, has_prev, cs, k0, (g + 1) * 128

                def st1(n):
                    b, hp = items[n]
                    g, has_prev, cs, k0, k1 = geom(b)
                    h0 = 2 * hp
                    kv = h0 // 4
                    os_ = b % 2
                    r0 = tok0 + b * 128
                    if hp == 0:
                        k.dma("sp", lambda e: e.dma_start(out=xt[os_][:], in_=xin[r0:r0 + 128, :]),
                              reads=[b_xin], writes=[b_xt[os_]])
                        k.op("act", lambda e: e.activation(out=acc[os_][:], in_=xt[os_][:], func=AF.Copy, scale=ALPHA),
                             reads=[b_xt[os_]], writes=[b_acc[os_]])
                    s2, s3, s4 = n % 2, n % 3, n % 4
                    pS = psS_bk[s2][:].rearrange("p (j c) -> p j c", j=2)
                    for j in range(2):
                        k.op("pe", lambda e: e.matmul(pS[:, j, cs], lhsT=QT[j * HD:(j + 1) * HD, hp, b * 128:(b + 1) * 128],
                                                      rhs=KT[j * HD:(j + 1) * HD, kv, k0:k1], start=True, stop=False),
                             reads=[b_QT, b_KT], writes=[_bS[s2]])
                        k.op("pe", lambda e: e.matmul(pS[:, j, cs], lhsT=cm.ident_b, rhs=bth[:, h0 + j, cs],
                                                      start=False, stop=False),
                             reads=[b_bt, cm.b_cb], writes=[_bS[s2]])
                        k.op("pe", lambda e: e.matmul(pS[:, j, cs], lhsT=cm.ident_b, rhs=btl[:, h0 + j, cs],
                                                      start=False, stop=True),
                             reads=[b_bt, cm.b_cb], writes=[_bS[s2]])
                    k.op("dve", lambda e: e.tensor_reduce(out=sm[s4][:, 0:2], in_=pS[:, :, cs], axis=AX.X, op=ALU.max),
                         reads=[_bS[s2]], writes=[b_sm[s4][0]])
                    k.op("dve", lambda e: e.scalar_tensor_tensor(out=sm[s4][:, 2:4], in0=sm[s4][:, 0:2], scalar=-1.0,
                                                                 in1=nsk[:, h0:h0 + 2], op0=ALU.mult, op1=ALU.min),
                         reads=[b_sm[s4][0], b_c], writes=[b_sm[s4][1]])
                    for j in range(2):
                        k.op("act", lambda e: e.activation(out=Pm[s3][:, j, cs], in_=pS[:, j, cs], func=AF.Exp,
                                                           bias=sm[s4][:, 2 + j:3 + j], accum_out=sm[s4][:, 4 + j:5 + j]),
                             reads=[_bS[s2], b_sm[s4][1]], writes=[b_Pm[s3], b_sm[s4][2]])
                    k.op("dve", lambda e: e.tensor_tensor(out=sm[s4][:, 6:8], in0=sk[:, h0:h0 + 2], in1=sm[s4][:, 2:4],
                                                          op=ALU.add),
                         reads=[b_sm[s4][1], b_c], writes=[b_sm[s4][3]])
                    k.op("act", lambda e: e.activation(out=sm[s4][:, 6:8], in_=sm[s4][:, 6:8], func=AF.Exp),
                         reads=[b_sm[s4][3]], writes=[b_sm[s4][3]])

                def st2(n):
                    b, hp = items[n]
                    g, has_prev, cs, k0, k1 = geom(b)
                    s2, s3, s4 = n % 2, n % 3, n % 4
                    k.op("dve", lambda e: e.tensor_tensor(out=sm[s4][:, 8:10], in0=sm[s4][:, 4:6], in1=sm[s4][:, 6:8],
                                                          op=ALU.add),
                         reads=[b_sm[s4][2], b_sm[s4][3]], writes=[b_sm[s4][4]])
                    k.op("dve", lambda e: e.reciprocal(out=sm[s4][:, 10:12], in_=sm[s4][:, 8:10]),
                         reads=[b_sm[s4][4]], writes=[b_sm[s4][5]])
                    pP = psPT_bk[s2][:, 0:512].rearrange("p (j c) -> p j c", j=2)
                    for j in range(2):
                        if has_prev:
                            k.op("pe", lambda e: e.transpose(out=pP[:, j, 0:128], in_=Pm[s3][:, j, 0:128],
                                                             identity=cm.ident_b),
                                 reads=[b_Pm[s3], cm.b_cb], writes=[_bP[s2]])
                        k.op("pe", lambda e: e.transpose(out=pP[:, j, 128:256], in_=Pm[s3][:, j, 128:256],
                                                         identity=cm.ident_b),
                             reads=[b_Pm[s3], cm.b_cb], writes=[_bP[s2]])
                    k.op("act", lambda e: e.activation(out=PT[s3][:, :, cs], in_=pP[:, :, cs], func=AF.Copy),
                         reads=[_bP[s2]], writes=[b_PT[s3]])

                def st3(n):
                    b, hp = items[n]
                    g, has_prev, cs, k0, k1 = geom(b)
                    h0 = 2 * hp
                    kv = h0 // 4
                    os_ = b % 2
                    s2, s3, s4 = n % 2, n % 3, n % 4
                    pO_ = psO_bk[s2][:, 0:2 * HD].rearrange("p (j d) -> p j d", j=2)
                    for j in range(2):
                        if has_prev:
                            k.op("pe", lambda e: e.matmul(pO_[:, j, :], lhsT=PT[s3][:, j, 0:128],
                                                          rhs=V[:, g - 1, kv * HD:(kv + 1) * HD], start=True, stop=False),
                                 reads=[b_PT[s3], b_V], writes=[_bO[s2]])
                        k.op("pe", lambda e: e.matmul(pO_[:, j, :], lhsT=PT[s3][:, j, 128:256],
                                                      rhs=V[:, g, kv * HD:(kv + 1) * HD], start=(not has_prev), stop=True),
                             reads=[b_PT[s3], b_V], writes=[_bO[s2]])
                    k.op("dve", lambda e: e.tensor_tensor(
                        out=Ot[os_][:, h0 * HD:(h0 + 2) * HD].rearrange("p (j d) -> p j d", j=2), in0=pO_,
                        in1=sm[s4][:, 10:12].unsqueeze(2).to_broadcast([128, 2, HD]), op=ALU.mult),
                        reads=[_bO[s2], b_sm[s4][5]], writes=[b_Ot[os_]])
                    if hp == NH // 2 - 1:
                        epilogue(b)

                dfr = Deferred()

                def epilogue(b):
                    os_ = b % 2
                    r0 = tok0 + b * 128

                    def e1():
                        for c in range(8):
                            k.op("pe", lambda e: e.transpose(out=psTb[:, c * 128:(c + 1) * 128],
                                                             in_=Ot[os_][:, c * 128:(c + 1) * 128], identity=cm.ident_b),
                                 reads=[b_Ot[os_], cm.b_cb], writes=[b_psTb])
                        k.op("act", lambda e: e.activation(out=OT[os_][:], in_=psTb[:].rearrange("p (c t) -> p c t", c=8),
                                                           func=AF.Copy),
                             reads=[b_psTb], writes=[b_OT[os_]])

                    def e2(hh):
                        def f():
                            for c in range(8):
                                k.op("pe", lambda e: e.matmul(pE[:], lhsT=OT[os_][:, c, :],
                                                              rhs=wo[:, c, hh * 512:(hh + 1) * 512],
                                                              start=(c == 0), stop=(c == 7)),
                                     reads=[b_OT[os_], b_w], writes=[b_pE])
                        return f

                    def e3(hh):
                        def f():
                            k.op("dve", lambda e: e.tensor_tensor(out=acc[os_][:, hh * 512:(hh + 1) * 512],
                                                                  in0=acc[os_][:, hh * 512:(hh + 1) * 512], in1=pE[:],
                                                                  op=ALU.add),
                                 reads=[b_pE, b_acc[os_]], writes=[b_acc[os_]])
                        return f

                    pa_, pb_, pc_ = layer_norm_parts(k, T + "ln", acc[os_], b_acc[os_], ot[os_], b_ot[os_], gam, bet,
                                                     b_gb, os_)

                    def store():
                        k.dma("sp", lambda e: e.dma_start(out=xout[r0:r0 + 128, :], in_=ot[os_][:]),
                              reads=[b_ot[os_]], writes=[b_xout])

                    e1()
                    dfr.add(1, e2(0))
                    dfr.add(2, e3(0))
                    dfr.add(2, e2(1))
                    dfr.add(3, e3(1))
                    dfr.add(3, pa_)
                    dfr.add(4, pb_)
                    dfr.add(5, pc_)
                    dfr.add(6, store)

                run_pipeline(len(items), [st1, st2, st3], dfr)
        barrier(k)
    k.es = es_outer
```
